# Optimizing a Trainium2 kernel written in Bass

```python
import jax
import jax.numpy as jnp
from jax import lax
import numpy as np

D_MODEL = 1024
BATCH = 8
SEQ = 4096
DEPTH = 2

CTX_LEN = 256
GRID_W = 64
M_WIDTH = D_MODEL // 2
M_HEAD_DIM = 128
M_HEADS = M_WIDTH // M_HEAD_DIM
N_WIDTH = D_MODEL - M_WIDTH
N_HEAD_DIM = 64
N_HEADS = N_WIDTH // N_HEAD_DIM
MIX_WIDTH = M_WIDTH + N_WIDTH
G_COLS = 4 * M_HEADS
Q_COLS = 2 * M_WIDTH + N_WIDTH
KV_COLS = 2 * M_WIDTH + G_COLS + 2 * N_WIDTH
N_IN = Q_COLS + KV_COLS
CONV_WIDTH = 3
CHUNK = 128
WIN_H = 8
WIN_W = 16
ROPE_AXIS_DIM = M_HEAD_DIM // 2
ROPE_BASE = 10000.0
N_EXPERTS = 16
N_GROUPS = 4
EXPERTS_PER_GROUP = N_EXPERTS // N_GROUPS
TOP_K = 2
MOE_D_FF = 512
NORM_EPS = 1e-6
F_BIAS_LO = 3.0
F_BIAS_HI = 6.0

kernel_name = "hybrid_mlstm_natten_grouped_moe_dit"


def rmsnorm(x, w):
    x32 = x.astype(jnp.float32)
    y = x32 * lax.rsqrt(jnp.mean(x32 * x32, axis=-1, keepdims=True) + NORM_EPS)
    return (y * w.astype(jnp.float32)).astype(x.dtype)


def modulate(h, shift, scale):
    return h * (1 + scale) + shift


def centred_dwconv(x, w, b):
    L = x.shape[1]
    pad = CONV_WIDTH // 2
    xp = jnp.pad(x, ((0, 0), (pad, pad), (0, 0)))
    y = b
    for j in range(CONV_WIDTH):
        y = y + xp[:, j:j + L] * w[j]
    return y


def axial_rope_tables(L):
    t = jnp.arange(L)
    row = (t // GRID_W).astype(jnp.float32)
    col = (t % GRID_W).astype(jnp.float32)
    inv = ROPE_BASE ** (-jnp.arange(0, ROPE_AXIS_DIM, 2, dtype=jnp.float32) / ROPE_AXIS_DIM)
    ang_r = row[:, None] * inv
    ang_c = col[:, None] * inv
    return (jnp.cos(ang_r)[:, None], jnp.sin(ang_r)[:, None],
            jnp.cos(ang_c)[:, None], jnp.sin(ang_c)[:, None])


def _rotate(x, cos, sin):
    x1, x2 = jnp.split(x, 2, axis=-1)
    cos = cos.astype(x.dtype)
    sin = sin.astype(x.dtype)
    return jnp.concatenate([x1 * cos - x2 * sin, x1 * sin + x2 * cos], axis=-1)


def axial_rope(x, tables):
    cr, sr, cc, sc = tables
    xr, xc = jnp.split(x, 2, axis=-1)
    return jnp.concatenate([_rotate(xr, cr, sr), _rotate(xc, cc, sc)], axis=-1)


def _to_chunks(a):
    B, L = a.shape[:2]
    a = a.reshape((B, L // CHUNK, CHUNK) + a.shape[2:])
    return jnp.moveaxis(jnp.moveaxis(a, 1, 0), 2, 3)


def mlstm_zero_state(B):
    f32 = jnp.float32
    return (jnp.zeros((B, M_HEADS, M_HEAD_DIM, M_HEAD_DIM), f32),
            jnp.zeros((B, M_HEADS, M_HEAD_DIM), f32),
            jnp.zeros((B, M_HEADS), f32))


def mlstm_scan(q, k, v, i_pre, f_pre, state):
    with_out = q is not None
    f32 = jnp.float32
    B, L = k.shape[:2]
    xs = [_to_chunks(a.astype(f32)) for a in (k, v, i_pre, f_pre)]
    if with_out:
        xs.append(_to_chunks(q.astype(f32)))
    causal = jnp.tril(jnp.ones((CHUNK, CHUNK), dtype=bool))

    def step(carry, inp):
        C, n, m = carry
        kc, vc, ic, fc = inp[:4]
        b = jnp.cumsum(jax.nn.log_sigmoid(fc), axis=-1)
        b_end = b[..., -1]
        g = b_end[..., None] - b + ic
        m_new = jnp.maximum(b_end + m, jnp.max(g, axis=-1))
        w_old = jnp.exp(b_end + m - m_new)
        w_tok = jnp.exp(g - m_new[..., None])
        C_new = w_old[..., None, None] * C + jnp.einsum('bhs,bhsd,bhse->bhde', w_tok, kc, vc)
        n_new = w_old[..., None] * n + jnp.einsum('bhs,bhsd->bhd', w_tok, kc)
        if not with_out:
            return (C_new, n_new, m_new), None
        qc = inp[4]
        logd = jnp.where(causal, b[..., :, None] - b[..., None, :] + ic[..., None, :], -jnp.inf)
        inter = b + m[..., None]
        m_row = jnp.maximum(inter, jnp.max(logd, axis=-1))
        dmat = jnp.exp(logd - m_row[..., None])
        w_inter = jnp.exp(inter - m_row)
        s = jnp.einsum('bhtd,bhsd->bhts', qc, kc) * dmat
        num = (jnp.einsum('bhts,bhse->bhte', s, vc)
               + w_inter[..., None] * jnp.einsum('bhtd,bhde->bhte', qc, C))
        qn = jnp.sum(s, axis=-1) + w_inter * jnp.einsum('bhtd,bhd->bht', qc, n)
        den = jnp.maximum(jnp.abs(qn), jnp.exp(-m_row))
        return (C_new, n_new, m_new), num / den[..., None]

    state, h = lax.scan(step, state, tuple(xs))
    if not with_out:
        return state, None
    h = jnp.moveaxis(jnp.moveaxis(h, 3, 2), 0, 1).reshape(B, L, M_HEADS, M_HEAD_DIM)
    return state, h.astype(v.dtype)


def _dir_gates(g, gate_b, d):
    g = g.astype(jnp.float32) + gate_b.astype(jnp.float32)
    i_pre = g[..., (2 * d) * M_HEADS:(2 * d + 1) * M_HEADS]
    f_pre = g[..., (2 * d + 1) * M_HEADS:(2 * d + 2) * M_HEADS]
    return i_pre, f_pre


def _ident(a):
    return a


def _rev(a):
    return jnp.flip(a, axis=1)


def mlstm_bidirectional(q_l, k_l, v_l, g_l, q_c, k_c, v_c, g_c, gate_b):
    B = k_l.shape[0]
    h_lat, h_ctx = [], []
    for d, rev in ((0, _ident), (1, _rev)):
        il, fl = _dir_gates(g_l, gate_b, d)
        ic, fc = _dir_gates(g_c, gate_b, d)
        st, hc = mlstm_scan(None if q_c is None else rev(q_c), rev(k_c), rev(v_c),
                            rev(ic), rev(fc), mlstm_zero_state(B))
        _, hl = mlstm_scan(rev(q_l), rev(k_l), rev(v_l), rev(il), rev(fl), st)
        h_lat.append(rev(hl))
        if q_c is not None:
            h_ctx.append(rev(hc))
    return h_lat[0] + h_lat[1], (h_ctx[0] + h_ctx[1] if q_c is not None else None)


def head_rmsnorm(h, w):
    B, L = h.shape[:2]
    return rmsnorm(h, w.reshape(M_HEADS, M_HEAD_DIM)).reshape(B, L, M_WIDTH)


def neighborhood_attention(q, k, v, k_ctx, v_ctx, rpb):
    B, L, H, hd = q.shape
    rows = L // GRID_W
    kh = min(WIN_H, rows)
    kw = WIN_W
    qg = (q * hd ** -0.5).reshape(B, rows, GRID_W, H, hd)
    kg = k.reshape(B, rows, GRID_W, H, hd)
    vg = v.reshape(B, rows, GRID_W, H, hd)
    cols = jnp.arange(GRID_W)
    col_idx = jnp.clip(cols - kw // 2, 0, GRID_W - kw)[:, None] + jnp.arange(kw)[None, :]
    dc = col_idx - cols[:, None] + (WIN_W - 1)

    def row_block(r):
        rs = jnp.clip(r - kh // 2, 0, rows - kh)
        kb = lax.dynamic_slice_in_dim(kg, rs, kh, axis=1)[:, :, col_idx]
        vb = lax.dynamic_slice_in_dim(vg, rs, kh, axis=1)[:, :, col_idx]
        qr = lax.dynamic_index_in_dim(qg, r, axis=1, keepdims=False)
        dr = rs + jnp.arange(kh) - r + (WIN_H - 1)
        bias = jnp.transpose(rpb[:, dr[:, None, None], dc[None]], (0, 2, 1, 3))
        s_win = (jnp.einsum('bwhd,biwjhd->bhwij', qr, kb) + bias).reshape(B, H, GRID_W, kh * kw)
        s_ctx = jnp.einsum('bwhd,bnhd->bhwn', qr, k_ctx)
        p = jax.nn.softmax(jnp.concatenate([s_win, s_ctx], axis=-1).astype(jnp.float32), axis=-1)
        p = p.astype(v.dtype)
        p_win = p[..., :kh * kw].reshape(B, H, GRID_W, kh, kw)
        return (jnp.einsum('bhwij,biwjhd->bwhd', p_win, vb)
                + jnp.einsum('bhwn,bnhd->bwhd', p[..., kh * kw:], v_ctx))

    out = lax.map(row_block, jnp.arange(rows))
    return jnp.moveaxis(out, 0, 1).reshape(B, L, H * hd)


def context_attention(q, k, v):
    B, N, H, hd = q.shape
    s = jnp.einsum('bnhd,bmhd->bhnm', q * hd ** -0.5, k)
    p = jax.nn.softmax(s.astype(jnp.float32), axis=-1).astype(v.dtype)
    return jnp.einsum('bhnm,bmhd->bnhd', p, v).reshape(B, N, H * hd)


def _q_side(p):
    return jnp.split(p, [M_WIDTH, 2 * M_WIDTH], axis=-1)


def _kv_side(p):
    return jnp.split(p, [M_WIDTH, 2 * M_WIDTH, 2 * M_WIDTH + G_COLS,
                         2 * M_WIDTH + G_COLS + N_WIDTH], axis=-1)


def _m_heads(a):
    return a.reshape(a.shape[:2] + (M_HEADS, M_HEAD_DIM))


def _n_heads(a):
    return a.reshape(a.shape[:2] + (N_HEADS, N_HEAD_DIM))


def hybrid_mixer(hx, hc, rope, w_in, conv_w, conv_b, gate_b, mnorm_w, rpb, w_out, with_ctx_out):
    px = hx @ w_in
    pc = hc @ (w_in if with_ctx_out else w_in[:, Q_COLS:])
    mq_l, mo_l, nq_l = _q_side(px[..., :Q_COLS])
    mk_l, mv_l, g_l, nk_l, nv_l = _kv_side(px[..., Q_COLS:])
    if with_ctx_out:
        mq_c, mo_c, nq_c = _q_side(pc[..., :Q_COLS])
        mk_c, mv_c, g_c, nk_c, nv_c = _kv_side(pc[..., Q_COLS:])
    else:
        mk_c, mv_c, g_c, nk_c, nv_c = _kv_side(pc)
    cw_q, cw_k = conv_w[:, :M_WIDTH], conv_w[:, M_WIDTH:]
    cb_q, cb_k = conv_b[:M_WIDTH], conv_b[M_WIDTH:]
    k_scale = M_HEAD_DIM ** -0.5
    q_l = axial_rope(_m_heads(jax.nn.silu(centred_dwconv(mq_l, cw_q, cb_q))), rope)
    k_l = axial_rope(_m_heads(jax.nn.silu(centred_dwconv(mk_l, cw_k, cb_k))), rope) * k_scale
    k_c = _m_heads(jax.nn.silu(centred_dwconv(mk_c, cw_k, cb_k))) * k_scale
    q_c = _m_heads(jax.nn.silu(centred_dwconv(mq_c, cw_q, cb_q))) if with_ctx_out else None
    h_l, h_c = mlstm_bidirectional(q_l, k_l, _m_heads(mv_l), g_l,
                                   q_c, k_c, _m_heads(mv_c), g_c, gate_b)
    m_out_l = head_rmsnorm(h_l, mnorm_w) * jax.nn.sigmoid(mo_l)
    nk_c, nv_c = _n_heads(nk_c), _n_heads(nv_c)
    n_out_l = neighborhood_attention(_n_heads(nq_l), _n_heads(nk_l), _n_heads(nv_l), nk_c, nv_c, rpb)
    y_l = jnp.concatenate([m_out_l, n_out_l], axis=-1) @ w_out
    if not with_ctx_out:
        return y_l, None
    m_out_c = head_rmsnorm(h_c, mnorm_w) * jax.nn.sigmoid(mo_c)
    n_out_c = context_attention(_n_heads(nq_c), nk_c, nv_c)
    y_c = jnp.concatenate([m_out_c, n_out_c], axis=-1) @ w_out
    return y_l, y_c


def grouped_moe(h, router_w, router_b, w1, w3, w2):
    shape = h.shape
    t = h.reshape(-1, shape[-1])
    scores = jax.nn.softmax((t @ router_w).astype(jnp.float32), axis=-1)
    sel = scores + router_b.astype(jnp.float32)
    gscore = jnp.sum(lax.top_k(sel.reshape(-1, N_GROUPS, EXPERTS_PER_GROUP), TOP_K)[0], axis=-1)
    best = jnp.argmax(gscore, axis=-1)
    in_grp = (jnp.arange(N_EXPERTS) // EXPERTS_PER_GROUP)[None, :] == best[:, None]
    _, idx = lax.top_k(jnp.where(in_grp, sel, -jnp.inf), TOP_K)
    wts = jnp.take_along_axis(scores, idx, axis=-1)
    wts = wts / jnp.sum(wts, axis=-1, keepdims=True)
    gate = jnp.sum(jax.nn.one_hot(idx, N_EXPERTS, dtype=jnp.float32) * wts[..., None], axis=1)
    gate = gate.astype(t.dtype)
    y = jnp.zeros_like(t)
    for e in range(N_EXPERTS):
        a = jax.nn.silu(t @ w1[e]) * (t @ w3[e])
        y = y + gate[:, e:e + 1] * (a @ w2[e])
    return y.reshape(shape)


def setup_inputs(seed: int = 0) -> dict:
    key = jax.random.key(seed)
    ks = jax.random.split(key, 24)
    f32 = jnp.float32

    def nrm(k, shape, s):
        return jax.random.normal(k, shape, f32) * s

    gate_base = jnp.concatenate([jnp.zeros((M_HEADS,), f32), jnp.linspace(F_BIAS_LO, F_BIAS_HI, M_HEADS),
                                 jnp.zeros((M_HEADS,), f32), jnp.linspace(F_BIAS_LO, F_BIAS_HI, M_HEADS)])
    return {
        "x": nrm(ks[0], (BATCH, SEQ, D_MODEL), 1.0),
        "c": nrm(ks[1], (BATCH, D_MODEL), 1.0),
        "ctx": nrm(ks[2], (BATCH, CTX_LEN, D_MODEL), 1.0),
        "c_ctx": nrm(ks[3], (D_MODEL,), 1.0),
        "ada_w": nrm(ks[4], (DEPTH, D_MODEL, 6 * D_MODEL), 0.5 * D_MODEL ** -0.5),
        "ada_b": nrm(ks[5], (DEPTH, 6 * D_MODEL), 0.02),
        "norm1_w": 1.0 + nrm(ks[6], (DEPTH, D_MODEL), 0.02),
        "w_in": nrm(ks[7], (DEPTH, D_MODEL, N_IN), D_MODEL ** -0.5),
        "conv_w": nrm(ks[8], (DEPTH, CONV_WIDTH, 2 * M_WIDTH), CONV_WIDTH ** -0.5),
        "conv_b": nrm(ks[9], (DEPTH, 2 * M_WIDTH), 0.02),
        "gate_b": gate_base[None, :] + nrm(ks[10], (DEPTH, G_COLS), 0.1),
        "mnorm_w": 1.0 + nrm(ks[11], (DEPTH, M_WIDTH), 0.02),
        "rpb": nrm(ks[12], (DEPTH, N_HEADS, 2 * WIN_H - 1, 2 * WIN_W - 1), 0.1),
        "w_out": nrm(ks[13], (DEPTH, MIX_WIDTH, D_MODEL), MIX_WIDTH ** -0.5),
        "norm2_w": 1.0 + nrm(ks[14], (DEPTH, D_MODEL), 0.02),
        "router_w": nrm(ks[15], (D_MODEL, N_EXPERTS), D_MODEL ** -0.5),
        "router_b": nrm(ks[16], (N_EXPERTS,), 0.01),
        "exp_w1": nrm(ks[17], (DEPTH, N_EXPERTS, D_MODEL, MOE_D_FF), D_MODEL ** -0.5),
        "exp_w3": nrm(ks[18], (DEPTH, N_EXPERTS, D_MODEL, MOE_D_FF), D_MODEL ** -0.5),
        "exp_w2": nrm(ks[19], (DEPTH, N_EXPERTS, MOE_D_FF, D_MODEL), MOE_D_FF ** -0.5),
        "final_norm_w": 1.0 + nrm(ks[20], (D_MODEL,), 0.02),
    }


def reference(x, c, ctx, c_ctx, ada_w, ada_b, norm1_w, w_in, conv_w, conv_b, gate_b, mnorm_w, rpb,
              w_out, norm2_w, router_w, router_b, exp_w1, exp_w3, exp_w2, final_norm_w):
    L = x.shape[1]
    rope = axial_rope_tables(L)
    s_lat = jax.nn.silu(c)
    s_ctx = jax.nn.silu(c_ctx)
    for layer in range(DEPTH):
        last = layer == DEPTH - 1
        mod_l = jnp.split((s_lat @ ada_w[layer] + ada_b[layer])[:, None, :], 6, axis=-1)
        mod_c = jnp.split(s_ctx @ ada_w[layer] + ada_b[layer], 6, axis=-1)
        hx = modulate(rmsnorm(x, norm1_w[layer]), mod_l[0], mod_l[1])
        hc = modulate(rmsnorm(ctx, norm1_w[layer]), mod_c[0], mod_c[1])
        y_l, y_c = hybrid_mixer(hx, hc, rope, w_in[layer], conv_w[layer], conv_b[layer], gate_b[layer],
                                mnorm_w[layer], rpb[layer], w_out[layer], not last)
        x = x + mod_l[2] * y_l
        hx = modulate(rmsnorm(x, norm2_w[layer]), mod_l[3], mod_l[4])
        if last:
            x = x + mod_l[5] * grouped_moe(hx, router_w, router_b,
                                           exp_w1[layer], exp_w3[layer], exp_w2[layer])
        else:
            ctx = ctx + mod_c[2] * y_c
            hc = modulate(rmsnorm(ctx, norm2_w[layer]), mod_c[3], mod_c[4])
            h_all = grouped_moe(jnp.concatenate([hx, hc], axis=1), router_w, router_b,
                                exp_w1[layer], exp_w3[layer], exp_w2[layer])
            x = x + mod_l[5] * h_all[:, :L]
            ctx = ctx + mod_c[5] * h_all[:, L:]
    return rmsnorm(x, final_norm_w)
```

```python
import numpy as np
from contextlib import ExitStack
import concourse.bass as bass
import concourse.mybir as mybir
from concourse.bass_utils import run_bass_kernel_spmd

F32 = mybir.dt.float32
BF16 = mybir.dt.bfloat16
I32 = mybir.dt.int32
U32 = mybir.dt.uint32
AF = mybir.ActivationFunctionType
ALU = mybir.AluOpType
AX = mybir.AxisListType

SEM_LIMIT = 30000
SAME_ENGINE_SYNC = True
MERGED_MIXER = False
NDMA = 24


class _U:
    __slots__ = ("w", "r")

    def __init__(self):
        self.w = {}
        self.r = {}


class KB:
    def __init__(self, nc, same_engine_sync=True):
        self.nc = nc
        self.E = {"pe": nc.tensor, "act": nc.scalar, "dve": nc.vector, "pool": nc.gpsimd, "sp": nc.sync}
        self.sems = []
        self.cur = {}
        for e in ("pe", "act", "dve", "pool"):
            self.cur[e] = [self._newsem(), 0]
        self.dsem = {q: [self._newsem() for _ in range(NDMA)] for q in ("sp", "pool")}
        self.dval = {q: [0] * NDMA for q in ("sp", "pool")}
        self.di = {"sp": 0, "pool": 0}
        self.waited = {e: {} for e in self.E}
        self.track = {}
        self.ses = same_engine_sync
        self.n_ins = 0
        self.n_wait = 0
        self.out_events = []
        self._uid = 0

    def _newsem(self):
        h = self.nc.alloc_semaphore(f"ks{len(self.sems)}")
        self.sems.append(h)
        return len(self.sems) - 1

    def sb(self, es, shape, dt=F32, name=None):
        self._uid += 1
        return es.enter_context(self.nc.sbuf_tensor(f"{name or 'sb'}_{self._uid}", list(shape), dt)).ap()

    def ps(self, es, shape, dt=F32, name=None):
        self._uid += 1
        esz = 4 if dt in (F32, I32, U32) else 2
        free = 1
        for d_ in shape[1:]:
            free *= d_
        per_bank = 2048 // esz
        nb = (free + per_bank - 1) // per_bank
        flat = es.enter_context(self.nc.psum_tensor(f"{name or 'ps'}_{self._uid}", [128, nb * per_bank], dt)).ap()
        v = flat[0:shape[0], 0:free]
        nd = len(shape) - 1
        if nd == 1:
            return v
        names = "abcd"[:nd]
        pat = f"p ({' '.join(names)}) -> p {' '.join(names)}"
        return v.rearrange(pat, **{names[i]: shape[1 + i] for i in range(1, nd)})

    def dram(self, name, shape, dt=F32, kind="Internal"):
        return self.nc.dram_tensor(name, list(shape), dt, kind=kind).ap()

    @staticmethod
    def _split(x):
        if isinstance(x, tuple):
            return x[0], x[1]
        return x, None

    def _units(self, x):
        ap, key = self._split(x)
        name = ap.tensor.name
        d = self.track.get(name)
        if d is None:
            d = {None: _U()}
            self.track[name] = d
        if key is None:
            return list(d.values())
        u = d.get(key)
        if u is None:
            u = _U()
            d[key] = u
        return [u, d[None]]

    def _wait(self, eng, sid, val, owner):
        if owner == eng and (eng == "pe" or not self.ses):
            return
        if self.waited[eng].get(sid, -1) >= val:
            return
        self.E[eng].wait_ge(self.sems[sid], val)
        self.waited[eng][sid] = val
        self.n_wait += 1

    def _pre(self, eng, outs, ins):
        for x in ins:
            for u in self._units(x):
                for sid, (val, owner) in u.w.items():
                    self._wait(eng, sid, val, owner)
        for x in outs:
            for u in self._units(x):
                for sid, (val, owner) in u.w.items():
                    self._wait(eng, sid, val, owner)
                for sid, (val, owner) in u.r.items():
                    self._wait(eng, sid, val, owner)

    def _post(self, sid, val, owner, outs, ins):
        for x in ins:
            ap, key = self._split(x)
            us = self._units(x)
            if key is not None:
                us = us[:1]
            for u in us:
                u.r[sid] = (val, owner)
        for x in outs:
            ap, key = self._split(x)
            us = self._units(x)
            if key is not None:
                us = us[:1]
            for u in us:
                u.w = {sid: (val, owner)}
                u.r = {}

    def op(self, eng, fn, outs, ins):
        self._pre(eng, outs, ins)
        c = self.cur[eng]
        if c[1] >= SEM_LIMIT:
            c[0] = self._newsem()
            c[1] = 0
        ins_obj = fn()
        c[1] += 1
        ins_obj.then_inc(self.sems[c[0]], 1)
        self._post(c[0], c[1], eng, outs, ins)
        self.n_ins += 1

    def dma(self, out, in_, q="sp", is_output=False, **kw):
        eng = q
        self._pre(eng, [out], [in_])
        i = self.di[q]
        self.di[q] = (i + 1) % NDMA
        sid = self.dsem[q][i]
        dval = self.dval[q]
        if dval[i] > 0:
            self._wait(eng, sid, dval[i], "dma")
        dval[i] += 16
        o, _ = self._split(out)
        s, _ = self._split(in_)
        self.E[eng].dma_start(out=o, in_=s, **kw).then_inc(self.sems[sid], 16)
        self._post(sid, dval[i], "dma", [out], [in_])
        if is_output:
            self.out_events.append((sid, dval[i]))
        self.n_ins += 1

    def barrier(self):
        for eng in ("pe", "act", "dve", "pool", "sp"):
            for e, c in self.cur.items():
                if c[1] > 0 and e != eng:
                    self._wait(eng, c[0], c[1], e)
            for q in ("sp", "pool"):
                for i in range(NDMA):
                    if self.dval[q][i] > 0:
                        self._wait(eng, self.dsem[q][i], self.dval[q][i], "dma")

    def finish(self):
        self.barrier()
        for sid, val in self.out_events:
            self._wait("sp", sid, val, "dma")
        for e, c in self.cur.items():
            if c[1] > 0:
                self._wait("sp", c[0], c[1], e)

    @staticmethod
    def _a(x):
        return x[0] if isinstance(x, tuple) else x

    def mm(self, out, lhsT, rhs, start=True, stop=True):
        a = self._a
        self.op("pe", lambda: self.nc.tensor.matmul(a(out), a(lhsT), a(rhs), start=start, stop=stop),
                [out], [lhsT, rhs])

    def tr(self, out, in_, ident):
        a = self._a
        self.op("pe", lambda: self.nc.tensor.transpose(a(out), a(in_), a(ident)), [out], [in_, ident])

    def act(self, out, in_, func, bias=None, scale=None, eng="act"):
        a = self._a
        kw = {}
        ins = [in_]
        if bias is not None:
            kw["bias"] = a(bias) if not isinstance(bias, (int, float)) else bias
            if not isinstance(bias, (int, float)):
                ins.append(bias)
        if scale is not None:
            kw["scale"] = a(scale) if not isinstance(scale, (int, float)) else scale
            if not isinstance(scale, (int, float)):
                ins.append(scale)
        self.op("act", lambda: self.nc.scalar.activation(out=a(out), in_=a(in_), func=func, **kw), [out], ins)

    def tt(self, out, in0, in1, op, eng="dve"):
        a = self._a
        self.op(eng, lambda: self.E[eng].tensor_tensor(out=a(out), in0=a(in0), in1=a(in1), op=op), [out], [in0, in1])

    def ts(self, out, in0, s1, op0, s2=None, op1=None, eng="dve"):
        a = self._a
        ins = [in0]
        v1 = s1
        v2 = s2
        if not isinstance(s1, (int, float)):
            ins.append(s1)
            v1 = a(s1)
        if s2 is not None and not isinstance(s2, (int, float)):
            ins.append(s2)
            v2 = a(s2)
        kw = {}
        if op1 is not None:
            kw["op1"] = op1
        self.op(eng, lambda: self.E[eng].tensor_scalar(out=a(out), in0=a(in0), scalar1=v1, scalar2=v2, op0=op0, **kw),
                [out], ins)

    def stt(self, out, in0, scalar, in1, op0, op1):
        a = self._a
        ins = [in0, in1]
        v = scalar
        if not isinstance(scalar, (int, float)):
            ins.append(scalar)
            v = a(scalar)
        self.op("dve", lambda: self.nc.vector.scalar_tensor_tensor(out=a(out), in0=a(in0), scalar=v, in1=a(in1),
                                                                    op0=op0, op1=op1), [out], ins)

    def copy(self, out, in_, eng="dve"):
        a = self._a
        if eng == "act":
            self.op("act", lambda: self.nc.scalar.copy(out=a(out), in_=a(in_)), [out], [in_])
        else:
            self.op(eng, lambda: self.E[eng].tensor_copy(out=a(out), in_=a(in_)), [out], [in_])

    def recip(self, out, in_):
        a = self._a
        self.op("dve", lambda: self.nc.vector.reciprocal(out=a(out), in_=a(in_)), [out], [in_])

    def reduce(self, out, in_, op, axis=AX.X):
        a = self._a
        self.op("dve", lambda: self.nc.vector.tensor_reduce(out=a(out), in_=a(in_), axis=axis, op=op), [out], [in_])

    def memset(self, ap, val, eng="dve"):
        a = self._a
        self.op(eng, lambda: self.E[eng].memset(a(ap), val), [ap], [])


D = 1024
L = 4096
NCTX = 256
NT = L + NCTX
NTL = 34
N_IN = 3600
EPS = 1e-6
NE = 16
DFF = 512
C_MQ, C_MO, C_NQ, C_MK, C_MV, C_G, C_NK, C_NV = 0, 512, 1024, 1536, 2048, 2560, 2576, 3088
NEG = -30000.0


def _host_consts():
    f32 = np.float32
    c = {}
    c["ident"] = np.eye(128, dtype=f32)
    s = np.arange(128)
    c["maskF"] = (s[:, None] <= s[None, :]).astype(f32)
    c["maskB"] = (s[:, None] >= s[None, :]).astype(f32)
    c["ones"] = np.ones((128, 128), f32)
    sel = np.zeros((2, 2, 128), f32)
    sel[0, 0, :] = 1.0
    sel[1, 1, :] = 1.0
    c["sel2"] = sel
    t = np.arange(L)
    row = (t // 64).astype(np.float64)
    col = (t % 64).astype(np.float64)
    inv = 10000.0 ** (-np.arange(0, 64, 2, dtype=np.float64) / 64)
    ang = np.zeros((128, L))
    sgn = np.zeros((128, 1))
    for d in range(128):
        i = d % 32
        ang[d] = (row if d < 64 else col) * inv[i]
        sgn[d] = -1.0 if (d % 64) < 32 else 1.0
    cos = np.cos(ang.astype(f32).astype(np.float64))
    sin = np.sin(ang.astype(f32).astype(np.float64)) * sgn
    ks = 128.0 ** -0.5
    c["rope"] = np.stack([cos, sin, cos * ks, sin * ks]).astype(f32)
    perm = np.zeros((128, 128), f32)
    for dp in range(128):
        d = dp + 32 if (dp % 64) < 32 else dp - 32
        perm[d, dp] = 1.0
    c["perm"] = perm
    return c


def _na_index_tables():
    types = [0, 1, 2, 30, 31]
    idx_r = np.zeros((5, 128, 640), np.int64)
    idx_c = np.zeros((5, 128, 640), np.int64)
    valid = np.zeros((5, 128, 640), bool)
    for ti, j in enumerate(types):
        cs = min(max(j - 2, 0), 27)
        for q in range(128):
            r = 2 * j + q // 64
            cq = q % 64
            rs = min(max(r - 4, 0), 56)
            c0 = min(max(cq - 8, 0), 48)
            for key in range(640):
                kr = 2 * cs + key // 64
                kc = key % 64
                if rs <= kr < rs + 8 and c0 <= kc < c0 + 16:
                    valid[ti, q, key] = True
                    idx_r[ti, q, key] = kr - r + 7
                    idx_c[ti, q, key] = kc - cq + 15
    return idx_r, idx_c, valid


_NA_IDX = None


def _na_bias_tables(rpb):
    global _NA_IDX
    if _NA_IDX is None:
        _NA_IDX = _na_index_tables()
    idx_r, idx_c, valid = _NA_IDX
    g = rpb[:, :, idx_r, idx_c]
    g = np.where(valid[None, None], g, np.float32(NEG)).astype(np.float32)
    return np.ascontiguousarray(np.transpose(g, (0, 2, 1, 3, 4)))


class Prog:
    def __init__(self, debug=(), stop=None, nlayers=2):
        self.debug = set(debug)
        self.stop = stop
        self.nlayers = nlayers
        nc = bass.Bass("TRN2", target_bir_lowering=False)
        self.nc = nc
        self.k = KB(nc, same_engine_sync=SAME_ENGINE_SYNC)
        self.inp = {}
        self.dbg = {}

    def din(self, name, shape, dt=F32):
        ap = self.nc.dram_tensor(name, list(shape), dt, kind="ExternalInput").ap()
        self.inp[name] = ap
        return ap

    def dscr(self, name, shape, dt=F32):
        kind = "ExternalOutput" if name in self.debug else "Internal"
        ap = self.nc.dram_tensor(name, list(shape), dt, kind=kind).ap()
        if name in self.debug:
            self.dbg[name] = ap
        return ap

    def build(self):
        nc, k = self.nc, self.k
        I = self.din
        self.x_in = I("x", [L, D])
        self.ctx_in = I("ctxin", [NCTX, D])
        self.sT_in = I("sT", [128, 8, 2])
        self.ada_w = I("ada_w", [2, D, 6 * D])
        self.ada_b2 = I("ada_b2", [2, 2, 6 * D])
        self.n1w = I("n1w", [2, 128, 8])
        self.n2w = I("n2w", [2, 128, 8])
        self.w_in = I("w_in", [2, D, N_IN])
        self.convw = I("convw", [2, 3, 1024])
        self.convb = I("convb", [2, 128, 8])
        self.gate_b = I("gate_b", [2, 16])
        self.mnw = I("mnw", [2, 128, 4])
        self.nabias = I("nabias", [2, 5, 8, 128, 640])
        self.w_out = I("w_out", [2, D, D])
        self.rw = I("rw", [128, 8, 16])
        self.rb = I("rb", [16])
        self.w1 = I("w1", [2, NE, D, DFF])
        self.w3 = I("w3", [2, NE, D, DFF])
        self.w2 = I("w2", [2, NE, DFF, D])
        self.fnw = I("fnw", [D])
        self.c_ident = I("ident", [128, 128])
        self.c_maskF = I("maskF", [128, 128])
        self.c_maskB = I("maskB", [128, 128])
        self.c_ones = I("ones", [128, 128])
        self.c_sel2 = I("sel2", [2, 2, 128])
        self.c_rope = I("rope", [4, 128, L])
        self.c_perm = I("perm", [128, 128])
        self.out = nc.dram_tensor("out", [L, D], F32, kind="ExternalOutput").ap()
        S = self.dscr
        self.xres = S("xres", [NT, D])
        self.qT_d = S("qT_d", [4, 128, NT], BF16)
        self.kT_d = S("kT_d", [4, 128, NT], BF16)
        self.nqT_d = S("nqT_d", [4, 128, NT], BF16)
        self.nkT_d = S("nkT_d", [4, 128, NT], BF16)
        self.mo_d = S("mo_d", [NT, 512])
        self.mv_d = S("mv_d", [NT, 512], BF16)
        self.g_d = S("g_d", [NT, 16])
        self.nv_d = S("nv_d", [NT, 512], BF16)
        self.mixT_d = S("mixT_d", [8, 128, NT], BF16)
        self.hT_dbg = S("hT_dbg", [128, 8, NT + 4], BF16)
        self.mod_dbg = S("mod_dbg", [2, 6 * D])
        self.gate_dbg = S("gate_dbg", [128, NTL, 16])
        self.hs_dbg = S("hs_dbg", [128, NTL, 512])

        with ExitStack() as es:
            self.es = es
            sb = lambda shape, dt=F32, name=None: k.sb(es, shape, dt, name)
            self.ident = sb([128, 128], F32, "ident")
            self.identb = sb([128, 128], BF16, "identb")
            self.maskF = sb([128, 128], F32, "maskF")
            self.maskB = sb([128, 128], F32, "maskB")
            self.ones = sb([128, 128], F32, "ones")
            self.permb = sb([128, 128], BF16, "permb")
            self.sel2 = sb([2, 2, 128], F32, "sel2")
            k.dma(self.ident, self.c_ident)
            k.dma(self.identb, self.c_ident, q="pool")
            k.dma(self.maskF, self.c_maskF)
            k.dma(self.maskB, self.c_maskB)
            k.dma(self.ones, self.c_ones)
            k.dma(self.permb, self.c_perm, q="pool")
            k.dma(self.sel2, self.c_sel2.rearrange("w t p -> t w p"))
            self.rw_sb = sb([128, 8, 16], F32, "rw")
            k.dma(self.rw_sb, self.rw)
            self.rb_bc = sb([128, 16], F32, "rb")
            k.dma(self.rb_bc, self.rb.partition_broadcast(128))
            self.eps_t = sb([128, 1], F32, "eps")
            k.memset(self.eps_t, EPS)
            self.silu_s = sb([128, 8, 2], F32, "silu_s")
            tmp = sb([128, 8, 2], F32, "sT")
            k.dma(tmp, self.sT_in)
            k.act(self.silu_s, tmp, AF.Silu)
            for layer in range(self.nlayers):
                self.layer(layer)
                if self.stop is not None and self.stop[0] == layer:
                    break
            k.finish()
        return nc

    def xsrc(self, layer, i):
        if layer == 0:
            return self.x_in[i * 128:(i + 1) * 128, :] if i < 32 else self.ctx_in[(i - 32) * 128:(i - 31) * 128, :]
        return self.xres[i * 128:(i + 1) * 128, :]

    def alloc_hT(self, es):
        k = self.k
        self.hT = k.sb(es, [128, 8, NT + 4], BF16, "hT")
        for j in range(8):
            for c0 in (0, L + 1, L + 2, NT + 3):
                k.memset(self.hT[:, j, c0:c0 + 1], 0.0, eng="pool")

    def hcol(self, tile_i):
        return 1 + tile_i * 128 if tile_i < 32 else L + 3 + (tile_i - 32) * 128

    def stopped(self, layer, phase):
        return self.stop is not None and self.stop == (layer, phase)


def _layer(self, layer):
    k = self.k
    last = layer == 1
    with ExitStack() as les:
        sbl = lambda shape, dt=F32, name=None: k.sb(les, shape, dt, name)
        self.modcol = sbl([128, 4, 8, 2], F32, "modcol")
        self.ws1 = sbl([128, 8, 2], F32, "ws1")
        self.ws2 = sbl([128, 8, 2], F32, "ws2")
        self.bc = {(v, w): sbl([128, D], F32, f"bc{v}{w}") for v in (2, 5) for w in (0, 1)}
        self.gate_sb = sbl([128, NTL, 16], F32, "gate")
        self.adaln(layer)
        if self.stopped(layer, "A"):
            return
        with ExitStack() as hs:
            self.alloc_hT(hs)
            self.phase_norm1(layer)
            if self.stopped(layer, "B"):
                k.dma(self.hT_dbg, self.hT)
                k.barrier()
                return
            self.phase_proj(layer)
            k.barrier()
        if self.stopped(layer, "C"):
            return
        if MERGED_MIXER:
            self.phase_mix(layer)
        else:
            self.phase_mlstm(layer)
            if self.stopped(layer, "D1"):
                return
            self.phase_na(layer)
        if self.stopped(layer, "D2"):
            return
        with ExitStack() as hs:
            self.alloc_hT(hs)
            self.phase_wout_norm2(layer)
            if self.stopped(layer, "E"):
                k.dma(self.hT_dbg, self.hT)
                k.dma(self.gate_dbg, self.gate_sb)
                k.barrier()
                return
            self.phase_moe(layer)
            k.barrier()


def _adaln(self, layer):
    k = self.k
    with ExitStack() as es:
        sb = lambda shape, dt=F32, name=None: k.sb(es, shape, dt, name)
        wbuf = [sb([128, 8, 512], F32, "adaw") for _ in range(2)]
        self.modrow = sb([2, 6 * D], F32, "modrow")
        adab = sb([2, 6 * D], F32, "adab")
        k.dma(adab, self.ada_b2[layer])
        pss = [k.ps(es, [2, 512], F32, "adaps") for _ in range(2)]
        wv = self.ada_w[layer].rearrange("(j p) n -> p j n", p=128)
        for n in range(12):
            w = wbuf[n % 2]
            k.dma(w, wv[:, :, n * 512:(n + 1) * 512])
            p = pss[n % 2]
            for j in range(8):
                k.mm(p, self.silu_s[:, j, :], w[:, j, :], start=(j == 0), stop=(j == 7))
            k.tt(self.modrow[:, n * 512:(n + 1) * 512], p, adab[:, n * 512:(n + 1) * 512], ALU.add)
        pcol = k.ps(es, [128, 4, 8, 2], F32, "pcol")
        for vi, v in enumerate((0, 1, 3, 4)):
            for j in range(8):
                k.tr(pcol[:, vi, j, :], self.modrow[:, v * D + j * 128: v * D + (j + 1) * 128], self.ident[0:2, 0:2])
        k.copy(self.modcol, pcol)
        nw1 = sb([128, 8], F32, "nw1")
        nw2 = sb([128, 8], F32, "nw2")
        k.dma(nw1, self.n1w[layer])
        k.dma(nw2, self.n2w[layer])
        for w in range(2):
            k.ts(self.ws1[:, :, w], self.modcol[:, 1, :, w], 1.0, ALU.add)
            k.tt(self.ws1[:, :, w], self.ws1[:, :, w], nw1, ALU.mult)
            k.ts(self.ws2[:, :, w], self.modcol[:, 3, :, w], 1.0, ALU.add)
            k.tt(self.ws2[:, :, w], self.ws2[:, :, w], nw2, ALU.mult)
        self.sh1 = self.modcol[:, 0]
        self.sh2 = self.modcol[:, 2]
        pb = [k.ps(es, [128, 512], F32, "pbc") for _ in range(2)]
        n = 0
        for v in (2, 5):
            for w in range(2):
                for half in range(2):
                    p = pb[n % 2]
                    n += 1
                    k.mm(p, self.sel2[:, w, :], self.modrow[:, v * D + half * 512: v * D + (half + 1) * 512])
                    k.copy(self.bc[(v, w)][:, half * 512:(half + 1) * 512], p, eng="act")
        if self.stopped(layer, "A"):
            k.dma(self.mod_dbg, self.modrow)
        k.barrier()


def _norm_stats(self, xt, bufs):
    k = self.k
    junk, xn, st = bufs
    k.act(junk, xt, AF.Square)
    k.reduce(st[:, 0:1], junk, ALU.add)
    k.act(st[:, 1:2], st[:, 0:1], AF.Sqrt, bias=self.eps_t, scale=1.0 / D)
    k.recip(st[:, 2:3], st[:, 1:2])
    k.ts(xn, xt, st[:, 2:3], ALU.mult)


def _norm_tr(self, i, xn, pt, ws, sh, w, r32=None):
    k = self.k
    for j in range(8):
        k.tr(pt[:, j, :], xn[:, j * 128:(j + 1) * 128], self.ident)
    c0 = self.hcol(i)
    for j in range(8):
        dst = (self.hT[:, j, c0:c0 + 128], (i, j))
        if r32 is not None:
            k.act(r32[:, j, :], pt[:, j, :], AF.Identity, bias=sh[:, j, w:w + 1], scale=ws[:, j, w:w + 1])
            k.copy(dst, r32[:, j, :], eng="dve")
        elif i % 2 == 0:
            k.act(dst, pt[:, j, :], AF.Identity, bias=sh[:, j, w:w + 1], scale=ws[:, j, w:w + 1])
        else:
            k.ts(dst, pt[:, j, :], ws[:, j, w:w + 1], ALU.mult, sh[:, j, w:w + 1], ALU.add)


def _phase_norm1(self, layer):
    k = self.k
    with ExitStack() as es:
        sb = lambda shape, dt=F32, name=None: k.sb(es, shape, dt, name)
        xts = [sb([128, D], F32, "xt") for _ in range(3)]
        nb = [(sb([128, D], F32, "junk"), sb([128, D], F32, "xn"), sb([128, 4], F32, "st")) for _ in range(3)]
        pts = [k.ps(es, [128, 8, 128], F32, "ptr") for _ in range(2)]

        def sa(i):
            k.dma(xts[i % 3], self.xsrc(layer, i))
            self.norm_stats(xts[i % 3], nb[i % 3])

        sa(0)
        for i in range(NTL):
            if i + 1 < NTL:
                sa(i + 1)
            self.norm_tr(i, nb[i % 3][1], pts[i % 2], self.ws1, self.sh1, 0 if i < 32 else 1)
        k.barrier()


Prog.layer = _layer
Prog.adaln = _adaln
Prog.norm_stats = _norm_stats
Prog.norm_tr = _norm_tr
Prog.phase_norm1 = _phase_norm1


def _col(v, n):
    return np.ascontiguousarray(np.asarray(v, np.float32).reshape(n, 128).T)


def make_in_maps(inputs):
    f32 = np.float32
    g = {k_: np.asarray(v) for k_, v in inputs.items()}
    consts = _host_consts()
    shared = dict(consts)
    shared["ada_w"] = np.ascontiguousarray(g["ada_w"], f32)
    shared["ada_b2"] = np.ascontiguousarray(np.repeat(g["ada_b"][:, None, :], 2, axis=1), f32)
    shared["n1w"] = np.stack([_col(g["norm1_w"][l], 8) for l in range(2)])
    shared["n2w"] = np.stack([_col(g["norm2_w"][l], 8) for l in range(2)])
    shared["w_in"] = np.ascontiguousarray(g["w_in"], f32)
    shared["convw"] = np.ascontiguousarray(g["conv_w"], f32)
    shared["convb"] = np.stack([_col(g["conv_b"][l], 8) for l in range(2)])
    shared["gate_b"] = np.ascontiguousarray(g["gate_b"], f32)
    shared["mnw"] = np.stack([_col(g["mnorm_w"][l], 4) for l in range(2)])
    shared["nabias"] = _na_bias_tables(np.asarray(g["rpb"], f32))
    shared["w_out"] = np.ascontiguousarray(g["w_out"], f32)
    shared["rw"] = np.ascontiguousarray(np.asarray(g["router_w"], f32).reshape(8, 128, 16).transpose(1, 0, 2))
    shared["rb"] = np.ascontiguousarray(g["router_b"], f32)
    shared["w1"] = np.ascontiguousarray(g["exp_w1"], f32)
    shared["w3"] = np.ascontiguousarray(g["exp_w3"], f32)
    shared["w2"] = np.ascontiguousarray(g["exp_w2"], f32)
    shared["fnw"] = np.ascontiguousarray(g["final_norm_w"], f32)
    cc = _col(g["c_ctx"], 8)
    maps = []
    for b in range(8):
        m = dict(shared)
        m["x"] = np.ascontiguousarray(g["x"][b], f32)
        m["ctxin"] = np.ascontiguousarray(g["ctx"][b], f32)
        m["sT"] = np.ascontiguousarray(np.stack([_col(g["c"][b], 8), cc], axis=-1))
        maps.append(m)
    return maps


_PROG = None


def kernel(**inputs):
    global _PROG
    if _PROG is None:
        p = Prog()
        p.build()
        _PROG = p
    p = _PROG
    maps = make_in_maps(inputs)
    maps = [{n: m[n] for n in p.inp} for m in maps]
    res = run_bass_kernel_spmd(p.nc, maps, core_ids=list(range(8)))
    return np.stack([np.asarray(r["out"], np.float32) for r in res.results])


def _phase_proj(self, layer):
    k = self.k
    wv = self.w_in[layer].rearrange("(j p) n -> p j n", p=128)
    groups = [(g * 512, 512, 1 + g * 512, False) for g in range(8)] + [(L, 256, L + 3, True)]
    ks = 128.0 ** -0.5
    with ExitStack() as es:
        sb = lambda shape, dt=F32, name=None: k.sb(es, shape, dt, name)
        psA = [k.ps(es, [128, 512], F32, "psA") for _ in range(3)]
        psB = [k.ps(es, [128, 512], F32, "psB") for _ in range(2)]
        convb = sb([128, 8], F32, "convb")
        k.dma(convb, self.convb[layer])
        gb_bc = sb([128, 16], F32, "gb_bc")
        k.dma(gb_bc, self.gate_b[layer].partition_broadcast(128))
        with ExitStack() as es1:
            sb1 = lambda shape, dt=F32, name=None: k.sb(es1, shape, dt, name)
            wst = sb1([128, 8, 512], F32, "wst")
            bcw = sb1([128, 3, 512], F32, "bcw")
            wj = sb1([128, 3, 8, 512], BF16, "wj")
            ropes = [sb1([128, 2, 512], F32, "rope") for _ in range(2)]
            xbs = [sb1([128, 512], BF16, "xb") for _ in range(2)]
            t1s = [sb1([128, 512], F32, "t1") for _ in range(2)]
            t2s = [sb1([128, 512], F32, "t2") for _ in range(2)]
            obs = [sb1([128, 512], BF16, "ob") for _ in range(2)]
            n = 0
            for blk, (c0, dst_d, ridx) in enumerate(((C_MQ, self.qT_d, 0), (C_MK, self.kT_d, 2))):
                k.dma(wst, wv[:, :, c0:c0 + 512])
                for j in range(3):
                    k.dma(bcw[:, j, :], self.convw[layer, j, blk * 512:(blk + 1) * 512].partition_broadcast(128))
                for j in range(3):
                    for dch in range(8):
                        k.tt(wj[:, j, dch, :], wst[:, dch, :], bcw[:, j, :], ALU.mult)
                for gi, (t0, nt, hc0, isctx) in enumerate(groups):
                    rp = ropes[gi % 2]
                    if not isctx:
                        k.dma(rp, self.c_rope[ridx:ridx + 2, :, t0:t0 + 512].rearrange("c p n -> p c n"))
                    for hh in range(4):
                        p = psA[n % 3]
                        xb, t1, t2, ob = xbs[n % 2], t1s[n % 2], t2s[n % 2], obs[n % 2]
                        nmm = 0
                        for j in range(3):
                            for dch in range(8):
                                k.mm(p[:, :nt], wj[:, j, dch, hh * 128:(hh + 1) * 128],
                                     self.hT[:, dch, hc0 - 1 + j: hc0 - 1 + j + nt],
                                     start=(nmm == 0), stop=(nmm == 23))
                                nmm += 1
                        k.act(xb[:, :nt], p[:, :nt], AF.Silu, bias=convb[:, blk * 4 + hh: blk * 4 + hh + 1])
                        if not isctx:
                            p2 = psB[n % 2]
                            k.mm(p2, self.permb, xb)
                            k.tt(t1, p2, rp[:, 1, :], ALU.mult)
                            k.tt(t2, xb, rp[:, 0, :], ALU.mult)
                            k.tt(ob, t1, t2, ALU.add)
                            k.dma(dst_d[hh, :, t0:t0 + nt], ob[:, :nt])
                        elif blk == 0:
                            k.dma(dst_d[hh, :, t0:t0 + nt], xb[:, :nt])
                        else:
                            k.act(ob[:, :nt], xb[:, :nt], AF.Copy, scale=ks)
                            k.dma(dst_d[hh, :, t0:t0 + nt], ob[:, :nt])
                        n += 1
            k.barrier()
        with ExitStack() as es2:
            sb2 = lambda shape, dt=F32, name=None: k.sb(es2, shape, dt, name)
            wbs = [sb2([128, 8, 512], BF16, "wb") for _ in range(2)]
            obs = [sb2([128, 512], BF16, "ob2") for _ in range(3)]
            o32s = [sb2([128, 512], F32, "o32") for _ in range(2)]
            n = 0
            wi = 0
            for (c0, dst_d, scale) in ((C_NQ, self.nqT_d, 0.125), (C_NK, self.nkT_d, 1.0)):
                wb = wbs[wi % 2]
                wi += 1
                k.dma(wb, wv[:, :, c0:c0 + 512], q="pool")
                for (t0, nt, hc0, isctx) in groups:
                    for tt_ in range(4):
                        p = psA[n % 3]
                        ob = obs[n % 3]
                        for dch in range(8):
                            k.mm(p[:, :nt], wb[:, dch, tt_ * 128:(tt_ + 1) * 128], self.hT[:, dch, hc0:hc0 + nt],
                                 start=(dch == 0), stop=(dch == 7))
                        k.act(ob[:, :nt], p[:, :nt], AF.Copy, scale=scale)
                        k.dma(dst_d[tt_, :, t0:t0 + nt], ob[:, :nt])
                        n += 1
            for (c0, ncol, kind) in ((C_MO, 512, "mo"), (C_MV, 512, "mv"), (C_G, 16, "g"), (C_NV, 512, "nv")):
                wb = wbs[wi % 2]
                wi += 1
                k.dma(wb[:, :, :ncol], wv[:, :, c0:c0 + ncol], q="pool")
                for i in range(NTL):
                    hc = self.hcol(i)
                    p = psA[n % 3]
                    for dch in range(8):
                        k.mm(p[:, :ncol], self.hT[:, dch, hc:hc + 128], wb[:, dch, :ncol],
                             start=(dch == 0), stop=(dch == 7))
                    rows = slice(i * 128, (i + 1) * 128)
                    if kind == "mo":
                        o = o32s[n % 2]
                        k.act(o, p, AF.Sigmoid)
                        k.dma(self.mo_d[rows, :], o)
                    elif kind == "g":
                        o = o32s[n % 2]
                        k.tt(o[:, :16], p[:, :16], gb_bc, ALU.add)
                        k.dma(self.g_d[rows, :], o[:, :16])
                    else:
                        ob = obs[n % 3]
                        if n % 2 == 0:
                            k.copy(ob, p, eng="act")
                        else:
                            k.copy(ob, p, eng="dve")
                        k.dma((self.mv_d if kind == "mv" else self.nv_d)[rows, :], ob)
                    n += 1
            k.barrier()


Prog.phase_proj = _phase_proj


def _phase_mlstm(self, layer):
    k = self.k
    with ExitStack() as es:
        sb = lambda shape, dt=F32, name=None: k.sb(es, shape, dt, name)
        EB = sb([128, NTL, 8], F32, "EB")
        WW = sb([128, NTL, 8], F32, "WW")
        UU = sb([128, NTL, 8], F32, "UU")
        ET = sb([128, NTL, 8], F32, "ET")
        hsum = sb([128, NTL, 512], F32, "hsum")
        one_c = self.ones[:, 0:1]
        with ExitStack() as es1:
            sb1 = lambda shape, dt=F32, name=None: k.sb(es1, shape, dt, name)
            gall = sb1([128, NTL, 2, 2, 4], F32, "gall")
            k.dma(gall, self.g_d.rearrange("(c p) (d t h) -> p c d t h", p=128, d=2, t=2))
            gv = gall.rearrange("p c d t h -> p d c t h")
            e1 = sb1([128, 2, NTL, 4], F32, "e1")
            l = sb1([128, 2, NTL, 4], F32, "lsp")
            cc = sb1([128, 2, NTL, 4], F32, "cc")
            ct = sb1([128, 2, NTL, 4], F32, "ct")
            t1 = sb1([128, 2, NTL, 4], F32, "gt1")
            t2 = sb1([128, 2, NTL, 4], F32, "gt2")
            pgf = k.ps(es1, [128, NTL, 4], F32, "pgf")
            pgb = k.ps(es1, [128, NTL, 4], F32, "pgb")
            ptot = k.ps(es1, [128, 2, NTL, 4], F32, "ptot")
            k.act(e1, gv[:, :, :, 1, :], AF.Exp, scale=-1.0)
            k.act(l, e1, AF.Ln, bias=one_c)
            k.mm(pgf, self.maskF, l[:, 0])
            k.mm(pgb, self.maskB, l[:, 1])
            k.mm(ptot, self.ones, l)
            k.copy(cc[:, 0], pgf, eng="act")
            k.copy(cc[:, 1], pgb, eng="act")
            k.copy(ct, ptot, eng="act")
            v = lambda X: X.rearrange("p c (d h) -> p d c h", d=2)
            k.act(v(EB), cc, AF.Exp, scale=-1.0)
            k.tt(t1, cc, gv[:, :, :, 0, :], ALU.add)
            k.act(v(WW), t1, AF.Exp)
            k.tt(t2, t1, ct, ALU.subtract)
            k.act(v(UU), t2, AF.Exp)
            k.act(v(ET), ct, AF.Exp, scale=-1.0)
            k.barrier()
        Cst = [sb([128, 129], F32, "Cst") for _ in range(8)]
        Cbf = [sb([128, 129], BF16, "Cbf") for _ in range(8)]
        for hd in range(8):
            k.memset(Cst[hd], 0.0)
            k.memset(Cbf[hd], 0.0, eng="pool")
        qTs = [[sb([128, 4, 128], BF16, "qT") for _ in range(2)] for _ in range(2)]
        kTs = [[sb([128, 4, 128], BF16, "kT") for _ in range(2)] for _ in range(2)]
        vaug = [[sb([128, 4, 129], BF16, "vaug") for _ in range(2)] for _ in range(2)]
        for vv in vaug:
            for v_ in vv:
                k.memset(v_[:, :, 128:129], 1.0)
        ktoks = [sb([128, 128], BF16, "ktok") for _ in range(3)]
        STs = [sb([128, 128], BF16, "ST") for _ in range(3)]
        uvs = [sb([128, 129], BF16, "uv") for _ in range(3)]
        sms = [sb([128, 6, 2, 2], F32, "sm") for _ in range(2)]
        sq = sb([128, 512], F32, "sq")
        tmpm = sb([128, 512], F32, "tmpm")
        mot = sb([128, 512], F32, "mot")
        mtok = sb([128, 512], BF16, "mtok")
        mixT = sb([128, 4, 128], BF16, "mixT")
        hst = sb([128, 12], F32, "hst")
        mnw = sb([128, 4], F32, "mnw")
        k.dma(mnw, self.mnw[layer])
        pks = [k.ps(es, [128, 1024], BF16, "pk") for _ in range(2)]
        pSs = [k.ps(es, [128, 512], F32, "pS") for _ in range(2)]
        pNt = k.ps(es, [128, 2, 512], F32, "pN")
        pCb = k.ps(es, [128, 512], F32, "pCb")
        pfin = k.ps(es, [128, 1024], BF16, "pfin")
        order = {0: [32, 33] + list(range(32)), 1: [33, 32] + list(range(31, -1, -1))}
        masks = (self.maskF, self.maskB)
        items = [(s_, d, h) for s_ in range(NTL) for d in (0, 1) for h in range(4)]
        seen = set()

        def bufs(s_, d):
            return qTs[d][s_ % 2], kTs[d][s_ % 2], vaug[d][s_ % 2]

        def stage_A(n):
            s_, d, h = items[n]
            c = order[d][s_]
            qT, kT, va = bufs(s_, d)
            if h == 0:
                rows = slice(c * 128, (c + 1) * 128)
                k.dma(qT, self.qT_d[:, :, rows].rearrange("h p n -> p h n"))
                k.dma(kT, self.kT_d[:, :, rows].rearrange("h p n -> p h n"))
                k.dma(va[:, :, 0:128], self.mv_d[rows, :].rearrange("p (h e) -> p h e", h=4))
            k.tr(pks[n % 2][:, 0:128], kT[:, h, :], self.identb)
            k.mm(pSs[n % 2][:, 0:128], kT[:, h, :], qT[:, h, :])

        def stage_B(n):
            s_, d, h = items[n]
            c = order[d][s_]
            hd = d * 4 + h
            qT, kT, va = bufs(s_, d)
            ktok, ST, uv = ktoks[n % 3], STs[n % 3], uvs[n % 3]
            k.copy(ktok, pks[n % 2][:, 0:128], eng="act")
            k.stt(ST, pSs[n % 2][:, 0:128], WW[:, c, hd:hd + 1], masks[d], ALU.mult, ALU.mult)
            k.act(uv, va[:, h, :], AF.Copy, scale=UU[:, c, hd:hd + 1])
            pN = pNt[:, h // 2, (h % 2) * 129:(h % 2) * 129 + 129]
            pC = pCb[:, 0:129]
            k.mm(pN, ST, va[:, h, :], start=True, stop=False)
            k.mm(pN, qT[:, h, :], Cbf[hd], start=False, stop=True)
            k.mm(pC, ktok, uv)
            k.stt(Cst[hd], Cst[hd], ET[:, c, hd:hd + 1], pC, ALU.mult, ALU.add)
            k.copy(Cbf[hd], Cst[hd], eng="act")

        def stage_E(s_, d):
            c = order[d][s_]
            sm = sms[d]
            eb4 = EB[:, c, d * 4:(d + 1) * 4].rearrange("p (b h) -> p b h", b=2)
            for b in range(2):
                qn2 = pNt[:, b, 0:258].rearrange("p (h e) -> p h e", e=129)[:, :, 128]
                k.tt(sm[:, 0, b, :], qn2, eb4[:, b, :], ALU.mult)
            k.ts(sm[:, 1], sm[:, 0], -1.0, ALU.mult)
            k.tt(sm[:, 2], sm[:, 0], sm[:, 1], ALU.max)
            k.ts(sm[:, 3], sm[:, 2], 1.0, ALU.max)
            k.recip(sm[:, 4], sm[:, 3])
            k.tt(sm[:, 5], sm[:, 4], eb4, ALU.mult)
            for h in range(4):
                sc = sm[:, 5, h // 2, h % 2:h % 2 + 1]
                num = pNt[:, h // 2, (h % 2) * 129:(h % 2) * 129 + 128]
                dst = (hsum[:, c, h * 128:(h + 1) * 128], c)
                if c not in seen:
                    k.ts(dst, num, sc, ALU.mult)
                else:
                    k.stt(dst, num, sc, dst, ALU.mult, ALU.add)
            if c in seen:
                return c
            seen.add(c)
            return None

        def finalize(c):
            rows = slice(c * 128, (c + 1) * 128)
            hs = (hsum[:, c, :], c)
            k.act(sq, hs, AF.Square)
            k.reduce(hst[:, 0:4], sq.rearrange("p (h e) -> p h e", h=4), ALU.add)
            k.act(hst[:, 4:8], hst[:, 0:4], AF.Sqrt, bias=self.eps_t, scale=1.0 / 128)
            k.recip(hst[:, 8:12], hst[:, 4:8])
            k.dma(mot, self.mo_d[rows, :])
            k.tt(tmpm, hs, mot, ALU.mult)
            for h in range(4):
                k.ts(mtok[:, h * 128:(h + 1) * 128], tmpm[:, h * 128:(h + 1) * 128], hst[:, 8 + h:9 + h], ALU.mult)
            for h in range(4):
                k.tr(pfin[:, h * 128:(h + 1) * 128], mtok[:, h * 128:(h + 1) * 128], self.identb)
            for h in range(4):
                k.act(mixT[:, h, :], pfin[:, h * 128:(h + 1) * 128], AF.Copy, scale=mnw[:, h:h + 1])
            k.dma(self.mixT_d[0:4, :, rows].rearrange("c p n -> p c n"), mixT)

        stage_A(0)
        pend_fin = []
        for n, (s_, d, h) in enumerate(items):
            if n + 1 < len(items):
                stage_A(n + 1)
            for c in pend_fin:
                finalize(c)
            pend_fin = []
            stage_B(n)
            if h == 3:
                done = stage_E(s_, d)
                if done is not None:
                    pend_fin.append(done)
        for c in pend_fin:
            finalize(c)
        if self.stopped(layer, "D1"):
            k.dma(self.hs_dbg, hsum)
        k.barrier()


Prog.phase_mlstm = _phase_mlstm


def _phase_na(self, layer):
    k = self.k
    last = layer == 1
    with ExitStack() as es:
        sb = lambda shape, dt=F32, name=None: k.sb(es, shape, dt, name)
        biasA = sb([128, 8, 640], BF16, "biasA")
        biasS = sb([128, 8, 640], BF16, "biasS")
        k.dma(biasA, self.nabias[layer, 2].rearrange("h q n -> q h n"), q="pool")
        nqv = self.nqT_d.rearrange("t (two p) n -> p (t two) n", p=64)
        nkv = self.nkT_d.rearrange("t (two p) n -> p (t two) n", p=64)
        kctx = sb([64, 8, 256], BF16, "kctx")
        k.dma(kctx, nkv[:, :, L:NT])
        vctx = sb([128, 2, 8, 65], BF16, "vctx")
        k.memset(vctx[:, :, :, 64:65], 1.0)
        for cc in range(2):
            k.dma(vctx[:, cc, :, 0:64],
                  self.nv_d[L + cc * 128:L + (cc + 1) * 128, :].rearrange("p (h e) -> p h e", h=8))
        qs = [sb([64, 8, 128], BF16, "naq") for _ in range(2)]
        kws = [sb([64, 8, 640], BF16, "nak") for _ in range(2)]
        vws = [sb([128, 5, 8, 65], BF16, "nav") for _ in range(2)]
        for v in vws:
            k.memset(v[:, :, :, 64:65], 1.0)
        PTs = [sb([128, 7, 128], BF16, "PT") for _ in range(2)]
        ots = [sb([128, 512], BF16, "otok") for _ in range(2)]
        rcs = [sb([128, 8], F32, "rc") for _ in range(2)]
        mxs = [sb([128, 512], BF16, "mx") for _ in range(2)]
        pSs = [k.ps(es, [128, 8, 128], F32, "naS") for _ in range(2)]
        pOs = [k.ps(es, [128, 512], F32, "naO") for _ in range(2)]
        ptr = k.ps(es, [128, 1024], BF16, "natr")
        types = {0: 0, 1: 1, 30: 3, 31: 4}
        blocks = [(j, True) for j in range(32)] + ([] if last else [(0, False), (1, False)])
        binfo = {}

        def prologue(bi):
            j, lat = blocks[bi]
            q, kw, vw = qs[bi % 2], kws[bi % 2], vws[bi % 2]
            bias = None
            if lat:
                tok0 = j * 128
                cs = min(max(j - 2, 0), 27)
                nwin = 5
                if j in types:
                    k.dma(biasS, self.nabias[layer, types[j]].rearrange("h q n -> q h n"), q="pool")
                    bias = biasS
                else:
                    bias = biasA
                k.dma(kw, nkv[:, :, cs * 128:cs * 128 + 640])
                for c in range(5):
                    k.dma(vw[:, c, :, 0:64],
                          self.nv_d[(cs + c) * 128:(cs + c + 1) * 128, :].rearrange("p (h e) -> p h e", h=8))
            else:
                tok0 = L + j * 128
                nwin = 0
            k.dma(q, nqv[:, :, tok0:tok0 + 128])
            binfo[bi] = (tok0, nwin, bias)

        def stage_S(bi, hh, n):
            if hh == 0:
                prologue(bi)
            tok0, nwin, bias = binfo[bi]
            q, kw = qs[bi % 2], kws[bi % 2]
            pS = pSs[n % 2]
            for c in range(nwin):
                k.mm(pS[:, c, :], kw[:, hh, c * 128:(c + 1) * 128], q[:, hh, :], start=True, stop=False)
                k.mm(pS[:, c, :], bias[:, hh, c * 128:(c + 1) * 128], self.identb, start=False, stop=True)
            for cc in range(2):
                k.mm(pS[:, nwin + cc, :], kctx[:, hh, cc * 128:(cc + 1) * 128], q[:, hh, :])

        def stage_rest(bi, hh, n):
            tok0, nwin, bias = binfo[bi]
            vw, ot, rc = vws[bi % 2], ots[bi % 2], rcs[bi % 2]
            pS, PT, pO = pSs[n % 2], PTs[n % 2], pOs[n % 2][:, 0:65]
            nch = nwin + 2
            if nch > 4:
                k.act(PT[:, 0:4, :], pS[:, 0:4, :], AF.Exp)
                k.act(PT[:, 4:nch, :], pS[:, 4:nch, :], AF.Exp)
            else:
                k.act(PT[:, 0:nch, :], pS[:, 0:nch, :], AF.Exp)
            for c in range(nch):
                rhs = vw[:, c, hh, :] if c < nwin else vctx[:, c - nwin, hh, :]
                k.mm(pO, PT[:, c, :], rhs, start=(c == 0), stop=(c == nch - 1))
            k.recip(rc[:, hh:hh + 1], pO[:, 64:65])
            k.ts(ot[:, hh * 64:(hh + 1) * 64], pO[:, 0:64], rc[:, hh:hh + 1], ALU.mult)

        def epilogue(bi):
            tok0 = binfo[bi][0]
            ot, mx = ots[bi % 2], mxs[bi % 2]
            po = (bi % 2) * 512
            for t4 in range(4):
                k.tr((ptr[:, po + t4 * 128:po + (t4 + 1) * 128], bi % 2), ot[:, t4 * 128:(t4 + 1) * 128], self.identb)
            k.copy(mx, (ptr[:, po:po + 512], bi % 2), eng="act")
            k.dma(self.mixT_d[4:8, :, tok0:tok0 + 128].rearrange("c p n -> p c n"),
                  mx.rearrange("p (c n) -> p c n", c=4))

        items = [(bi, hh) for bi in range(len(blocks)) for hh in range(8)]
        stage_S(items[0][0], items[0][1], 0)
        pending_epi = None
        for n, (bi, hh) in enumerate(items):
            if n + 1 < len(items):
                stage_S(items[n + 1][0], items[n + 1][1], n + 1)
            if pending_epi is not None:
                epilogue(pending_epi)
                pending_epi = None
            stage_rest(bi, hh, n)
            if hh == 7:
                pending_epi = bi
        epilogue(pending_epi)
        k.barrier()


Prog.phase_na = _phase_na


def _router(self, LG, n, es):
    k = self.k
    t16 = lambda nm: k.sb(es, [128, n, 16], F32, nm)
    t1 = lambda nm: k.sb(es, [128, n], F32, nm)
    B16 = lambda a: a.unsqueeze(2).to_broadcast([128, n, 16])
    mx, ssum, rs, gm, m1, m2, wsum, rws = [t1(f"r1_{j}") for j in range(8)]
    e, sc, sel, m16, tt16, msel, is1, msel2, is2, wts = [t16(f"r16_{j}") for j in range(10)]
    ps6 = k.sb(es, [128, n, 4, 6], F32, "ps6")
    gs = k.sb(es, [128, n, 4], F32, "gs")
    ing = k.sb(es, [128, n, 4], F32, "ing")
    k.reduce(mx, LG, ALU.max)
    k.tt(e, LG, B16(mx), ALU.subtract)
    k.act(e, e, AF.Exp)
    k.reduce(ssum, e, ALU.add)
    k.recip(rs, ssum)
    k.tt(sc, e, B16(rs), ALU.mult)
    k.tt(sel, sc, self.rb_bc.unsqueeze(1).to_broadcast([128, n, 16]), ALU.add)
    selv = sel.rearrange("p n (g e) -> p n g e", g=4)
    for pi, (a, b) in enumerate(((0, 1), (0, 2), (0, 3), (1, 2), (1, 3), (2, 3))):
        k.tt(ps6[:, :, :, pi], selv[:, :, :, a], selv[:, :, :, b], ALU.add)
    k.reduce(gs, ps6, ALU.max)
    k.reduce(gm, gs, ALU.max)
    k.tt(ing, gs, gm.unsqueeze(2).to_broadcast([128, n, 4]), ALU.is_equal)
    k.copy(m16.rearrange("p n (g e) -> p n g e", g=4), ing.unsqueeze(3).to_broadcast([128, n, 4, 4]))
    k.ts(tt16, m16, 10.0, ALU.mult, -10.0, ALU.add)
    k.tt(msel, sel, m16, ALU.mult)
    k.tt(msel, msel, tt16, ALU.add)
    k.reduce(m1, msel, ALU.max)
    k.tt(is1, msel, B16(m1), ALU.is_equal)
    k.stt(msel2, is1, -20.0, msel, ALU.mult, ALU.add)
    k.reduce(m2, msel2, ALU.max)
    k.tt(is2, msel2, B16(m2), ALU.is_equal)
    k.tt(is1, is1, is2, ALU.add)
    k.tt(wts, sc, is1, ALU.mult)
    k.reduce(wsum, wts, ALU.add)
    k.recip(rws, wsum)
    k.tt(self.gate_sb[:, 0:n, :], wts, B16(rws), ALU.mult)


def _phase_wout_norm2(self, layer):
    k = self.k
    last = layer == 1
    ntl = 32 if last else NTL
    with ExitStack() as es:
        sb = lambda shape, dt=F32, name=None: k.sb(es, shape, dt, name)
        wo = sb([128, 8, D], BF16, "wo")
        wov = self.w_out[layer].rearrange("(j p) n -> p j n", p=128)
        for half in range(2):
            k.dma(wo[:, :, half * 512:(half + 1) * 512], wov[:, :, half * 512:(half + 1) * 512], q="pool")
        mixs = [sb([128, 8, 128], BF16, "mix") for _ in range(2)]
        xts = [sb([128, D], F32, "xt2") for _ in range(4)]
        tmps = [sb([128, D], F32, "tmp2") for _ in range(2)]
        nb = [(sb([128, D], F32, "junk"), sb([128, D], F32, "xn"), sb([128, 4], F32, "st")) for _ in range(3)]
        pts = [k.ps(es, [128, 8, 128], F32, "ptr") for _ in range(2)]
        r32s = [sb([128, 8, 128], F32, "r32") for _ in range(3)]
        LG = sb([128, ntl, 16], F32, "LG")
        py = k.ps(es, [128, 2, 512], F32, "py")
        plogs = [k.ps(es, [128, 16], F32, "plog") for _ in range(2)]

        def stage1(i):
            w = 0 if i < 32 else 1
            rows = slice(i * 128, (i + 1) * 128)
            mix, xt, tmp = mixs[i % 2], xts[i % 4], tmps[i % 2]
            k.dma(mix, self.mixT_d[:, :, rows].rearrange("c p n -> p c n"))
            k.dma(xt, self.xsrc(layer, i))
            for half in range(2):
                for mch in range(8):
                    k.mm(py[:, half, :], mix[:, mch, :], wo[:, mch, half * 512:(half + 1) * 512],
                         start=(mch == 0), stop=(mch == 7))
            for half in range(2):
                k.tt(tmp[:, half * 512:(half + 1) * 512], py[:, half, :], self.bc[(2, w)][:, half * 512:(half + 1) * 512], ALU.mult)
            k.tt(xt, xt, tmp, ALU.add)
            k.dma((self.xres[rows, :], i), xt)

        def stage2a(i):
            self.norm_stats(xts[i % 4], nb[i % 3])

        def stage2b(i):
            self.norm_tr(i, nb[i % 3][1], pts[i % 2], self.ws2, self.sh2, 0 if i < 32 else 1, r32=r32s[i % 3])

        def stage2c(i):
            r32, plog = r32s[i % 3], plogs[i % 2]
            for dch in range(8):
                k.mm(plog, r32[:, dch, :], self.rw_sb[:, dch, :], start=(dch == 0), stop=(dch == 7))
            k.copy((LG[:, i, :], i), plog)

        for t in range(ntl + 3):
            if t < ntl:
                stage1(t)
            if 0 <= t - 1 < ntl:
                stage2a(t - 1)
            if 0 <= t - 2 < ntl:
                stage2b(t - 2)
            if 0 <= t - 3 < ntl:
                stage2c(t - 3)
        self.router(LG, ntl, es)
        k.barrier()


Prog.router = _router
Prog.phase_wout_norm2 = _phase_wout_norm2


def _phase_moe(self, layer):
    k = self.k
    last = layer == 1
    ntl = 32 if last else NTL
    nblk = 4
    bounds = [round(b * ntl / nblk) for b in range(nblk + 1)]
    with ExitStack() as es:
        sb = lambda shape, dt=F32, name=None: k.sb(es, shape, dt, name)
        nbmax = max(bounds[b + 1] - bounds[b] for b in range(nblk))
        yacc = sb([128, nbmax, 2, 512], F32, "yacc")
        w1s = [sb([128, 8, DFF], BF16, "w1") for _ in range(2)]
        w3s = [sb([128, 8, DFF], BF16, "w3") for _ in range(2)]
        w2s = [sb([128, 4, D], BF16, "w2") for _ in range(2)]
        aTs = [sb([128, 4, 512], BF16, "aT") for _ in range(2)]
        sTs = [sb([128, 512], F32, "sT") for _ in range(2)]
        xts = [sb([128, D], F32, "xt3") for _ in range(2)]
        tmps = [sb([128, D], F32, "tmp3") for _ in range(2)]
        st = sb([128, 4], F32, "st3")
        if last:
            fnw_bc = sb([128, D], F32, "fnw")
            k.dma(fnw_bc, self.fnw.partition_broadcast(128))
        p1s = [k.ps(es, [128, 512], F32, "p1") for _ in range(2)]
        p3s = [k.ps(es, [128, 512], F32, "p3") for _ in range(2)]
        pys = [k.ps(es, [128, 2, 512], F32, "pym") for _ in range(2)]
        w1v = self.w1[layer].rearrange("e (j p) n -> e p j n", p=128)
        w3v = self.w3[layer].rearrange("e (j p) n -> e p j n", p=128)
        w2v = self.w2[layer].rearrange("e (j p) n -> e p j n", p=128)
        nw = 0
        nh = 0
        ny = 0
        for b in range(nblk):
            tiles = list(range(bounds[b], bounds[b + 1]))
            groups = []
            for i in tiles:
                if groups and len(groups[-1]) < 4 and self.hcol(groups[-1][-1]) + 128 == self.hcol(i):
                    groups[-1].append(i)
                else:
                    groups.append([i])
            for e in range(NE):
                w1, w3, w2 = w1s[nw % 2], w3s[nw % 2], w2s[nw % 2]
                nw += 1
                k.dma(w1, w1v[e], q="pool")
                k.dma(w3, w3v[e], q="pool")
                k.dma(w2, w2v[e], q="pool")
                for grp in groups:
                    nt = 128 * len(grp)
                    c0 = self.hcol(grp[0])
                    aT = aTs[nh % 2]
                    for fch in range(4):
                        p1, p3, sT = p1s[nh % 2], p3s[nh % 2], sTs[nh % 2]
                        nh += 1
                        for dch in range(8):
                            k.mm(p1[:, :nt], w1[:, dch, fch * 128:(fch + 1) * 128], self.hT[:, dch, c0:c0 + nt],
                                 start=(dch == 0), stop=(dch == 7))
                        for dch in range(8):
                            k.mm(p3[:, :nt], w3[:, dch, fch * 128:(fch + 1) * 128], self.hT[:, dch, c0:c0 + nt],
                                 start=(dch == 0), stop=(dch == 7))
                        k.act(sT[:, :nt], p1[:, :nt], AF.Silu)
                        k.tt(aT[:, fch, :nt], p3[:, :nt], sT[:, :nt], ALU.mult)
                    for tl, i in enumerate(grp):
                        bt = i - bounds[b]
                        py = pys[ny % 2]
                        ny += 1
                        for half in range(2):
                            for fch in range(4):
                                k.mm(py[:, half, :], aT[:, fch, tl * 128:(tl + 1) * 128],
                                     w2[:, fch, half * 512:(half + 1) * 512], start=(fch == 0), stop=(fch == 3))
                        gcol = (self.gate_sb[:, i, e:e + 1], i)
                        for half in range(2):
                            ya = (yacc[:, bt, half, :], (bt, half))
                            if e == 0:
                                k.ts(ya, py[:, half, :], gcol, ALU.mult)
                            else:
                                k.stt(ya, py[:, half, :], gcol, ya, ALU.mult, ALU.add)
            for i in tiles:
                bt = i - bounds[b]
                w = 0 if i < 32 else 1
                rows = slice(i * 128, (i + 1) * 128)
                xt, tmp = xts[i % 2], tmps[i % 2]
                k.dma(xt, (self.xres[rows, :], i))
                for half in range(2):
                    k.tt(tmp[:, half * 512:(half + 1) * 512], (yacc[:, bt, half, :], (bt, half)),
                         self.bc[(5, w)][:, half * 512:(half + 1) * 512], ALU.mult)
                k.tt(xt, xt, tmp, ALU.add)
                if not last:
                    k.dma((self.xres[rows, :], i), xt)
                else:
                    k.act(tmp, xt, AF.Square)
                    k.reduce(st[:, 0:1], tmp, ALU.add)
                    k.act(st[:, 1:2], st[:, 0:1], AF.Sqrt, bias=self.eps_t, scale=1.0 / D)
                    k.recip(st[:, 2:3], st[:, 1:2])
                    k.stt(tmp, xt, st[:, 2:3], fnw_bc, ALU.mult, ALU.mult)
                    k.dma(self.out[rows, :], tmp, is_output=True)
        k.barrier()


Prog.phase_moe = _phase_moe


def _phase_mix(self, layer):
    k = self.k
    last = layer == 1
    with ExitStack() as es:
        sb = lambda shape, dt=F32, name=None: k.sb(es, shape, dt, name)
        EB = sb([128, NTL, 8], F32, "EB")
        WW = sb([128, NTL, 8], F32, "WW")
        UU = sb([128, NTL, 8], F32, "UU")
        ET = sb([128, NTL, 8], F32, "ET")
        hsum = sb([128, NTL, 512], F32, "hsum")
        one_c = self.ones[:, 0:1]
        with ExitStack() as es1:
            sb1 = lambda shape, dt=F32, name=None: k.sb(es1, shape, dt, name)
            gall = sb1([128, NTL, 2, 2, 4], F32, "gall")
            k.dma(gall, self.g_d.rearrange("(c p) (d t h) -> p c d t h", p=128, d=2, t=2))
            gv = gall.rearrange("p c d t h -> p d c t h")
            e1 = sb1([128, 2, NTL, 4], F32, "e1")
            l = sb1([128, 2, NTL, 4], F32, "lsp")
            cc = sb1([128, 2, NTL, 4], F32, "cc")
            ct = sb1([128, 2, NTL, 4], F32, "ct")
            t1 = sb1([128, 2, NTL, 4], F32, "gt1")
            t2 = sb1([128, 2, NTL, 4], F32, "gt2")
            pgf = k.ps(es1, [128, NTL, 4], F32, "pgf")
            pgb = k.ps(es1, [128, NTL, 4], F32, "pgb")
            ptot = k.ps(es1, [128, 2, NTL, 4], F32, "ptot")
            k.act(e1, gv[:, :, :, 1, :], AF.Exp, scale=-1.0)
            k.act(l, e1, AF.Ln, bias=one_c)
            k.mm(pgf, self.maskF, l[:, 0])
            k.mm(pgb, self.maskB, l[:, 1])
            k.mm(ptot, self.ones, l)
            k.copy(cc[:, 0], pgf, eng="act")
            k.copy(cc[:, 1], pgb, eng="act")
            k.copy(ct, ptot, eng="act")
            v = lambda X: X.rearrange("p c (d h) -> p d c h", d=2)
            k.act(v(EB), cc, AF.Exp, scale=-1.0)
            k.tt(t1, cc, gv[:, :, :, 0, :], ALU.add)
            k.act(v(WW), t1, AF.Exp)
            k.tt(t2, t1, ct, ALU.subtract)
            k.act(v(UU), t2, AF.Exp)
            k.act(v(ET), ct, AF.Exp, scale=-1.0)
            k.barrier()
        Cst = [sb([128, 129], F32, "Cst") for _ in range(8)]
        Cbf = [sb([128, 129], BF16, "Cbf") for _ in range(8)]
        for hd in range(8):
            k.memset(Cst[hd], 0.0)
            k.memset(Cbf[hd], 0.0, eng="pool")
        qTs = [[sb([128, 4, 128], BF16, "qT") for _ in range(2)] for _ in range(2)]
        kTs = [[sb([128, 4, 128], BF16, "kT") for _ in range(2)] for _ in range(2)]
        vaug = [[sb([128, 4, 129], BF16, "vaug") for _ in range(2)] for _ in range(2)]
        for vv in vaug:
            for v_ in vv:
                k.memset(v_[:, :, 128:129], 1.0)
        ktoks = [sb([128, 128], BF16, "ktok") for _ in range(3)]
        STs = [sb([128, 128], BF16, "ST") for _ in range(3)]
        uvs = [sb([128, 129], BF16, "uv") for _ in range(3)]
        sms = [sb([128, 6, 2, 2], F32, "sm") for _ in range(2)]
        sq = sb([128, 512], F32, "sq")
        tmpm = sb([128, 512], F32, "tmpm")
        mot = sb([128, 512], F32, "mot")
        mtok = sb([128, 512], BF16, "mtok")
        mixTm = sb([128, 4, 128], BF16, "mixTm")
        hst = sb([128, 12], F32, "hst")
        mnw = sb([128, 4], F32, "mnw")
        k.dma(mnw, self.mnw[layer])
        biasA = sb([128, 8, 640], BF16, "biasA")
        biasS = sb([128, 8, 640], BF16, "biasS")
        k.dma(biasA, self.nabias[layer, 2].rearrange("h q n -> q h n"), q="pool")
        nqv = self.nqT_d.rearrange("t (two p) n -> p (t two) n", p=64)
        nkv = self.nkT_d.rearrange("t (two p) n -> p (t two) n", p=64)
        kctx = sb([64, 8, 256], BF16, "kctx")
        k.dma(kctx, nkv[:, :, L:NT])
        vctx = sb([128, 2, 8, 65], BF16, "vctx")
        k.memset(vctx[:, :, :, 64:65], 1.0)
        for cc_ in range(2):
            k.dma(vctx[:, cc_, :, 0:64],
                  self.nv_d[L + cc_ * 128:L + (cc_ + 1) * 128, :].rearrange("p (h e) -> p h e", h=8))
        qs = [sb([64, 8, 128], BF16, "naq") for _ in range(2)]
        kws = [sb([64, 8, 640], BF16, "nak") for _ in range(2)]
        vws = [sb([128, 5, 8, 65], BF16, "nav") for _ in range(2)]
        for v_ in vws:
            k.memset(v_[:, :, :, 64:65], 1.0)
        PTs = [sb([128, 7, 128], BF16, "PT") for _ in range(2)]
        ots = [sb([128, 512], BF16, "otok") for _ in range(2)]
        rcs = [sb([128, 8], F32, "rc") for _ in range(2)]
        mxs = [sb([128, 512], BF16, "mx") for _ in range(2)]
        naS = k.ps(es, [128, 4, 128], F32, "naS")
        naO = k.ps(es, [128, 512], F32, "naO")
        trb = k.ps(es, [128, 1024], BF16, "trb")
        pk = k.ps(es, [128, 1024], BF16, "pk")
        pS = k.ps(es, [128, 512], F32, "pS")
        pNt = k.ps(es, [128, 2, 512], F32, "pN")
        pCb = k.ps(es, [128, 512], F32, "pCb")

        order = {0: [32, 33] + list(range(32)), 1: [33, 32] + list(range(31, -1, -1))}
        masks = (self.maskF, self.maskB)
        mitems = [(s_, d, h) for s_ in range(NTL) for d in (0, 1) for h in range(4)]
        seen = set()

        def mbufs(s_, d):
            return qTs[d][s_ % 2], kTs[d][s_ % 2], vaug[d][s_ % 2]

        def ml_A(n):
            s_, d, h = mitems[n]
            c = order[d][s_]
            qT, kT, va = mbufs(s_, d)
            if h == 0:
                rows = slice(c * 128, (c + 1) * 128)
                k.dma(qT, self.qT_d[:, :, rows].rearrange("h p n -> p h n"))
                k.dma(kT, self.kT_d[:, :, rows].rearrange("h p n -> p h n"))
                k.dma(va[:, :, 0:128], self.mv_d[rows, :].rearrange("p (h e) -> p h e", h=4))
            k.tr(pk[:, 0:128], kT[:, h, :], self.identb)
            k.mm(pS[:, 0:128], kT[:, h, :], qT[:, h, :])

        def ml_B(n):
            s_, d, h = mitems[n]
            c = order[d][s_]
            hd = d * 4 + h
            qT, kT, va = mbufs(s_, d)
            ktok, ST, uv = ktoks[n % 3], STs[n % 3], uvs[n % 3]
            k.copy(ktok, pk[:, 0:128], eng="act")
            k.stt(ST, pS[:, 0:128], WW[:, c, hd:hd + 1], masks[d], ALU.mult, ALU.mult)
            k.act(uv, va[:, h, :], AF.Copy, scale=UU[:, c, hd:hd + 1])

        def ml_C(n):
            s_, d, h = mitems[n]
            c = order[d][s_]
            hd = d * 4 + h
            qT, kT, va = mbufs(s_, d)
            ktok, ST, uv = ktoks[n % 3], STs[n % 3], uvs[n % 3]
            pN = pNt[:, h // 2, (h % 2) * 129:(h % 2) * 129 + 129]
            pC = pCb[:, 0:129]
            k.mm(pN, ST, va[:, h, :], start=True, stop=False)
            k.mm(pN, qT[:, h, :], Cbf[hd], start=False, stop=True)
            k.mm(pC, ktok, uv)
            k.stt(Cst[hd], Cst[hd], ET[:, c, hd:hd + 1], pC, ALU.mult, ALU.add)
            k.copy(Cbf[hd], Cst[hd], eng="pool")

        def ml_E(s_, d):
            c = order[d][s_]
            sm = sms[d]
            eb4 = EB[:, c, d * 4:(d + 1) * 4].rearrange("p (b h) -> p b h", b=2)
            for b in range(2):
                qn2 = pNt[:, b, 0:258].rearrange("p (h e) -> p h e", e=129)[:, :, 128]
                k.tt(sm[:, 0, b, :], qn2, eb4[:, b, :], ALU.mult)
            k.ts(sm[:, 1], sm[:, 0], -1.0, ALU.mult)
            k.tt(sm[:, 2], sm[:, 0], sm[:, 1], ALU.max)
            k.ts(sm[:, 3], sm[:, 2], 1.0, ALU.max)
            k.recip(sm[:, 4], sm[:, 3])
            k.tt(sm[:, 5], sm[:, 4], eb4, ALU.mult)
            for h in range(4):
                sc = sm[:, 5, h // 2, h % 2:h % 2 + 1]
                num = pNt[:, h // 2, (h % 2) * 129:(h % 2) * 129 + 128]
                dst = (hsum[:, c, h * 128:(h + 1) * 128], c)
                if c not in seen:
                    k.ts(dst, num, sc, ALU.mult)
                else:
                    k.stt(dst, num, sc, dst, ALU.mult, ALU.add)
            if c in seen:
                return c
            seen.add(c)
            return None

        def ml_fin(c):
            rows = slice(c * 128, (c + 1) * 128)
            hs = (hsum[:, c, :], c)
            k.act(sq, hs, AF.Square)
            k.reduce(hst[:, 0:4], sq.rearrange("p (h e) -> p h e", h=4), ALU.add)
            k.act(hst[:, 4:8], hst[:, 0:4], AF.Sqrt, bias=self.eps_t, scale=1.0 / 128)
            k.recip(hst[:, 8:12], hst[:, 4:8])
            k.dma(mot, self.mo_d[rows, :])
            k.tt(tmpm, hs, mot, ALU.mult)
            for h in range(4):
                k.ts(mtok[:, h * 128:(h + 1) * 128], tmpm[:, h * 128:(h + 1) * 128], hst[:, 8 + h:9 + h], ALU.mult)
            for h in range(4):
                k.tr(trb[:, h * 128:(h + 1) * 128], mtok[:, h * 128:(h + 1) * 128], self.identb)
            for h in range(4):
                k.act(mixTm[:, h, :], trb[:, h * 128:(h + 1) * 128], AF.Copy, scale=mnw[:, h:h + 1])
            k.dma(self.mixT_d[0:4, :, rows].rearrange("c p n -> p c n"), mixTm)

        types = {0: 0, 1: 1, 30: 3, 31: 4}
        blocks = [(j, True) for j in range(32)] + ([] if last else [(0, False), (1, False)])
        nitems = [(bi, hh) for bi in range(len(blocks)) for hh in range(8)]
        binfo = {}

        def na_prologue(bi):
            j, lat = blocks[bi]
            q, kw, vw = qs[bi % 2], kws[bi % 2], vws[bi % 2]
            bias = None
            if lat:
                tok0 = j * 128
                cs = min(max(j - 2, 0), 27)
                nwin = 5
                if j in types:
                    k.dma(biasS, self.nabias[layer, types[j]].rearrange("h q n -> q h n"), q="pool")
                    bias = biasS
                else:
                    bias = biasA
                k.dma(kw, nkv[:, :, cs * 128:cs * 128 + 640])
                for c in range(5):
                    k.dma(vw[:, c, :, 0:64],
                          self.nv_d[(cs + c) * 128:(cs + c + 1) * 128, :].rearrange("p (h e) -> p h e", h=8))
            else:
                tok0 = L + j * 128
                nwin = 0
            k.dma(q, nqv[:, :, tok0:tok0 + 128])
            binfo[bi] = (tok0, nwin, bias)

        def na_S(m, lo, hi):
            bi, hh = nitems[m]
            tok0, nwin, bias = binfo[bi]
            q, kw = qs[bi % 2], kws[bi % 2]
            for c in range(lo, hi):
                o = naS[:, c - lo, :]
                if c < nwin:
                    k.mm(o, kw[:, hh, c * 128:(c + 1) * 128], q[:, hh, :], start=True, stop=False)
                    k.mm(o, bias[:, hh, c * 128:(c + 1) * 128], self.identb, start=False, stop=True)
                else:
                    cc_ = c - nwin
                    k.mm(o, kctx[:, hh, cc_ * 128:(cc_ + 1) * 128], q[:, hh, :])

        def na_exp(m, lo, hi):
            PT = PTs[m % 2]
            k.act(PT[:, lo:hi, :], naS[:, 0:hi - lo, :], AF.Exp)

        def na_PV(m):
            bi, hh = nitems[m]
            tok0, nwin, bias = binfo[bi]
            vw, ot, rc = vws[bi % 2], ots[bi % 2], rcs[bi % 2]
            PT, pO = PTs[m % 2], naO[:, 0:65]
            nch = nwin + 2
            for c in range(nch):
                rhs = vw[:, c, hh, :] if c < nwin else vctx[:, c - nwin, hh, :]
                k.mm(pO, PT[:, c, :], rhs, start=(c == 0), stop=(c == nch - 1))
            k.recip(rc[:, hh:hh + 1], pO[:, 64:65])
            k.ts(ot[:, hh * 64:(hh + 1) * 64], pO[:, 0:64], rc[:, hh:hh + 1], ALU.mult)

        def na_epi(bi):
            tok0 = binfo[bi][0]
            ot, mx = ots[bi % 2], mxs[bi % 2]
            for t4 in range(4):
                k.tr(trb[:, 512 + t4 * 128:512 + (t4 + 1) * 128], ot[:, t4 * 128:(t4 + 1) * 128], self.identb)
            k.copy(mx, trb[:, 512:1024], eng="act")
            k.dma(self.mixT_d[4:8, :, tok0:tok0 + 128].rearrange("c p n -> p c n"),
                  mx.rearrange("p (c n) -> p c n", c=4))

        npair = max(len(mitems), len(nitems))
        pend_fin = []
        pend_epi = None
        for n in range(npair):
            has_m = n < len(mitems)
            has_n = n < len(nitems)
            if has_n:
                bi, hh = nitems[n]
                if hh == 0:
                    na_prologue(bi)
                nch = binfo[bi][1] + 2
                n1 = min(4, nch)
                na_S(n, 0, n1)
            if has_m:
                ml_A(n)
            if has_n:
                na_exp(n, 0, n1)
            if has_m:
                ml_B(n)
            if has_n and nch > 4:
                na_S(n, 4, nch)
            if has_m:
                ml_C(n)
            if has_n:
                if nch > 4:
                    na_exp(n, 4, nch)
                if pend_epi is not None:
                    na_epi(pend_epi)
                    pend_epi = None
                na_PV(n)
                if hh == 7:
                    pend_epi = bi
            for c in pend_fin:
                ml_fin(c)
            pend_fin = []
            if has_m and mitems[n][2] == 3:
                done = ml_E(mitems[n][0], mitems[n][1])
                if done is not None:
                    pend_fin.append(done)
        if pend_epi is not None:
            na_epi(pend_epi)
        for c in pend_fin:
            ml_fin(c)
        if self.stopped(layer, "D2"):
            k.dma(self.hs_dbg, hsum)
        k.barrier()


Prog.phase_mix = _phase_mix
```

```python
import numpy as np
from contextlib import ExitStack
import concourse.bass as bass
import concourse.mybir as mybir
from concourse.bass_utils import run_bass_kernel_spmd

F32 = mybir.dt.float32
BF16 = mybir.dt.bfloat16
I32 = mybir.dt.int32
U32 = mybir.dt.uint32
AF = mybir.ActivationFunctionType
ALU = mybir.AluOpType
AX = mybir.AxisListType

SEM_LIMIT = 30000
SAME_ENGINE_SYNC = True
MERGED_MIXER = False
NDMA = 24


class _U:
    __slots__ = ("w", "r")

    def __init__(self):
        self.w = {}
        self.r = {}


class KB:
    def __init__(self, nc, same_engine_sync=True):
        self.nc = nc
        self.E = {"pe": nc.tensor, "act": nc.scalar, "dve": nc.vector, "pool": nc.gpsimd, "sp": nc.sync}
        self.sems = []
        self.cur = {}
        for e in ("pe", "act", "dve", "pool"):
            self.cur[e] = [self._newsem(), 0]
        self.dsem = {q: [self._newsem() for _ in range(NDMA)] for q in ("sp", "pool")}
        self.dval = {q: [0] * NDMA for q in ("sp", "pool")}
        self.di = {"sp": 0, "pool": 0}
        self.waited = {e: {} for e in self.E}
        self.track = {}
        self.ses = same_engine_sync
        self.old = []
        self.n_ins = 0
        self.n_wait = 0
        self.out_events = []
        self._uid = 0

    def _newsem(self):
        h = self.nc.alloc_semaphore(f"ks{len(self.sems)}")
        self.sems.append(h)
        return len(self.sems) - 1

    def sb(self, es, shape, dt=F32, name=None):
        self._uid += 1
        return es.enter_context(self.nc.sbuf_tensor(f"{name or 'sb'}_{self._uid}", list(shape), dt)).ap()

    def ps(self, es, shape, dt=F32, name=None):
        self._uid += 1
        esz = 4 if dt in (F32, I32, U32) else 2
        free = 1
        for d_ in shape[1:]:
            free *= d_
        per_bank = 2048 // esz
        nb = (free + per_bank - 1) // per_bank
        flat = es.enter_context(self.nc.psum_tensor(f"{name or 'ps'}_{self._uid}", [128, nb * per_bank], dt)).ap()
        v = flat[0:shape[0], 0:free]
        nd = len(shape) - 1
        if nd == 1:
            return v
        names = "abcd"[:nd]
        pat = f"p ({' '.join(names)}) -> p {' '.join(names)}"
        return v.rearrange(pat, **{names[i]: shape[1 + i] for i in range(1, nd)})

    def dram(self, name, shape, dt=F32, kind="Internal"):
        return self.nc.dram_tensor(name, list(shape), dt, kind=kind).ap()

    @staticmethod
    def _split(x):
        if isinstance(x, tuple):
            return x[0], x[1]
        return x, None

    def _units(self, x):
        ap, key = self._split(x)
        name = ap.tensor.name
        d = self.track.get(name)
        if d is None:
            d = {None: _U()}
            self.track[name] = d
        if key is None:
            return list(d.values())
        u = d.get(key)
        if u is None:
            u = _U()
            d[key] = u
        return [u, d[None]]

    def _wait(self, eng, sid, val, owner):
        if owner == eng and (eng == "pe" or not self.ses):
            return
        if self.waited[eng].get(sid, -1) >= val:
            return
        self.E[eng].wait_ge(self.sems[sid], val)
        self.waited[eng][sid] = val
        self.n_wait += 1

    def _pre(self, eng, outs, ins):
        for x in ins:
            for u in self._units(x):
                for sid, (val, owner) in u.w.items():
                    self._wait(eng, sid, val, owner)
        for x in outs:
            for u in self._units(x):
                for sid, (val, owner) in u.w.items():
                    self._wait(eng, sid, val, owner)
                for sid, (val, owner) in u.r.items():
                    self._wait(eng, sid, val, owner)

    def _post(self, sid, val, owner, outs, ins):
        for x in ins:
            ap, key = self._split(x)
            us = self._units(x)
            if key is not None:
                us = us[:1]
            for u in us:
                u.r[sid] = (val, owner)
        for x in outs:
            ap, key = self._split(x)
            us = self._units(x)
            if key is not None:
                us = us[:1]
            for u in us:
                u.w = {sid: (val, owner)}
                u.r = {}

    def op(self, eng, fn, outs, ins):
        self._pre(eng, outs, ins)
        c = self.cur[eng]
        if c[1] >= SEM_LIMIT:
            self.old.append((c[0], c[1]))
            c[0] = self._newsem()
            c[1] = 0
        ins_obj = fn()
        c[1] += 1
        ins_obj.then_inc(self.sems[c[0]], 1)
        self._post(c[0], c[1], eng, outs, ins)
        self.n_ins += 1

    def dma(self, out, in_, q="sp", is_output=False, **kw):
        eng = q
        self._pre(eng, [out], [in_])
        i = self.di[q]
        self.di[q] = (i + 1) % NDMA
        sid = self.dsem[q][i]
        dval = self.dval[q]
        if dval[i] > 0:
            self._wait(eng, sid, dval[i], "dma")
        dval[i] += 16
        o, _ = self._split(out)
        s, _ = self._split(in_)
        self.E[eng].dma_start(out=o, in_=s, **kw).then_inc(self.sems[sid], 16)
        self._post(sid, dval[i], "dma", [out], [in_])
        if is_output:
            self.out_events.append((sid, dval[i]))
        self.n_ins += 1

    def barrier(self):
        for eng in ("pe", "act", "dve", "pool", "sp"):
            for sid, val in self.old:
                self._wait(eng, sid, val, "barrier")
            for e, c in self.cur.items():
                if c[1] > 0:
                    self._wait(eng, c[0], c[1], "barrier")
            for q in ("sp", "pool"):
                for i in range(NDMA):
                    if self.dval[q][i] > 0:
                        self._wait(eng, self.dsem[q][i], self.dval[q][i], "dma")

    def finish(self):
        self.barrier()
        for sid, val in self.out_events:
            self._wait("sp", sid, val, "dma")
        for e, c in self.cur.items():
            if c[1] > 0:
                self._wait("sp", c[0], c[1], e)

    @staticmethod
    def _a(x):
        return x[0] if isinstance(x, tuple) else x

    def mm(self, out, lhsT, rhs, start=True, stop=True):
        a = self._a
        self.op("pe", lambda: self.nc.tensor.matmul(a(out), a(lhsT), a(rhs), start=start, stop=stop),
                [out], [lhsT, rhs])

    def tr(self, out, in_, ident):
        a = self._a
        self.op("pe", lambda: self.nc.tensor.transpose(a(out), a(in_), a(ident)), [out], [in_, ident])

    def act(self, out, in_, func, bias=None, scale=None, eng="act"):
        a = self._a
        kw = {}
        ins = [in_]
        if bias is not None:
            kw["bias"] = a(bias) if not isinstance(bias, (int, float)) else bias
            if not isinstance(bias, (int, float)):
                ins.append(bias)
        if scale is not None:
            kw["scale"] = a(scale) if not isinstance(scale, (int, float)) else scale
            if not isinstance(scale, (int, float)):
                ins.append(scale)
        self.op("act", lambda: self.nc.scalar.activation(out=a(out), in_=a(in_), func=func, **kw), [out], ins)

    def tt(self, out, in0, in1, op, eng="dve"):
        a = self._a
        self.op(eng, lambda: self.E[eng].tensor_tensor(out=a(out), in0=a(in0), in1=a(in1), op=op), [out], [in0, in1])

    def ts(self, out, in0, s1, op0, s2=None, op1=None, eng="dve"):
        a = self._a
        ins = [in0]
        v1 = s1
        v2 = s2
        if not isinstance(s1, (int, float)):
            ins.append(s1)
            v1 = a(s1)
        if s2 is not None and not isinstance(s2, (int, float)):
            ins.append(s2)
            v2 = a(s2)
        kw = {}
        if op1 is not None:
            kw["op1"] = op1
        self.op(eng, lambda: self.E[eng].tensor_scalar(out=a(out), in0=a(in0), scalar1=v1, scalar2=v2, op0=op0, **kw),
                [out], ins)

    def stt(self, out, in0, scalar, in1, op0, op1):
        a = self._a
        ins = [in0, in1]
        v = scalar
        if not isinstance(scalar, (int, float)):
            ins.append(scalar)
            v = a(scalar)
        self.op("dve", lambda: self.nc.vector.scalar_tensor_tensor(out=a(out), in0=a(in0), scalar=v, in1=a(in1),
                                                                    op0=op0, op1=op1), [out], ins)

    def copy(self, out, in_, eng="dve"):
        a = self._a
        if eng == "act":
            self.op("act", lambda: self.nc.scalar.copy(out=a(out), in_=a(in_)), [out], [in_])
        else:
            self.op(eng, lambda: self.E[eng].tensor_copy(out=a(out), in_=a(in_)), [out], [in_])

    def recip(self, out, in_):
        a = self._a
        self.op("dve", lambda: self.nc.vector.reciprocal(out=a(out), in_=a(in_)), [out], [in_])

    def reduce(self, out, in_, op, axis=AX.X):
        a = self._a
        self.op("dve", lambda: self.nc.vector.tensor_reduce(out=a(out), in_=a(in_), axis=axis, op=op), [out], [in_])

    def memset(self, ap, val, eng="dve"):
        a = self._a
        self.op(eng, lambda: self.E[eng].memset(a(ap), val), [ap], [])


D = 1024
L = 4096
NCTX = 256
NT = L + NCTX
NTL = 34
N_IN = 3600
EPS = 1e-6
NE = 16
DFF = 512
C_MQ, C_MO, C_NQ, C_MK, C_MV, C_G, C_NK, C_NV = 0, 512, 1024, 1536, 2048, 2560, 2576, 3088
NEG = -30000.0


def _host_consts():
    f32 = np.float32
    c = {}
    c["ident"] = np.eye(128, dtype=f32)
    s = np.arange(128)
    c["maskF"] = (s[:, None] <= s[None, :]).astype(f32)
    c["maskB"] = (s[:, None] >= s[None, :]).astype(f32)
    c["ones"] = np.ones((128, 128), f32)
    sel = np.zeros((2, 2, 128), f32)
    sel[0, 0, :] = 1.0
    sel[1, 1, :] = 1.0
    c["sel2"] = sel
    t = np.arange(L)
    row = (t // 64).astype(np.float64)
    col = (t % 64).astype(np.float64)
    inv = 10000.0 ** (-np.arange(0, 64, 2, dtype=np.float64) / 64)
    ang = np.zeros((128, L))
    sgn = np.zeros((128, 1))
    for d in range(128):
        i = d % 32
        ang[d] = (row if d < 64 else col) * inv[i]
        sgn[d] = -1.0 if (d % 64) < 32 else 1.0
    cos = np.cos(ang.astype(f32).astype(np.float64))
    sin = np.sin(ang.astype(f32).astype(np.float64)) * sgn
    ks = 128.0 ** -0.5
    c["rope"] = np.stack([cos, sin, cos * ks, sin * ks]).astype(f32)
    perm = np.zeros((128, 128), f32)
    for dp in range(128):
        d = dp + 32 if (dp % 64) < 32 else dp - 32
        perm[d, dp] = 1.0
    c["perm"] = perm
    return c


def _na_index_tables():
    types = [0, 1, 2, 30, 31]
    idx_r = np.zeros((5, 128, 640), np.int64)
    idx_c = np.zeros((5, 128, 640), np.int64)
    valid = np.zeros((5, 128, 640), bool)
    for ti, j in enumerate(types):
        cs = min(max(j - 2, 0), 27)
        for q in range(128):
            r = 2 * j + q // 64
            cq = q % 64
            rs = min(max(r - 4, 0), 56)
            c0 = min(max(cq - 8, 0), 48)
            for key in range(640):
                kr = 2 * cs + key // 64
                kc = key % 64
                if rs <= kr < rs + 8 and c0 <= kc < c0 + 16:
                    valid[ti, q, key] = True
                    idx_r[ti, q, key] = kr - r + 7
                    idx_c[ti, q, key] = kc - cq + 15
    return idx_r, idx_c, valid


_NA_IDX = None


def _na_bias_tables(rpb):
    global _NA_IDX
    if _NA_IDX is None:
        _NA_IDX = _na_index_tables()
    idx_r, idx_c, valid = _NA_IDX
    g = rpb[:, :, idx_r, idx_c]
    g = np.where(valid[None, None], g, np.float32(NEG)).astype(np.float32)
    return np.ascontiguousarray(np.transpose(g, (0, 2, 1, 3, 4)))


class Prog:
    def __init__(self, debug=(), stop=None, nlayers=2):
        self.debug = set(debug)
        self.stop = stop
        self.nlayers = nlayers
        nc = bass.Bass("TRN2", target_bir_lowering=False)
        self.nc = nc
        self.k = KB(nc, same_engine_sync=SAME_ENGINE_SYNC)
        self.inp = {}
        self.dbg = {}

    def din(self, name, shape, dt=F32):
        ap = self.nc.dram_tensor(name, list(shape), dt, kind="ExternalInput").ap()
        self.inp[name] = ap
        return ap

    def dscr(self, name, shape, dt=F32):
        kind = "ExternalOutput" if name in self.debug else "Internal"
        ap = self.nc.dram_tensor(name, list(shape), dt, kind=kind).ap()
        if name in self.debug:
            self.dbg[name] = ap
        return ap

    def build(self):
        nc, k = self.nc, self.k
        I = self.din
        self.x_in = I("x", [L, D])
        self.ctx_in = I("ctxin", [NCTX, D])
        self.sT_in = I("sT", [128, 8, 2])
        self.ada_w = I("ada_w", [2, D, 6 * D])
        self.ada_b2 = I("ada_b2", [2, 2, 6 * D])
        self.n1w = I("n1w", [2, 128, 8])
        self.n2w = I("n2w", [2, 128, 8])
        self.w_in = I("w_in", [2, D, N_IN])
        self.convw = I("convw", [2, 3, 1024])
        self.convb = I("convb", [2, 128, 8])
        self.gate_b = I("gate_b", [2, 16])
        self.mnw = I("mnw", [2, 128, 4])
        self.nabias = I("nabias", [2, 5, 8, 128, 640])
        self.w_out = I("w_out", [2, D, D])
        self.rw = I("rw", [128, 8, 16])
        self.rb = I("rb", [16])
        self.w1 = I("w1", [2, NE, D, DFF])
        self.w3 = I("w3", [2, NE, D, DFF])
        self.w2 = I("w2", [2, NE, DFF, D])
        self.fnw = I("fnw", [D])
        self.c_ident = I("ident", [128, 128])
        self.c_maskF = I("maskF", [128, 128])
        self.c_maskB = I("maskB", [128, 128])
        self.c_ones = I("ones", [128, 128])
        self.c_sel2 = I("sel2", [2, 2, 128])
        self.c_rope = I("rope", [4, 128, L])
        self.c_perm = I("perm", [128, 128])
        self.out = nc.dram_tensor("out", [L, D], F32, kind="ExternalOutput").ap()
        S = self.dscr
        self.xres = S("xres", [NT, D])
        self.qT_d = S("qT_d", [4, 128, NT], BF16)
        self.kT_d = S("kT_d", [4, 128, NT], BF16)
        self.nqT_d = S("nqT_d", [4, 128, NT], BF16)
        self.nkT_d = S("nkT_d", [4, 128, NT], BF16)
        self.mo_d = S("mo_d", [NT, 512])
        self.mv_d = S("mv_d", [NT, 512], BF16)
        self.g_d = S("g_d", [NT, 16])
        self.nv_d = S("nv_d", [NT, 512], BF16)
        self.mixT_d = S("mixT_d", [8, 128, NT], BF16)
        self.hT_dbg = S("hT_dbg", [128, 8, NT + 4], BF16)
        self.mod_dbg = S("mod_dbg", [2, 6 * D])
        self.gate_dbg = S("gate_dbg", [128, NTL, 16])
        self.hs_dbg = S("hs_dbg", [128, NTL, 512])

        with ExitStack() as es:
            self.es = es
            sb = lambda shape, dt=F32, name=None: k.sb(es, shape, dt, name)
            self.ident = sb([128, 128], F32, "ident")
            self.identb = sb([128, 128], BF16, "identb")
            self.maskF = sb([128, 128], F32, "maskF")
            self.maskB = sb([128, 128], F32, "maskB")
            self.ones = sb([128, 128], F32, "ones")
            self.permb = sb([128, 128], BF16, "permb")
            self.sel2 = sb([2, 2, 128], F32, "sel2")
            k.dma(self.ident, self.c_ident)
            k.dma(self.identb, self.c_ident, q="pool")
            k.dma(self.maskF, self.c_maskF)
            k.dma(self.maskB, self.c_maskB)
            k.dma(self.ones, self.c_ones)
            k.dma(self.permb, self.c_perm, q="pool")
            k.dma(self.sel2, self.c_sel2.rearrange("w t p -> t w p"))
            self.rw_sb = sb([128, 8, 16], F32, "rw")
            k.dma(self.rw_sb, self.rw)
            self.rb_bc = sb([128, 16], F32, "rb")
            k.dma(self.rb_bc, self.rb.partition_broadcast(128))
            self.eps_t = sb([128, 1], F32, "eps")
            k.memset(self.eps_t, EPS)
            self.silu_s = sb([128, 8, 2], F32, "silu_s")
            tmp = sb([128, 8, 2], F32, "sT")
            k.dma(tmp, self.sT_in)
            k.act(self.silu_s, tmp, AF.Silu)
            for layer in range(self.nlayers):
                self.layer(layer)
                if self.stop is not None and self.stop[0] == layer:
                    break
            k.finish()
        return nc

    def xsrc(self, layer, i):
        if layer == 0:
            return self.x_in[i * 128:(i + 1) * 128, :] if i < 32 else self.ctx_in[(i - 32) * 128:(i - 31) * 128, :]
        return self.xres[i * 128:(i + 1) * 128, :]

    def alloc_hT(self, es):
        k = self.k
        self.hT = k.sb(es, [128, 8, NT + 4], BF16, "hT")
        for j in range(8):
            for c0 in (0, L + 1, L + 2, NT + 3):
                k.memset(self.hT[:, j, c0:c0 + 1], 0.0, eng="pool")

    def hcol(self, tile_i):
        return 1 + tile_i * 128 if tile_i < 32 else L + 3 + (tile_i - 32) * 128

    def stopped(self, layer, phase):
        return self.stop is not None and self.stop == (layer, phase)


def _layer(self, layer):
    k = self.k
    last = layer == 1
    with ExitStack() as les:
        sbl = lambda shape, dt=F32, name=None: k.sb(les, shape, dt, name)
        self.modcol = sbl([128, 4, 8, 2], F32, "modcol")
        self.ws1 = sbl([128, 8, 2], F32, "ws1")
        self.ws2 = sbl([128, 8, 2], F32, "ws2")
        self.bc = {(v, w): sbl([128, D], F32, f"bc{v}{w}") for v in (2, 5) for w in (0, 1)}
        self.gate_sb = sbl([128, NTL, 16], F32, "gate")
        self.adaln(layer)
        if self.stopped(layer, "A"):
            return
        with ExitStack() as hs:
            self.alloc_hT(hs)
            self.phase_norm1(layer)
            if self.stopped(layer, "B"):
                k.dma(self.hT_dbg, self.hT)
                k.barrier()
                return
            self.phase_proj(layer)
            k.barrier()
        if self.stopped(layer, "C"):
            return
        if MERGED_MIXER:
            self.phase_mix(layer)
        else:
            self.phase_mlstm(layer)
            if self.stopped(layer, "D1"):
                return
            self.phase_na(layer)
        if self.stopped(layer, "D2"):
            return
        with ExitStack() as hs:
            self.alloc_hT(hs)
            self.phase_wout_norm2(layer)
            if self.stopped(layer, "E"):
                k.dma(self.hT_dbg, self.hT)
                k.dma(self.gate_dbg, self.gate_sb)
                k.barrier()
                return
            self.phase_moe(layer)
            k.barrier()


def _adaln(self, layer):
    k = self.k
    with ExitStack() as es:
        sb = lambda shape, dt=F32, name=None: k.sb(es, shape, dt, name)
        wbuf = [sb([128, 8, 512], F32, "adaw") for _ in range(2)]
        self.modrow = sb([2, 6 * D], F32, "modrow")
        adab = sb([2, 6 * D], F32, "adab")
        k.dma(adab, self.ada_b2[layer])
        pss = [k.ps(es, [2, 512], F32, "adaps") for _ in range(2)]
        wv = self.ada_w[layer].rearrange("(j p) n -> p j n", p=128)
        for n in range(12):
            w = wbuf[n % 2]
            k.dma(w, wv[:, :, n * 512:(n + 1) * 512])
            p = pss[n % 2]
            for j in range(8):
                k.mm(p, self.silu_s[:, j, :], w[:, j, :], start=(j == 0), stop=(j == 7))
            k.tt(self.modrow[:, n * 512:(n + 1) * 512], p, adab[:, n * 512:(n + 1) * 512], ALU.add)
        pcol = k.ps(es, [128, 4, 8, 2], F32, "pcol")
        for vi, v in enumerate((0, 1, 3, 4)):
            for j in range(8):
                k.tr(pcol[:, vi, j, :], self.modrow[:, v * D + j * 128: v * D + (j + 1) * 128], self.ident[0:2, 0:2])
        k.copy(self.modcol, pcol)
        nw1 = sb([128, 8], F32, "nw1")
        nw2 = sb([128, 8], F32, "nw2")
        k.dma(nw1, self.n1w[layer])
        k.dma(nw2, self.n2w[layer])
        for w in range(2):
            k.ts(self.ws1[:, :, w], self.modcol[:, 1, :, w], 1.0, ALU.add)
            k.tt(self.ws1[:, :, w], self.ws1[:, :, w], nw1, ALU.mult)
            k.ts(self.ws2[:, :, w], self.modcol[:, 3, :, w], 1.0, ALU.add)
            k.tt(self.ws2[:, :, w], self.ws2[:, :, w], nw2, ALU.mult)
        self.sh1 = self.modcol[:, 0]
        self.sh2 = self.modcol[:, 2]
        pb = [k.ps(es, [128, 512], F32, "pbc") for _ in range(2)]
        n = 0
        for v in (2, 5):
            for w in range(2):
                for half in range(2):
                    p = pb[n % 2]
                    n += 1
                    k.mm(p, self.sel2[:, w, :], self.modrow[:, v * D + half * 512: v * D + (half + 1) * 512])
                    k.copy(self.bc[(v, w)][:, half * 512:(half + 1) * 512], p, eng="act")
        if self.stopped(layer, "A"):
            k.dma(self.mod_dbg, self.modrow)
        k.barrier()


def _norm_stats(self, xt, bufs):
    k = self.k
    junk, xn, st = bufs
    k.act(junk, xt, AF.Square)
    k.reduce(st[:, 0:1], junk, ALU.add)
    k.act(st[:, 1:2], st[:, 0:1], AF.Sqrt, bias=self.eps_t, scale=1.0 / D)
    k.recip(st[:, 2:3], st[:, 1:2])
    k.ts(xn, xt, st[:, 2:3], ALU.mult)


def _norm_tr(self, i, xn, pt, ws, sh, w, r32=None):
    k = self.k
    for j in range(8):
        k.tr(pt[:, j, :], xn[:, j * 128:(j + 1) * 128], self.ident)
    c0 = self.hcol(i)
    for j in range(8):
        dst = (self.hT[:, j, c0:c0 + 128], (i, j))
        if r32 is not None:
            k.act(r32[:, j, :], pt[:, j, :], AF.Identity, bias=sh[:, j, w:w + 1], scale=ws[:, j, w:w + 1])
            k.copy(dst, r32[:, j, :], eng="dve")
        elif i % 2 == 0:
            k.act(dst, pt[:, j, :], AF.Identity, bias=sh[:, j, w:w + 1], scale=ws[:, j, w:w + 1])
        else:
            k.ts(dst, pt[:, j, :], ws[:, j, w:w + 1], ALU.mult, sh[:, j, w:w + 1], ALU.add)


def _phase_norm1(self, layer):
    k = self.k
    with ExitStack() as es:
        sb = lambda shape, dt=F32, name=None: k.sb(es, shape, dt, name)
        xts = [sb([128, D], F32, "xt") for _ in range(3)]
        nb = [(sb([128, D], F32, "junk"), sb([128, D], F32, "xn"), sb([128, 4], F32, "st")) for _ in range(3)]
        pts = [k.ps(es, [128, 8, 128], F32, "ptr") for _ in range(2)]

        def sa(i):
            k.dma(xts[i % 3], self.xsrc(layer, i))
            self.norm_stats(xts[i % 3], nb[i % 3])

        sa(0)
        for i in range(NTL):
            if i + 1 < NTL:
                sa(i + 1)
            self.norm_tr(i, nb[i % 3][1], pts[i % 2], self.ws1, self.sh1, 0 if i < 32 else 1)
        k.barrier()


Prog.layer = _layer
Prog.adaln = _adaln
Prog.norm_stats = _norm_stats
Prog.norm_tr = _norm_tr
Prog.phase_norm1 = _phase_norm1


def _col(v, n):
    return np.ascontiguousarray(np.asarray(v, np.float32).reshape(n, 128).T)


def make_in_maps(inputs):
    f32 = np.float32
    g = {k_: np.asarray(v) for k_, v in inputs.items()}
    consts = _host_consts()
    shared = dict(consts)
    shared["ada_w"] = np.ascontiguousarray(g["ada_w"], f32)
    shared["ada_b2"] = np.ascontiguousarray(np.repeat(g["ada_b"][:, None, :], 2, axis=1), f32)
    shared["n1w"] = np.stack([_col(g["norm1_w"][l], 8) for l in range(2)])
    shared["n2w"] = np.stack([_col(g["norm2_w"][l], 8) for l in range(2)])
    shared["w_in"] = np.ascontiguousarray(g["w_in"], f32)
    shared["convw"] = np.ascontiguousarray(g["conv_w"], f32)
    shared["convb"] = np.stack([_col(g["conv_b"][l], 8) for l in range(2)])
    shared["gate_b"] = np.ascontiguousarray(g["gate_b"], f32)
    shared["mnw"] = np.stack([_col(g["mnorm_w"][l], 4) for l in range(2)])
    shared["nabias"] = _na_bias_tables(np.asarray(g["rpb"], f32))
    shared["w_out"] = np.ascontiguousarray(g["w_out"], f32)
    shared["rw"] = np.ascontiguousarray(np.asarray(g["router_w"], f32).reshape(8, 128, 16).transpose(1, 0, 2))
    shared["rb"] = np.ascontiguousarray(g["router_b"], f32)
    shared["w1"] = np.ascontiguousarray(g["exp_w1"], f32)
    shared["w3"] = np.ascontiguousarray(g["exp_w3"], f32)
    shared["w2"] = np.ascontiguousarray(g["exp_w2"], f32)
    shared["fnw"] = np.ascontiguousarray(g["final_norm_w"], f32)
    cc = _col(g["c_ctx"], 8)
    maps = []
    for b in range(8):
        m = dict(shared)
        m["x"] = np.ascontiguousarray(g["x"][b], f32)
        m["ctxin"] = np.ascontiguousarray(g["ctx"][b], f32)
        m["sT"] = np.ascontiguousarray(np.stack([_col(g["c"][b], 8), cc], axis=-1))
        maps.append(m)
    return maps


_PROG = None


def kernel(**inputs):
    global _PROG
    if _PROG is None:
        p = Prog()
        p.build()
        _PROG = p
    p = _PROG
    maps = make_in_maps(inputs)
    maps = [{n: m[n] for n in p.inp} for m in maps]
    res = run_bass_kernel_spmd(p.nc, maps, core_ids=list(range(8)))
    return np.stack([np.asarray(r["out"], np.float32) for r in res.results])


def _phase_proj(self, layer):
    k = self.k
    wv = self.w_in[layer].rearrange("(j p) n -> p j n", p=128)
    groups = [(g * 512, 512, 1 + g * 512, False) for g in range(8)] + [(L, 256, L + 3, True)]
    ks = 128.0 ** -0.5
    with ExitStack() as es:
        sb = lambda shape, dt=F32, name=None: k.sb(es, shape, dt, name)
        psA = [k.ps(es, [128, 512], F32, "psA") for _ in range(3)]
        psB = [k.ps(es, [128, 512], F32, "psB") for _ in range(2)]
        convb = sb([128, 8], F32, "convb")
        k.dma(convb, self.convb[layer])
        gb_bc = sb([128, 16], F32, "gb_bc")
        k.dma(gb_bc, self.gate_b[layer].partition_broadcast(128))
        with ExitStack() as es1:
            sb1 = lambda shape, dt=F32, name=None: k.sb(es1, shape, dt, name)
            wst = sb1([128, 8, 512], F32, "wst")
            bcw = sb1([128, 3, 512], F32, "bcw")
            wj = sb1([128, 3, 8, 512], BF16, "wj")
            ropes = [sb1([128, 2, 512], F32, "rope") for _ in range(2)]
            xbs = [sb1([128, 512], BF16, "xb") for _ in range(2)]
            t1s = [sb1([128, 512], F32, "t1") for _ in range(2)]
            t2s = [sb1([128, 512], F32, "t2") for _ in range(2)]
            obs = [sb1([128, 512], BF16, "ob") for _ in range(2)]
            n = 0
            for blk, (c0, dst_d, ridx) in enumerate(((C_MQ, self.qT_d, 0), (C_MK, self.kT_d, 2))):
                k.dma(wst, wv[:, :, c0:c0 + 512])
                for j in range(3):
                    k.dma(bcw[:, j, :], self.convw[layer, j, blk * 512:(blk + 1) * 512].partition_broadcast(128))
                for j in range(3):
                    for dch in range(8):
                        k.tt(wj[:, j, dch, :], wst[:, dch, :], bcw[:, j, :], ALU.mult)
                for gi, (t0, nt, hc0, isctx) in enumerate(groups):
                    rp = ropes[gi % 2]
                    if not isctx:
                        k.dma(rp, self.c_rope[ridx:ridx + 2, :, t0:t0 + 512].rearrange("c p n -> p c n"))
                    for hh in range(4):
                        p = psA[n % 3]
                        xb, t1, t2, ob = xbs[n % 2], t1s[n % 2], t2s[n % 2], obs[n % 2]
                        nmm = 0
                        for j in range(3):
                            for dch in range(8):
                                k.mm(p[:, :nt], wj[:, j, dch, hh * 128:(hh + 1) * 128],
                                     self.hT[:, dch, hc0 - 1 + j: hc0 - 1 + j + nt],
                                     start=(nmm == 0), stop=(nmm == 23))
                                nmm += 1
                        k.act(xb[:, :nt], p[:, :nt], AF.Silu, bias=convb[:, blk * 4 + hh: blk * 4 + hh + 1])
                        if not isctx:
                            p2 = psB[n % 2]
                            k.mm(p2, self.permb, xb)
                            k.tt(t1, p2, rp[:, 1, :], ALU.mult)
                            k.tt(t2, xb, rp[:, 0, :], ALU.mult)
                            k.tt(ob, t1, t2, ALU.add)
                            k.dma(dst_d[hh, :, t0:t0 + nt], ob[:, :nt])
                        elif blk == 0:
                            k.dma(dst_d[hh, :, t0:t0 + nt], xb[:, :nt])
                        else:
                            k.act(ob[:, :nt], xb[:, :nt], AF.Copy, scale=ks)
                            k.dma(dst_d[hh, :, t0:t0 + nt], ob[:, :nt])
                        n += 1
            k.barrier()
        with ExitStack() as es2:
            sb2 = lambda shape, dt=F32, name=None: k.sb(es2, shape, dt, name)
            wbs = [sb2([128, 8, 512], BF16, "wb") for _ in range(2)]
            obs = [sb2([128, 512], BF16, "ob2") for _ in range(3)]
            o32s = [sb2([128, 512], F32, "o32") for _ in range(2)]
            n = 0
            wi = 0
            for (c0, dst_d, scale) in ((C_NQ, self.nqT_d, 0.125), (C_NK, self.nkT_d, 1.0)):
                wb = wbs[wi % 2]
                wi += 1
                k.dma(wb, wv[:, :, c0:c0 + 512], q="pool")
                for (t0, nt, hc0, isctx) in groups:
                    for tt_ in range(4):
                        p = psA[n % 3]
                        ob = obs[n % 3]
                        for dch in range(8):
                            k.mm(p[:, :nt], wb[:, dch, tt_ * 128:(tt_ + 1) * 128], self.hT[:, dch, hc0:hc0 + nt],
                                 start=(dch == 0), stop=(dch == 7))
                        k.act(ob[:, :nt], p[:, :nt], AF.Copy, scale=scale)
                        k.dma(dst_d[tt_, :, t0:t0 + nt], ob[:, :nt])
                        n += 1
            for (c0, ncol, kind) in ((C_MO, 512, "mo"), (C_MV, 512, "mv"), (C_G, 16, "g"), (C_NV, 512, "nv")):
                wb = wbs[wi % 2]
                wi += 1
                k.dma(wb[:, :, :ncol], wv[:, :, c0:c0 + ncol], q="pool")
                for i in range(NTL):
                    hc = self.hcol(i)
                    p = psA[n % 3]
                    for dch in range(8):
                        k.mm(p[:, :ncol], self.hT[:, dch, hc:hc + 128], wb[:, dch, :ncol],
                             start=(dch == 0), stop=(dch == 7))
                    rows = slice(i * 128, (i + 1) * 128)
                    if kind == "mo":
                        o = o32s[n % 2]
                        k.act(o, p, AF.Sigmoid)
                        k.dma(self.mo_d[rows, :], o)
                    elif kind == "g":
                        o = o32s[n % 2]
                        k.tt(o[:, :16], p[:, :16], gb_bc, ALU.add)
                        k.dma(self.g_d[rows, :], o[:, :16])
                    else:
                        ob = obs[n % 3]
                        if n % 2 == 0:
                            k.copy(ob, p, eng="act")
                        else:
                            k.copy(ob, p, eng="dve")
                        k.dma((self.mv_d if kind == "mv" else self.nv_d)[rows, :], ob)
                    n += 1
            k.barrier()


Prog.phase_proj = _phase_proj


def _phase_mlstm(self, layer):
    k = self.k
    with ExitStack() as es:
        sb = lambda shape, dt=F32, name=None: k.sb(es, shape, dt, name)
        EB = sb([128, NTL, 8], F32, "EB")
        WW = sb([128, NTL, 8], F32, "WW")
        UU = sb([128, NTL, 8], F32, "UU")
        ET = sb([128, NTL, 8], F32, "ET")
        hsum = sb([128, NTL, 512], F32, "hsum")
        one_c = self.ones[:, 0:1]
        with ExitStack() as es1:
            sb1 = lambda shape, dt=F32, name=None: k.sb(es1, shape, dt, name)
            gall = sb1([128, NTL, 2, 2, 4], F32, "gall")
            k.dma(gall, self.g_d.rearrange("(c p) (d t h) -> p c d t h", p=128, d=2, t=2))
            gv = gall.rearrange("p c d t h -> p d c t h")
            e1 = sb1([128, 2, NTL, 4], F32, "e1")
            l = sb1([128, 2, NTL, 4], F32, "lsp")
            cc = sb1([128, 2, NTL, 4], F32, "cc")
            ct = sb1([128, 2, NTL, 4], F32, "ct")
            t1 = sb1([128, 2, NTL, 4], F32, "gt1")
            t2 = sb1([128, 2, NTL, 4], F32, "gt2")
            pgf = k.ps(es1, [128, NTL, 4], F32, "pgf")
            pgb = k.ps(es1, [128, NTL, 4], F32, "pgb")
            ptot = k.ps(es1, [128, 2, NTL, 4], F32, "ptot")
            k.act(e1, gv[:, :, :, 1, :], AF.Exp, scale=-1.0)
            k.act(l, e1, AF.Ln, bias=one_c)
            k.mm(pgf, self.maskF, l[:, 0])
            k.mm(pgb, self.maskB, l[:, 1])
            k.mm(ptot, self.ones, l)
            k.copy(cc[:, 0], pgf, eng="act")
            k.copy(cc[:, 1], pgb, eng="act")
            k.copy(ct, ptot, eng="act")
            v = lambda X: X.rearrange("p c (d h) -> p d c h", d=2)
            k.act(v(EB), cc, AF.Exp, scale=-1.0)
            k.tt(t1, cc, gv[:, :, :, 0, :], ALU.add)
            k.act(v(WW), t1, AF.Exp)
            k.tt(t2, t1, ct, ALU.subtract)
            k.act(v(UU), t2, AF.Exp)
            k.act(v(ET), ct, AF.Exp, scale=-1.0)
            k.barrier()
        Cst = [sb([128, 129], F32, "Cst") for _ in range(8)]
        Cbf = [sb([128, 129], BF16, "Cbf") for _ in range(8)]
        for hd in range(8):
            k.memset(Cst[hd], 0.0)
            k.memset(Cbf[hd], 0.0, eng="pool")
        qTs = [[sb([128, 4, 128], BF16, "qT") for _ in range(2)] for _ in range(2)]
        kTs = [[sb([128, 4, 128], BF16, "kT") for _ in range(2)] for _ in range(2)]
        vaug = [[sb([128, 4, 129], BF16, "vaug") for _ in range(2)] for _ in range(2)]
        for vv in vaug:
            for v_ in vv:
                k.memset(v_[:, :, 128:129], 1.0)
        ktoks = [sb([128, 128], BF16, "ktok") for _ in range(3)]
        STs = [sb([128, 128], BF16, "ST") for _ in range(3)]
        uvs = [sb([128, 129], BF16, "uv") for _ in range(3)]
        sms = [sb([128, 6, 2, 2], F32, "sm") for _ in range(2)]
        sq = sb([128, 512], F32, "sq")
        tmpm = sb([128, 512], F32, "tmpm")
        mot = sb([128, 512], F32, "mot")
        mtok = sb([128, 512], BF16, "mtok")
        mixT = sb([128, 4, 128], BF16, "mixT")
        hst = sb([128, 12], F32, "hst")
        mnw = sb([128, 4], F32, "mnw")
        k.dma(mnw, self.mnw[layer])
        pks = [k.ps(es, [128, 1024], BF16, "pk") for _ in range(2)]
        pSs = [k.ps(es, [128, 512], F32, "pS") for _ in range(2)]
        pNt = k.ps(es, [128, 2, 512], F32, "pN")
        pCb = k.ps(es, [128, 512], F32, "pCb")
        pfin = k.ps(es, [128, 1024], BF16, "pfin")
        order = {0: [32, 33] + list(range(32)), 1: [33, 32] + list(range(31, -1, -1))}
        masks = (self.maskF, self.maskB)
        items = [(s_, d, h) for s_ in range(NTL) for d in (0, 1) for h in range(4)]
        seen = set()

        def bufs(s_, d):
            return qTs[d][s_ % 2], kTs[d][s_ % 2], vaug[d][s_ % 2]

        def stage_A(n):
            s_, d, h = items[n]
            c = order[d][s_]
            qT, kT, va = bufs(s_, d)
            if h == 0:
                rows = slice(c * 128, (c + 1) * 128)
                k.dma(qT, self.qT_d[:, :, rows].rearrange("h p n -> p h n"))
                k.dma(kT, self.kT_d[:, :, rows].rearrange("h p n -> p h n"))
                k.dma(va[:, :, 0:128], self.mv_d[rows, :].rearrange("p (h e) -> p h e", h=4))
            k.tr(pks[n % 2][:, 0:128], kT[:, h, :], self.identb)
            k.mm(pSs[n % 2][:, 0:128], kT[:, h, :], qT[:, h, :])

        def stage_B(n):
            s_, d, h = items[n]
            c = order[d][s_]
            hd = d * 4 + h
            qT, kT, va = bufs(s_, d)
            ktok, ST, uv = ktoks[n % 3], STs[n % 3], uvs[n % 3]
            k.copy(ktok, pks[n % 2][:, 0:128], eng="act")
            k.stt(ST, pSs[n % 2][:, 0:128], WW[:, c, hd:hd + 1], masks[d], ALU.mult, ALU.mult)
            k.act(uv, va[:, h, :], AF.Copy, scale=UU[:, c, hd:hd + 1])
            pN = pNt[:, h // 2, (h % 2) * 129:(h % 2) * 129 + 129]
            pC = pCb[:, 0:129]
            k.mm(pN, ST, va[:, h, :], start=True, stop=False)
            k.mm(pN, qT[:, h, :], Cbf[hd], start=False, stop=True)
            k.mm(pC, ktok, uv)
            k.stt(Cst[hd], Cst[hd], ET[:, c, hd:hd + 1], pC, ALU.mult, ALU.add)
            k.copy(Cbf[hd], Cst[hd], eng="act")

        def stage_E(s_, d):
            c = order[d][s_]
            sm = sms[d]
            eb4 = EB[:, c, d * 4:(d + 1) * 4].rearrange("p (b h) -> p b h", b=2)
            for b in range(2):
                qn2 = pNt[:, b, 0:258].rearrange("p (h e) -> p h e", e=129)[:, :, 128]
                k.tt(sm[:, 0, b, :], qn2, eb4[:, b, :], ALU.mult)
            k.ts(sm[:, 1], sm[:, 0], -1.0, ALU.mult)
            k.tt(sm[:, 2], sm[:, 0], sm[:, 1], ALU.max)
            k.ts(sm[:, 3], sm[:, 2], 1.0, ALU.max)
            k.recip(sm[:, 4], sm[:, 3])
            k.tt(sm[:, 5], sm[:, 4], eb4, ALU.mult)
            for h in range(4):
                sc = sm[:, 5, h // 2, h % 2:h % 2 + 1]
                num = pNt[:, h // 2, (h % 2) * 129:(h % 2) * 129 + 128]
                dst = (hsum[:, c, h * 128:(h + 1) * 128], c)
                if c not in seen:
                    k.ts(dst, num, sc, ALU.mult)
                else:
                    k.stt(dst, num, sc, dst, ALU.mult, ALU.add)
            if c in seen:
                return c
            seen.add(c)
            return None

        def finalize(c):
            rows = slice(c * 128, (c + 1) * 128)
            hs = (hsum[:, c, :], c)
            k.act(sq, hs, AF.Square)
            k.reduce(hst[:, 0:4], sq.rearrange("p (h e) -> p h e", h=4), ALU.add)
            k.act(hst[:, 4:8], hst[:, 0:4], AF.Sqrt, bias=self.eps_t, scale=1.0 / 128)
            k.recip(hst[:, 8:12], hst[:, 4:8])
            k.dma(mot, self.mo_d[rows, :])
            k.tt(tmpm, hs, mot, ALU.mult)
            for h in range(4):
                k.ts(mtok[:, h * 128:(h + 1) * 128], tmpm[:, h * 128:(h + 1) * 128], hst[:, 8 + h:9 + h], ALU.mult)
            for h in range(4):
                k.tr(pfin[:, h * 128:(h + 1) * 128], mtok[:, h * 128:(h + 1) * 128], self.identb)
            for h in range(4):
                k.act(mixT[:, h, :], pfin[:, h * 128:(h + 1) * 128], AF.Copy, scale=mnw[:, h:h + 1])
            k.dma(self.mixT_d[0:4, :, rows].rearrange("c p n -> p c n"), mixT)

        stage_A(0)
        pend_fin = []
        for n, (s_, d, h) in enumerate(items):
            if n + 1 < len(items):
                stage_A(n + 1)
            for c in pend_fin:
                finalize(c)
            pend_fin = []
            stage_B(n)
            if h == 3:
                done = stage_E(s_, d)
                if done is not None:
                    pend_fin.append(done)
        for c in pend_fin:
            finalize(c)
        if self.stopped(layer, "D1"):
            k.dma(self.hs_dbg, hsum)
        k.barrier()


Prog.phase_mlstm = _phase_mlstm


def _phase_na(self, layer):
    k = self.k
    last = layer == 1
    with ExitStack() as es:
        sb = lambda shape, dt=F32, name=None: k.sb(es, shape, dt, name)
        biasA = sb([128, 8, 640], BF16, "biasA")
        biasS = sb([128, 8, 640], BF16, "biasS")
        k.dma(biasA, self.nabias[layer, 2].rearrange("h q n -> q h n"), q="pool")
        nqv = self.nqT_d.rearrange("t (two p) n -> p (t two) n", p=64)
        nkv = self.nkT_d.rearrange("t (two p) n -> p (t two) n", p=64)
        kctx = sb([64, 8, 256], BF16, "kctx")
        k.dma(kctx, nkv[:, :, L:NT])
        vctx = sb([128, 2, 8, 65], BF16, "vctx")
        k.memset(vctx[:, :, :, 64:65], 1.0)
        for cc in range(2):
            k.dma(vctx[:, cc, :, 0:64],
                  self.nv_d[L + cc * 128:L + (cc + 1) * 128, :].rearrange("p (h e) -> p h e", h=8))
        qs = [sb([64, 8, 128], BF16, "naq") for _ in range(2)]
        kws = [sb([64, 8, 640], BF16, "nak") for _ in range(2)]
        vws = [sb([128, 5, 8, 65], BF16, "nav") for _ in range(2)]
        for v in vws:
            k.memset(v[:, :, :, 64:65], 1.0)
        PTs = [sb([128, 7, 128], BF16, "PT") for _ in range(2)]
        ots = [sb([128, 512], BF16, "otok") for _ in range(2)]
        rcs = [sb([128, 8], F32, "rc") for _ in range(2)]
        mxs = [sb([128, 512], BF16, "mx") for _ in range(2)]
        pSs = [k.ps(es, [128, 8, 128], F32, "naS") for _ in range(2)]
        pOs = [k.ps(es, [128, 512], F32, "naO") for _ in range(2)]
        ptr = k.ps(es, [128, 1024], BF16, "natr")
        types = {0: 0, 1: 1, 30: 3, 31: 4}
        blocks = [(j, True) for j in range(32)] + ([] if last else [(0, False), (1, False)])
        binfo = {}

        def prologue(bi):
            j, lat = blocks[bi]
            q, kw, vw = qs[bi % 2], kws[bi % 2], vws[bi % 2]
            bias = None
            if lat:
                tok0 = j * 128
                cs = min(max(j - 2, 0), 27)
                nwin = 5
                if j in types:
                    k.dma(biasS, self.nabias[layer, types[j]].rearrange("h q n -> q h n"), q="pool")
                    bias = biasS
                else:
                    bias = biasA
                k.dma(kw, nkv[:, :, cs * 128:cs * 128 + 640])
                for c in range(5):
                    k.dma(vw[:, c, :, 0:64],
                          self.nv_d[(cs + c) * 128:(cs + c + 1) * 128, :].rearrange("p (h e) -> p h e", h=8))
            else:
                tok0 = L + j * 128
                nwin = 0
            k.dma(q, nqv[:, :, tok0:tok0 + 128])
            binfo[bi] = (tok0, nwin, bias)

        def stage_S(bi, hh, n):
            if hh == 0:
                prologue(bi)
            tok0, nwin, bias = binfo[bi]
            q, kw = qs[bi % 2], kws[bi % 2]
            pS = pSs[n % 2]
            for c in range(nwin):
                k.mm(pS[:, c, :], kw[:, hh, c * 128:(c + 1) * 128], q[:, hh, :], start=True, stop=False)
                k.mm(pS[:, c, :], bias[:, hh, c * 128:(c + 1) * 128], self.identb, start=False, stop=True)
            for cc in range(2):
                k.mm(pS[:, nwin + cc, :], kctx[:, hh, cc * 128:(cc + 1) * 128], q[:, hh, :])

        def stage_rest(bi, hh, n):
            tok0, nwin, bias = binfo[bi]
            vw, ot, rc = vws[bi % 2], ots[bi % 2], rcs[bi % 2]
            pS, PT, pO = pSs[n % 2], PTs[n % 2], pOs[n % 2][:, 0:65]
            nch = nwin + 2
            if nch > 4:
                k.act(PT[:, 0:4, :], pS[:, 0:4, :], AF.Exp)
                k.act(PT[:, 4:nch, :], pS[:, 4:nch, :], AF.Exp)
            else:
                k.act(PT[:, 0:nch, :], pS[:, 0:nch, :], AF.Exp)
            for c in range(nch):
                rhs = vw[:, c, hh, :] if c < nwin else vctx[:, c - nwin, hh, :]
                k.mm(pO, PT[:, c, :], rhs, start=(c == 0), stop=(c == nch - 1))
            k.recip(rc[:, hh:hh + 1], pO[:, 64:65])
            k.ts(ot[:, hh * 64:(hh + 1) * 64], pO[:, 0:64], rc[:, hh:hh + 1], ALU.mult)

        def epilogue(bi):
            tok0 = binfo[bi][0]
            ot, mx = ots[bi % 2], mxs[bi % 2]
            po = (bi % 2) * 512
            for t4 in range(4):
                k.tr((ptr[:, po + t4 * 128:po + (t4 + 1) * 128], bi % 2), ot[:, t4 * 128:(t4 + 1) * 128], self.identb)
            k.copy(mx, (ptr[:, po:po + 512], bi % 2), eng="act")
            k.dma(self.mixT_d[4:8, :, tok0:tok0 + 128].rearrange("c p n -> p c n"),
                  mx.rearrange("p (c n) -> p c n", c=4))

        items = [(bi, hh) for bi in range(len(blocks)) for hh in range(8)]
        stage_S(items[0][0], items[0][1], 0)
        pending_epi = None
        for n, (bi, hh) in enumerate(items):
            if n + 1 < len(items):
                stage_S(items[n + 1][0], items[n + 1][1], n + 1)
            if pending_epi is not None:
                epilogue(pending_epi)
                pending_epi = None
            stage_rest(bi, hh, n)
            if hh == 7:
                pending_epi = bi
        epilogue(pending_epi)
        k.barrier()


Prog.phase_na = _phase_na


def _router(self, LG, n, es):
    k = self.k
    t16 = lambda nm: k.sb(es, [128, n, 16], F32, nm)
    t1 = lambda nm: k.sb(es, [128, n], F32, nm)
    B16 = lambda a: a.unsqueeze(2).to_broadcast([128, n, 16])
    mx, ssum, rs, gm, m1, m2, wsum, rws = [t1(f"r1_{j}") for j in range(8)]
    e, sc, sel, m16, tt16, msel, is1, msel2, is2, wts = [t16(f"r16_{j}") for j in range(10)]
    ps6 = k.sb(es, [128, n, 4, 6], F32, "ps6")
    gs = k.sb(es, [128, n, 4], F32, "gs")
    ing = k.sb(es, [128, n, 4], F32, "ing")
    k.reduce(mx, LG, ALU.max)
    k.tt(e, LG, B16(mx), ALU.subtract)
    k.act(e, e, AF.Exp)
    k.reduce(ssum, e, ALU.add)
    k.recip(rs, ssum)
    k.tt(sc, e, B16(rs), ALU.mult)
    k.tt(sel, sc, self.rb_bc.unsqueeze(1).to_broadcast([128, n, 16]), ALU.add)
    selv = sel.rearrange("p n (g e) -> p n g e", g=4)
    for pi, (a, b) in enumerate(((0, 1), (0, 2), (0, 3), (1, 2), (1, 3), (2, 3))):
        k.tt(ps6[:, :, :, pi], selv[:, :, :, a], selv[:, :, :, b], ALU.add)
    k.reduce(gs, ps6, ALU.max)
    k.reduce(gm, gs, ALU.max)
    k.tt(ing, gs, gm.unsqueeze(2).to_broadcast([128, n, 4]), ALU.is_equal)
    k.copy(m16.rearrange("p n (g e) -> p n g e", g=4), ing.unsqueeze(3).to_broadcast([128, n, 4, 4]))
    k.ts(tt16, m16, 10.0, ALU.mult, -10.0, ALU.add)
    k.tt(msel, sel, m16, ALU.mult)
    k.tt(msel, msel, tt16, ALU.add)
    k.reduce(m1, msel, ALU.max)
    k.tt(is1, msel, B16(m1), ALU.is_equal)
    k.stt(msel2, is1, -20.0, msel, ALU.mult, ALU.add)
    k.reduce(m2, msel2, ALU.max)
    k.tt(is2, msel2, B16(m2), ALU.is_equal)
    k.tt(is1, is1, is2, ALU.add)
    k.tt(wts, sc, is1, ALU.mult)
    k.reduce(wsum, wts, ALU.add)
    k.recip(rws, wsum)
    k.tt(self.gate_sb[:, 0:n, :], wts, B16(rws), ALU.mult)


def _phase_wout_norm2(self, layer):
    k = self.k
    last = layer == 1
    ntl = 32 if last else NTL
    with ExitStack() as es:
        sb = lambda shape, dt=F32, name=None: k.sb(es, shape, dt, name)
        wo = sb([128, 8, D], BF16, "wo")
        wov = self.w_out[layer].rearrange("(j p) n -> p j n", p=128)
        for half in range(2):
            k.dma(wo[:, :, half * 512:(half + 1) * 512], wov[:, :, half * 512:(half + 1) * 512], q="pool")
        mixs = [sb([128, 8, 128], BF16, "mix") for _ in range(2)]
        xts = [sb([128, D], F32, "xt2") for _ in range(4)]
        tmps = [sb([128, D], F32, "tmp2") for _ in range(2)]
        nb = [(sb([128, D], F32, "junk"), sb([128, D], F32, "xn"), sb([128, 4], F32, "st")) for _ in range(3)]
        pts = [k.ps(es, [128, 8, 128], F32, "ptr") for _ in range(2)]
        r32s = [sb([128, 8, 128], F32, "r32") for _ in range(3)]
        LG = sb([128, ntl, 16], F32, "LG")
        py = k.ps(es, [128, 2, 512], F32, "py")
        plogs = [k.ps(es, [128, 16], F32, "plog") for _ in range(2)]

        def stage1(i):
            w = 0 if i < 32 else 1
            rows = slice(i * 128, (i + 1) * 128)
            mix, xt, tmp = mixs[i % 2], xts[i % 4], tmps[i % 2]
            k.dma(mix, self.mixT_d[:, :, rows].rearrange("c p n -> p c n"))
            k.dma(xt, self.xsrc(layer, i))
            for half in range(2):
                for mch in range(8):
                    k.mm(py[:, half, :], mix[:, mch, :], wo[:, mch, half * 512:(half + 1) * 512],
                         start=(mch == 0), stop=(mch == 7))
            for half in range(2):
                k.tt(tmp[:, half * 512:(half + 1) * 512], py[:, half, :], self.bc[(2, w)][:, half * 512:(half + 1) * 512], ALU.mult)
            k.tt(xt, xt, tmp, ALU.add)
            k.dma((self.xres[rows, :], i), xt)

        def stage2a(i):
            self.norm_stats(xts[i % 4], nb[i % 3])

        def stage2b(i):
            self.norm_tr(i, nb[i % 3][1], pts[i % 2], self.ws2, self.sh2, 0 if i < 32 else 1, r32=r32s[i % 3])

        def stage2c(i):
            r32, plog = r32s[i % 3], plogs[i % 2]
            for dch in range(8):
                k.mm(plog, r32[:, dch, :], self.rw_sb[:, dch, :], start=(dch == 0), stop=(dch == 7))
            k.copy((LG[:, i, :], i), plog)

        for t in range(ntl + 3):
            if t < ntl:
                stage1(t)
            if 0 <= t - 1 < ntl:
                stage2a(t - 1)
            if 0 <= t - 2 < ntl:
                stage2b(t - 2)
            if 0 <= t - 3 < ntl:
                stage2c(t - 3)
        self.router(LG, ntl, es)
        k.barrier()


Prog.router = _router
Prog.phase_wout_norm2 = _phase_wout_norm2


def _phase_moe(self, layer):
    k = self.k
    last = layer == 1
    ntl = 32 if last else NTL
    nblk = 4
    bounds = [round(b * ntl / nblk) for b in range(nblk + 1)]
    with ExitStack() as es:
        sb = lambda shape, dt=F32, name=None: k.sb(es, shape, dt, name)
        nbmax = max(bounds[b + 1] - bounds[b] for b in range(nblk))
        yacc = sb([128, nbmax, 2, 512], F32, "yacc")
        w1s = [sb([128, 8, DFF], BF16, "w1") for _ in range(2)]
        w3s = [sb([128, 8, DFF], BF16, "w3") for _ in range(2)]
        w2s = [sb([128, 4, D], BF16, "w2") for _ in range(2)]
        aTs = [sb([128, 4, 512], BF16, "aT") for _ in range(2)]
        sTs = [sb([128, 512], F32, "sT") for _ in range(2)]
        xts = [sb([128, D], F32, "xt3") for _ in range(2)]
        tmps = [sb([128, D], F32, "tmp3") for _ in range(2)]
        st = sb([128, 4], F32, "st3")
        if last:
            fnw_bc = sb([128, D], F32, "fnw")
            k.dma(fnw_bc, self.fnw.partition_broadcast(128))
        p1s = [k.ps(es, [128, 512], F32, "p1") for _ in range(2)]
        p3s = [k.ps(es, [128, 512], F32, "p3") for _ in range(2)]
        pys = [k.ps(es, [128, 2, 512], F32, "pym") for _ in range(2)]
        w1v = self.w1[layer].rearrange("e (j p) n -> e p j n", p=128)
        w3v = self.w3[layer].rearrange("e (j p) n -> e p j n", p=128)
        w2v = self.w2[layer].rearrange("e (j p) n -> e p j n", p=128)
        nw = 0
        nh = 0
        ny = 0
        for b in range(nblk):
            tiles = list(range(bounds[b], bounds[b + 1]))
            groups = []
            for i in tiles:
                if groups and len(groups[-1]) < 4 and self.hcol(groups[-1][-1]) + 128 == self.hcol(i):
                    groups[-1].append(i)
                else:
                    groups.append([i])
            for e in range(NE):
                w1, w3, w2 = w1s[nw % 2], w3s[nw % 2], w2s[nw % 2]
                nw += 1
                k.dma(w1, w1v[e], q="pool")
                k.dma(w3, w3v[e], q="pool")
                k.dma(w2, w2v[e], q="pool")
                for grp in groups:
                    nt = 128 * len(grp)
                    c0 = self.hcol(grp[0])
                    aT = aTs[nh % 2]
                    for fch in range(4):
                        p1, p3, sT = p1s[nh % 2], p3s[nh % 2], sTs[nh % 2]
                        nh += 1
                        for dch in range(8):
                            k.mm(p1[:, :nt], w1[:, dch, fch * 128:(fch + 1) * 128], self.hT[:, dch, c0:c0 + nt],
                                 start=(dch == 0), stop=(dch == 7))
                        for dch in range(8):
                            k.mm(p3[:, :nt], w3[:, dch, fch * 128:(fch + 1) * 128], self.hT[:, dch, c0:c0 + nt],
                                 start=(dch == 0), stop=(dch == 7))
                        k.act(sT[:, :nt], p1[:, :nt], AF.Silu)
                        k.tt(aT[:, fch, :nt], p3[:, :nt], sT[:, :nt], ALU.mult)
                    for tl, i in enumerate(grp):
                        bt = i - bounds[b]
                        py = pys[ny % 2]
                        ny += 1
                        for half in range(2):
                            for fch in range(4):
                                k.mm(py[:, half, :], aT[:, fch, tl * 128:(tl + 1) * 128],
                                     w2[:, fch, half * 512:(half + 1) * 512], start=(fch == 0), stop=(fch == 3))
                        gcol = (self.gate_sb[:, i, e:e + 1], i)
                        for half in range(2):
                            ya = (yacc[:, bt, half, :], (bt, half))
                            if e == 0:
                                k.ts(ya, py[:, half, :], gcol, ALU.mult)
                            else:
                                k.stt(ya, py[:, half, :], gcol, ya, ALU.mult, ALU.add)
            for i in tiles:
                bt = i - bounds[b]
                w = 0 if i < 32 else 1
                rows = slice(i * 128, (i + 1) * 128)
                xt, tmp = xts[i % 2], tmps[i % 2]
                k.dma(xt, (self.xres[rows, :], i))
                for half in range(2):
                    k.tt(tmp[:, half * 512:(half + 1) * 512], (yacc[:, bt, half, :], (bt, half)),
                         self.bc[(5, w)][:, half * 512:(half + 1) * 512], ALU.mult)
                k.tt(xt, xt, tmp, ALU.add)
                if not last:
                    k.dma((self.xres[rows, :], i), xt)
                else:
                    k.act(tmp, xt, AF.Square)
                    k.reduce(st[:, 0:1], tmp, ALU.add)
                    k.act(st[:, 1:2], st[:, 0:1], AF.Sqrt, bias=self.eps_t, scale=1.0 / D)
                    k.recip(st[:, 2:3], st[:, 1:2])
                    k.stt(tmp, xt, st[:, 2:3], fnw_bc, ALU.mult, ALU.mult)
                    k.dma(self.out[rows, :], tmp, is_output=True)
        k.barrier()


Prog.phase_moe = _phase_moe


def _phase_mix(self, layer):
    k = self.k
    last = layer == 1
    with ExitStack() as es:
        sb = lambda shape, dt=F32, name=None: k.sb(es, shape, dt, name)
        EB = sb([128, NTL, 8], F32, "EB")
        WW = sb([128, NTL, 8], F32, "WW")
        UU = sb([128, NTL, 8], F32, "UU")
        ET = sb([128, NTL, 8], F32, "ET")
        hsum = sb([128, NTL, 512], F32, "hsum")
        one_c = self.ones[:, 0:1]
        with ExitStack() as es1:
            sb1 = lambda shape, dt=F32, name=None: k.sb(es1, shape, dt, name)
            gall = sb1([128, NTL, 2, 2, 4], F32, "gall")
            k.dma(gall, self.g_d.rearrange("(c p) (d t h) -> p c d t h", p=128, d=2, t=2))
            gv = gall.rearrange("p c d t h -> p d c t h")
            e1 = sb1([128, 2, NTL, 4], F32, "e1")
            l = sb1([128, 2, NTL, 4], F32, "lsp")
            cc = sb1([128, 2, NTL, 4], F32, "cc")
            ct = sb1([128, 2, NTL, 4], F32, "ct")
            t1 = sb1([128, 2, NTL, 4], F32, "gt1")
            t2 = sb1([128, 2, NTL, 4], F32, "gt2")
            pgf = k.ps(es1, [128, NTL, 4], F32, "pgf")
            pgb = k.ps(es1, [128, NTL, 4], F32, "pgb")
            ptot = k.ps(es1, [128, 2, NTL, 4], F32, "ptot")
            k.act(e1, gv[:, :, :, 1, :], AF.Exp, scale=-1.0)
            k.act(l, e1, AF.Ln, bias=one_c)
            k.mm(pgf, self.maskF, l[:, 0])
            k.mm(pgb, self.maskB, l[:, 1])
            k.mm(ptot, self.ones, l)
            k.copy(cc[:, 0], pgf, eng="act")
            k.copy(cc[:, 1], pgb, eng="act")
            k.copy(ct, ptot, eng="act")
            v = lambda X: X.rearrange("p c (d h) -> p d c h", d=2)
            k.act(v(EB), cc, AF.Exp, scale=-1.0)
            k.tt(t1, cc, gv[:, :, :, 0, :], ALU.add)
            k.act(v(WW), t1, AF.Exp)
            k.tt(t2, t1, ct, ALU.subtract)
            k.act(v(UU), t2, AF.Exp)
            k.act(v(ET), ct, AF.Exp, scale=-1.0)
            k.barrier()
        Cst = [sb([128, 129], F32, "Cst") for _ in range(8)]
        Cbf = [sb([128, 129], BF16, "Cbf") for _ in range(8)]
        for hd in range(8):
            k.memset(Cst[hd], 0.0)
            k.memset(Cbf[hd], 0.0, eng="pool")
        qTs = [[sb([128, 4, 128], BF16, "qT") for _ in range(2)] for _ in range(2)]
        kTs = [[sb([128, 4, 128], BF16, "kT") for _ in range(2)] for _ in range(2)]
        vaug = [[sb([128, 4, 129], BF16, "vaug") for _ in range(2)] for _ in range(2)]
        for vv in vaug:
            for v_ in vv:
                k.memset(v_[:, :, 128:129], 1.0)
        ktoks = [sb([128, 128], BF16, "ktok") for _ in range(3)]
        STs = [sb([128, 128], BF16, "ST") for _ in range(3)]
        uvs = [sb([128, 129], BF16, "uv") for _ in range(3)]
        sms = [sb([128, 6, 2, 2], F32, "sm") for _ in range(2)]
        sq = sb([128, 512], F32, "sq")
        tmpm = sb([128, 512], F32, "tmpm")
        mot = sb([128, 512], F32, "mot")
        mtok = sb([128, 512], BF16, "mtok")
        mixTm = sb([128, 4, 128], BF16, "mixTm")
        hst = sb([128, 12], F32, "hst")
        mnw = sb([128, 4], F32, "mnw")
        k.dma(mnw, self.mnw[layer])
        biasA = sb([128, 8, 640], BF16, "biasA")
        biasS = sb([128, 8, 640], BF16, "biasS")
        k.dma(biasA, self.nabias[layer, 2].rearrange("h q n -> q h n"), q="pool")
        nqv = self.nqT_d.rearrange("t (two p) n -> p (t two) n", p=64)
        nkv = self.nkT_d.rearrange("t (two p) n -> p (t two) n", p=64)
        kctx = sb([64, 8, 256], BF16, "kctx")
        k.dma(kctx, nkv[:, :, L:NT])
        vctx = sb([128, 2, 8, 65], BF16, "vctx")
        k.memset(vctx[:, :, :, 64:65], 1.0)
        for cc_ in range(2):
            k.dma(vctx[:, cc_, :, 0:64],
                  self.nv_d[L + cc_ * 128:L + (cc_ + 1) * 128, :].rearrange("p (h e) -> p h e", h=8))
        qs = [sb([64, 8, 128], BF16, "naq") for _ in range(2)]
        kws = [sb([64, 8, 640], BF16, "nak") for _ in range(2)]
        vws = [sb([128, 5, 8, 65], BF16, "nav") for _ in range(2)]
        for v_ in vws:
            k.memset(v_[:, :, :, 64:65], 1.0)
        PTs = [sb([128, 7, 128], BF16, "PT") for _ in range(2)]
        ots = [sb([128, 512], BF16, "otok") for _ in range(2)]
        rcs = [sb([128, 8], F32, "rc") for _ in range(2)]
        mxs = [sb([128, 512], BF16, "mx") for _ in range(2)]
        naS = k.ps(es, [128, 4, 128], F32, "naS")
        naO = k.ps(es, [128, 512], F32, "naO")
        trb = k.ps(es, [128, 1024], BF16, "trb")
        pk = k.ps(es, [128, 1024], BF16, "pk")
        pS = k.ps(es, [128, 512], F32, "pS")
        pNt = k.ps(es, [128, 2, 512], F32, "pN")
        pCb = k.ps(es, [128, 512], F32, "pCb")

        order = {0: [32, 33] + list(range(32)), 1: [33, 32] + list(range(31, -1, -1))}
        masks = (self.maskF, self.maskB)
        mitems = [(s_, d, h) for s_ in range(NTL) for d in (0, 1) for h in range(4)]
        seen = set()

        def mbufs(s_, d):
            return qTs[d][s_ % 2], kTs[d][s_ % 2], vaug[d][s_ % 2]

        def ml_A(n):
            s_, d, h = mitems[n]
            c = order[d][s_]
            qT, kT, va = mbufs(s_, d)
            if h == 0:
                rows = slice(c * 128, (c + 1) * 128)
                k.dma(qT, self.qT_d[:, :, rows].rearrange("h p n -> p h n"))
                k.dma(kT, self.kT_d[:, :, rows].rearrange("h p n -> p h n"))
                k.dma(va[:, :, 0:128], self.mv_d[rows, :].rearrange("p (h e) -> p h e", h=4))
            k.tr(pk[:, 0:128], kT[:, h, :], self.identb)
            k.mm(pS[:, 0:128], kT[:, h, :], qT[:, h, :])

        def ml_B(n):
            s_, d, h = mitems[n]
            c = order[d][s_]
            hd = d * 4 + h
            qT, kT, va = mbufs(s_, d)
            ktok, ST, uv = ktoks[n % 3], STs[n % 3], uvs[n % 3]
            k.copy(ktok, pk[:, 0:128], eng="act")
            k.stt(ST, pS[:, 0:128], WW[:, c, hd:hd + 1], masks[d], ALU.mult, ALU.mult)
            k.act(uv, va[:, h, :], AF.Copy, scale=UU[:, c, hd:hd + 1])

        def ml_C(n):
            s_, d, h = mitems[n]
            c = order[d][s_]
            hd = d * 4 + h
            qT, kT, va = mbufs(s_, d)
            ktok, ST, uv = ktoks[n % 3], STs[n % 3], uvs[n % 3]
            pN = pNt[:, h // 2, (h % 2) * 129:(h % 2) * 129 + 129]
            pC = pCb[:, 0:129]
            k.mm(pN, ST, va[:, h, :], start=True, stop=False)
            k.mm(pN, qT[:, h, :], Cbf[hd], start=False, stop=True)
            k.mm(pC, ktok, uv)
            k.stt(Cst[hd], Cst[hd], ET[:, c, hd:hd + 1], pC, ALU.mult, ALU.add)
            k.copy(Cbf[hd], Cst[hd], eng="pool")

        def ml_E(s_, d):
            c = order[d][s_]
            sm = sms[d]
            eb4 = EB[:, c, d * 4:(d + 1) * 4].rearrange("p (b h) -> p b h", b=2)
            for b in range(2):
                qn2 = pNt[:, b, 0:258].rearrange("p (h e) -> p h e", e=129)[:, :, 128]
                k.tt(sm[:, 0, b, :], qn2, eb4[:, b, :], ALU.mult)
            k.ts(sm[:, 1], sm[:, 0], -1.0, ALU.mult)
            k.tt(sm[:, 2], sm[:, 0], sm[:, 1], ALU.max)
            k.ts(sm[:, 3], sm[:, 2], 1.0, ALU.max)
            k.recip(sm[:, 4], sm[:, 3])
            k.tt(sm[:, 5], sm[:, 4], eb4, ALU.mult)
            for h in range(4):
                sc = sm[:, 5, h // 2, h % 2:h % 2 + 1]
                num = pNt[:, h // 2, (h % 2) * 129:(h % 2) * 129 + 128]
                dst = (hsum[:, c, h * 128:(h + 1) * 128], c)
                if c not in seen:
                    k.ts(dst, num, sc, ALU.mult)
                else:
                    k.stt(dst, num, sc, dst, ALU.mult, ALU.add)
            if c in seen:
                return c
            seen.add(c)
            return None

        def ml_fin(c):
            rows = slice(c * 128, (c + 1) * 128)
            hs = (hsum[:, c, :], c)
            k.act(sq, hs, AF.Square)
            k.reduce(hst[:, 0:4], sq.rearrange("p (h e) -> p h e", h=4), ALU.add)
            k.act(hst[:, 4:8], hst[:, 0:4], AF.Sqrt, bias=self.eps_t, scale=1.0 / 128)
            k.recip(hst[:, 8:12], hst[:, 4:8])
            k.dma(mot, self.mo_d[rows, :])
            k.tt(tmpm, hs, mot, ALU.mult)
            for h in range(4):
                k.ts(mtok[:, h * 128:(h + 1) * 128], tmpm[:, h * 128:(h + 1) * 128], hst[:, 8 + h:9 + h], ALU.mult)
            for h in range(4):
                k.tr(trb[:, h * 128:(h + 1) * 128], mtok[:, h * 128:(h + 1) * 128], self.identb)
            for h in range(4):
                k.act(mixTm[:, h, :], trb[:, h * 128:(h + 1) * 128], AF.Copy, scale=mnw[:, h:h + 1])
            k.dma(self.mixT_d[0:4, :, rows].rearrange("c p n -> p c n"), mixTm)

        types = {0: 0, 1: 1, 30: 3, 31: 4}
        blocks = [(j, True) for j in range(32)] + ([] if last else [(0, False), (1, False)])
        nitems = [(bi, hh) for bi in range(len(blocks)) for hh in range(8)]
        binfo = {}

        def na_prologue(bi):
            j, lat = blocks[bi]
            q, kw, vw = qs[bi % 2], kws[bi % 2], vws[bi % 2]
            bias = None
            if lat:
                tok0 = j * 128
                cs = min(max(j - 2, 0), 27)
                nwin = 5
                if j in types:
                    k.dma(biasS, self.nabias[layer, types[j]].rearrange("h q n -> q h n"), q="pool")
                    bias = biasS
                else:
                    bias = biasA
                k.dma(kw, nkv[:, :, cs * 128:cs * 128 + 640])
                for c in range(5):
                    k.dma(vw[:, c, :, 0:64],
                          self.nv_d[(cs + c) * 128:(cs + c + 1) * 128, :].rearrange("p (h e) -> p h e", h=8))
            else:
                tok0 = L + j * 128
                nwin = 0
            k.dma(q, nqv[:, :, tok0:tok0 + 128])
            binfo[bi] = (tok0, nwin, bias)

        def na_S(m, lo, hi):
            bi, hh = nitems[m]
            tok0, nwin, bias = binfo[bi]
            q, kw = qs[bi % 2], kws[bi % 2]
            for c in range(lo, hi):
                o = naS[:, c - lo, :]
                if c < nwin:
                    k.mm(o, kw[:, hh, c * 128:(c + 1) * 128], q[:, hh, :], start=True, stop=False)
                    k.mm(o, bias[:, hh, c * 128:(c + 1) * 128], self.identb, start=False, stop=True)
                else:
                    cc_ = c - nwin
                    k.mm(o, kctx[:, hh, cc_ * 128:(cc_ + 1) * 128], q[:, hh, :])

        def na_exp(m, lo, hi):
            PT = PTs[m % 2]
            k.act(PT[:, lo:hi, :], naS[:, 0:hi - lo, :], AF.Exp)

        def na_PV(m):
            bi, hh = nitems[m]
            tok0, nwin, bias = binfo[bi]
            vw, ot, rc = vws[bi % 2], ots[bi % 2], rcs[bi % 2]
            PT, pO = PTs[m % 2], naO[:, 0:65]
            nch = nwin + 2
            for c in range(nch):
                rhs = vw[:, c, hh, :] if c < nwin else vctx[:, c - nwin, hh, :]
                k.mm(pO, PT[:, c, :], rhs, start=(c == 0), stop=(c == nch - 1))
            k.recip(rc[:, hh:hh + 1], pO[:, 64:65])
            k.ts(ot[:, hh * 64:(hh + 1) * 64], pO[:, 0:64], rc[:, hh:hh + 1], ALU.mult)

        def na_epi(bi):
            tok0 = binfo[bi][0]
            ot, mx = ots[bi % 2], mxs[bi % 2]
            for t4 in range(4):
                k.tr(trb[:, 512 + t4 * 128:512 + (t4 + 1) * 128], ot[:, t4 * 128:(t4 + 1) * 128], self.identb)
            k.copy(mx, trb[:, 512:1024], eng="act")
            k.dma(self.mixT_d[4:8, :, tok0:tok0 + 128].rearrange("c p n -> p c n"),
                  mx.rearrange("p (c n) -> p c n", c=4))

        npair = max(len(mitems), len(nitems))
        pend_fin = []
        pend_epi = None
        for n in range(npair):
            has_m = n < len(mitems)
            has_n = n < len(nitems)
            if has_n:
                bi, hh = nitems[n]
                if hh == 0:
                    na_prologue(bi)
                nch = binfo[bi][1] + 2
                n1 = min(4, nch)
                na_S(n, 0, n1)
            if has_m:
                ml_A(n)
            if has_n:
                na_exp(n, 0, n1)
            if has_m:
                ml_B(n)
            if has_n and nch > 4:
                na_S(n, 4, nch)
            if has_m:
                ml_C(n)
            if has_n:
                if nch > 4:
                    na_exp(n, 4, nch)
                if pend_epi is not None:
                    na_epi(pend_epi)
                    pend_epi = None
                na_PV(n)
                if hh == 7:
                    pend_epi = bi
            for c in pend_fin:
                ml_fin(c)
            pend_fin = []
            if has_m and mitems[n][2] == 3:
                done = ml_E(mitems[n][0], mitems[n][1])
                if done is not None:
                    pend_fin.append(done)
        if pend_epi is not None:
            na_epi(pend_epi)
        for c in pend_fin:
            ml_fin(c)
        if self.stopped(layer, "D2"):
            k.dma(self.hs_dbg, hsum)
        k.barrier()


Prog.phase_mix = _phase_mix
```

```python
import numpy as np
from contextlib import ExitStack
import concourse.bass as bass
import concourse.mybir as mybir
from concourse.bass_utils import run_bass_kernel_spmd

F32 = mybir.dt.float32
BF16 = mybir.dt.bfloat16
I32 = mybir.dt.int32
U32 = mybir.dt.uint32
AF = mybir.ActivationFunctionType
ALU = mybir.AluOpType
AX = mybir.AxisListType

SEM_LIMIT = 30000
SAME_ENGINE_SYNC = True
MERGED_MIXER = False
NDMA = 24


class _U:
    __slots__ = ("w", "r")

    def __init__(self):
        self.w = {}
        self.r = {}


class KB:
    def __init__(self, nc, same_engine_sync=True):
        self.nc = nc
        self.E = {"pe": nc.tensor, "act": nc.scalar, "dve": nc.vector, "pool": nc.gpsimd, "sp": nc.sync}
        self.sems = []
        self.cur = {}
        for e in ("pe", "act", "dve", "pool"):
            self.cur[e] = [self._newsem(), 0]
        self.dsem = {q: [self._newsem() for _ in range(NDMA)] for q in ("sp", "pool")}
        self.dval = {q: [0] * NDMA for q in ("sp", "pool")}
        self.di = {"sp": 0, "pool": 0}
        self.waited = {e: {} for e in self.E}
        self.track = {}
        self.ses = same_engine_sync
        self.old = []
        self.n_ins = 0
        self.n_wait = 0
        self.out_events = []
        self._uid = 0

    def _newsem(self):
        h = self.nc.alloc_semaphore(f"ks{len(self.sems)}")
        self.sems.append(h)
        return len(self.sems) - 1

    def sb(self, es, shape, dt=F32, name=None):
        self._uid += 1
        return es.enter_context(self.nc.sbuf_tensor(f"{name or 'sb'}_{self._uid}", list(shape), dt)).ap()

    def ps(self, es, shape, dt=F32, name=None):
        self._uid += 1
        esz = 4 if dt in (F32, I32, U32) else 2
        free = 1
        for d_ in shape[1:]:
            free *= d_
        per_bank = 2048 // esz
        nb = (free + per_bank - 1) // per_bank
        flat = es.enter_context(self.nc.psum_tensor(f"{name or 'ps'}_{self._uid}", [128, nb * per_bank], dt)).ap()
        v = flat[0:shape[0], 0:free]
        nd = len(shape) - 1
        if nd == 1:
            return v
        names = "abcd"[:nd]
        pat = f"p ({' '.join(names)}) -> p {' '.join(names)}"
        return v.rearrange(pat, **{names[i]: shape[1 + i] for i in range(1, nd)})

    def dram(self, name, shape, dt=F32, kind="Internal"):
        return self.nc.dram_tensor(name, list(shape), dt, kind=kind).ap()

    @staticmethod
    def _split(x):
        if isinstance(x, tuple):
            return x[0], x[1]
        return x, None

    def _units(self, x):
        ap, key = self._split(x)
        name = ap.tensor.name
        d = self.track.get(name)
        if d is None:
            d = {None: _U()}
            self.track[name] = d
        if key is None:
            return list(d.values())
        u = d.get(key)
        if u is None:
            u = _U()
            d[key] = u
        return [u, d[None]]

    def _wait(self, eng, sid, val, owner):
        if owner == eng and (eng == "pe" or not self.ses):
            return
        if self.waited[eng].get(sid, -1) >= val:
            return
        self.E[eng].wait_ge(self.sems[sid], val)
        self.waited[eng][sid] = val
        self.n_wait += 1

    def _pre(self, eng, outs, ins):
        for x in ins:
            for u in self._units(x):
                for sid, (val, owner) in u.w.items():
                    self._wait(eng, sid, val, owner)
        for x in outs:
            for u in self._units(x):
                for sid, (val, owner) in u.w.items():
                    self._wait(eng, sid, val, owner)
                for sid, (val, owner) in u.r.items():
                    self._wait(eng, sid, val, owner)

    def _post(self, sid, val, owner, outs, ins):
        for x in ins:
            ap, key = self._split(x)
            us = self._units(x)
            if key is not None:
                us = us[:1]
            for u in us:
                u.r[sid] = (val, owner)
        for x in outs:
            ap, key = self._split(x)
            us = self._units(x)
            if key is not None:
                us = us[:1]
            for u in us:
                u.w = {sid: (val, owner)}
                u.r = {}

    def op(self, eng, fn, outs, ins):
        self._pre(eng, outs, ins)
        c = self.cur[eng]
        if c[1] >= SEM_LIMIT:
            self.old.append((c[0], c[1]))
            c[0] = self._newsem()
            c[1] = 0
        ins_obj = fn()
        c[1] += 1
        ins_obj.then_inc(self.sems[c[0]], 1)
        self._post(c[0], c[1], eng, outs, ins)
        self.n_ins += 1

    def dma(self, out, in_, q="sp", is_output=False, **kw):
        eng = q
        self._pre(eng, [out], [in_])
        i = self.di[q]
        self.di[q] = (i + 1) % NDMA
        sid = self.dsem[q][i]
        dval = self.dval[q]
        if dval[i] > 0:
            self._wait(eng, sid, dval[i], "dma")
        dval[i] += 16
        o, _ = self._split(out)
        s, _ = self._split(in_)
        self.E[eng].dma_start(out=o, in_=s, **kw).then_inc(self.sems[sid], 16)
        self._post(sid, dval[i], "dma", [out], [in_])
        if is_output:
            self.out_events.append((sid, dval[i]))
        self.n_ins += 1

    def barrier(self):
        for eng in ("pe", "act", "dve", "pool", "sp"):
            for sid, val in self.old:
                self._wait(eng, sid, val, "barrier")
            for e, c in self.cur.items():
                if c[1] > 0:
                    self._wait(eng, c[0], c[1], "barrier")
            for q in ("sp", "pool"):
                for i in range(NDMA):
                    if self.dval[q][i] > 0:
                        self._wait(eng, self.dsem[q][i], self.dval[q][i], "dma")

    def finish(self):
        self.barrier()
        for sid, val in self.out_events:
            self._wait("sp", sid, val, "dma")
        for e, c in self.cur.items():
            if c[1] > 0:
                self._wait("sp", c[0], c[1], e)

    @staticmethod
    def _a(x):
        return x[0] if isinstance(x, tuple) else x

    def mm(self, out, lhsT, rhs, start=True, stop=True):
        a = self._a
        self.op("pe", lambda: self.nc.tensor.matmul(a(out), a(lhsT), a(rhs), start=start, stop=stop),
                [out], [lhsT, rhs])

    def tr(self, out, in_, ident):
        a = self._a
        self.op("pe", lambda: self.nc.tensor.transpose(a(out), a(in_), a(ident)), [out], [in_, ident])

    def act(self, out, in_, func, bias=None, scale=None, eng="act"):
        a = self._a
        kw = {}
        ins = [in_]
        if bias is not None:
            kw["bias"] = a(bias) if not isinstance(bias, (int, float)) else bias
            if not isinstance(bias, (int, float)):
                ins.append(bias)
        if scale is not None:
            kw["scale"] = a(scale) if not isinstance(scale, (int, float)) else scale
            if not isinstance(scale, (int, float)):
                ins.append(scale)
        self.op("act", lambda: self.nc.scalar.activation(out=a(out), in_=a(in_), func=func, **kw), [out], ins)

    def tt(self, out, in0, in1, op, eng="dve"):
        a = self._a
        self.op(eng, lambda: self.E[eng].tensor_tensor(out=a(out), in0=a(in0), in1=a(in1), op=op), [out], [in0, in1])

    def ts(self, out, in0, s1, op0, s2=None, op1=None, eng="dve"):
        a = self._a
        ins = [in0]
        v1 = s1
        v2 = s2
        if not isinstance(s1, (int, float)):
            ins.append(s1)
            v1 = a(s1)
        if s2 is not None and not isinstance(s2, (int, float)):
            ins.append(s2)
            v2 = a(s2)
        kw = {}
        if op1 is not None:
            kw["op1"] = op1
        self.op(eng, lambda: self.E[eng].tensor_scalar(out=a(out), in0=a(in0), scalar1=v1, scalar2=v2, op0=op0, **kw),
                [out], ins)

    def stt(self, out, in0, scalar, in1, op0, op1):
        a = self._a
        ins = [in0, in1]
        v = scalar
        if not isinstance(scalar, (int, float)):
            ins.append(scalar)
            v = a(scalar)
        self.op("dve", lambda: self.nc.vector.scalar_tensor_tensor(out=a(out), in0=a(in0), scalar=v, in1=a(in1),
                                                                    op0=op0, op1=op1), [out], ins)

    def copy(self, out, in_, eng="dve"):
        a = self._a
        if eng == "act":
            self.op("act", lambda: self.nc.scalar.copy(out=a(out), in_=a(in_)), [out], [in_])
        else:
            self.op(eng, lambda: self.E[eng].tensor_copy(out=a(out), in_=a(in_)), [out], [in_])

    def recip(self, out, in_):
        a = self._a
        self.op("dve", lambda: self.nc.vector.reciprocal(out=a(out), in_=a(in_)), [out], [in_])

    def reduce(self, out, in_, op, axis=AX.X):
        a = self._a
        self.op("dve", lambda: self.nc.vector.tensor_reduce(out=a(out), in_=a(in_), axis=axis, op=op), [out], [in_])

    def memset(self, ap, val, eng="dve"):
        a = self._a
        self.op(eng, lambda: self.E[eng].memset(a(ap), val), [ap], [])


D = 1024
L = 4096
NCTX = 256
NT = L + NCTX
NTL = 34
N_IN = 3600
EPS = 1e-6
NE = 16
DFF = 512
C_MQ, C_MO, C_NQ, C_MK, C_MV, C_G, C_NK, C_NV = 0, 512, 1024, 1536, 2048, 2560, 2576, 3088
NEG = -30000.0


def _host_consts():
    f32 = np.float32
    c = {}
    c["ident"] = np.eye(128, dtype=f32)
    s = np.arange(128)
    c["maskF"] = (s[:, None] <= s[None, :]).astype(f32)
    c["maskB"] = (s[:, None] >= s[None, :]).astype(f32)
    c["ones"] = np.ones((128, 128), f32)
    sel = np.zeros((2, 2, 128), f32)
    sel[0, 0, :] = 1.0
    sel[1, 1, :] = 1.0
    c["sel2"] = sel
    t = np.arange(L)
    row = (t // 64).astype(np.float64)
    col = (t % 64).astype(np.float64)
    inv = 10000.0 ** (-np.arange(0, 64, 2, dtype=np.float64) / 64)
    ang = np.zeros((128, L))
    sgn = np.zeros((128, 1))
    for d in range(128):
        i = d % 32
        ang[d] = (row if d < 64 else col) * inv[i]
        sgn[d] = -1.0 if (d % 64) < 32 else 1.0
    cos = np.cos(ang.astype(f32).astype(np.float64))
    sin = np.sin(ang.astype(f32).astype(np.float64)) * sgn
    ks = 128.0 ** -0.5
    c["rope"] = np.stack([cos, sin, cos * ks, sin * ks]).astype(f32)
    perm = np.zeros((128, 128), f32)
    for dp in range(128):
        d = dp + 32 if (dp % 64) < 32 else dp - 32
        perm[d, dp] = 1.0
    c["perm"] = perm
    return c


def _na_index_tables():
    types = [0, 1, 2, 30, 31]
    idx_r = np.zeros((5, 128, 640), np.int64)
    idx_c = np.zeros((5, 128, 640), np.int64)
    valid = np.zeros((5, 128, 640), bool)
    for ti, j in enumerate(types):
        cs = min(max(j - 2, 0), 27)
        for q in range(128):
            r = 2 * j + q // 64
            cq = q % 64
            rs = min(max(r - 4, 0), 56)
            c0 = min(max(cq - 8, 0), 48)
            for key in range(640):
                kr = 2 * cs + key // 64
                kc = key % 64
                if rs <= kr < rs + 8 and c0 <= kc < c0 + 16:
                    valid[ti, q, key] = True
                    idx_r[ti, q, key] = kr - r + 7
                    idx_c[ti, q, key] = kc - cq + 15
    return idx_r, idx_c, valid


_NA_IDX = None


def _na_bias_tables(rpb):
    global _NA_IDX
    if _NA_IDX is None:
        _NA_IDX = _na_index_tables()
    idx_r, idx_c, valid = _NA_IDX
    g = rpb[:, :, idx_r, idx_c]
    g = np.where(valid[None, None], g, np.float32(NEG)).astype(np.float32)
    return np.ascontiguousarray(np.transpose(g, (0, 2, 1, 3, 4)))


class Prog:
    def __init__(self, debug=(), stop=None, nlayers=2):
        self.debug = set(debug)
        self.stop = stop
        self.nlayers = nlayers
        nc = bass.Bass("TRN2", target_bir_lowering=False)
        self.nc = nc
        self.k = KB(nc, same_engine_sync=SAME_ENGINE_SYNC)
        self.inp = {}
        self.dbg = {}

    def din(self, name, shape, dt=F32):
        ap = self.nc.dram_tensor(name, list(shape), dt, kind="ExternalInput").ap()
        self.inp[name] = ap
        return ap

    def dscr(self, name, shape, dt=F32):
        kind = "ExternalOutput" if name in self.debug else "Internal"
        ap = self.nc.dram_tensor(name, list(shape), dt, kind=kind).ap()
        if name in self.debug:
            self.dbg[name] = ap
        return ap

    def build(self):
        nc, k = self.nc, self.k
        I = self.din
        self.x_in = I("x", [L, D])
        self.ctx_in = I("ctxin", [NCTX, D])
        self.sT_in = I("sT", [128, 8, 2])
        self.ada_w = I("ada_w", [2, D, 6 * D])
        self.ada_b2 = I("ada_b2", [2, 2, 6 * D])
        self.n1w = I("n1w", [2, 128, 8])
        self.n2w = I("n2w", [2, 128, 8])
        self.w_in = I("w_in", [2, D, N_IN])
        self.convw = I("convw", [2, 3, 1024])
        self.convb = I("convb", [2, 128, 8])
        self.convwT = I("convwT", [2, 128, 8, 3])
        self.gate_b = I("gate_b", [2, 16])
        self.mnw = I("mnw", [2, 128, 4])
        self.nabias = I("nabias", [2, 5, 8, 128, 640])
        self.w_out = I("w_out", [2, D, D])
        self.rw = I("rw", [128, 8, 16])
        self.rb = I("rb", [16])
        self.w1 = I("w1", [2, NE, D, DFF])
        self.w3 = I("w3", [2, NE, D, DFF])
        self.w2 = I("w2", [2, NE, DFF, D])
        self.fnw = I("fnw", [D])
        self.c_ident = I("ident", [128, 128])
        self.c_maskF = I("maskF", [128, 128])
        self.c_maskB = I("maskB", [128, 128])
        self.c_ones = I("ones", [128, 128])
        self.c_sel2 = I("sel2", [2, 2, 128])
        self.c_rope = I("rope", [4, 128, L])
        self.c_perm = I("perm", [128, 128])
        self.out = nc.dram_tensor("out", [L, D], F32, kind="ExternalOutput").ap()
        S = self.dscr
        self.xres = S("xres", [NT, D])
        self.qT_d = S("qT_d", [4, 128, NT], BF16)
        self.kT_d = S("kT_d", [4, 128, NT], BF16)
        self.nqT_d = S("nqT_d", [4, 128, NT], BF16)
        self.nkT_d = S("nkT_d", [4, 128, NT], BF16)
        self.mo_d = S("mo_d", [NT, 512])
        self.mv_d = S("mv_d", [NT, 512], BF16)
        self.g_d = S("g_d", [NT, 16])
        self.nv_d = S("nv_d", [NT, 512], BF16)
        self.mixT_d = S("mixT_d", [8, 128, NT], BF16)
        self.hT_dbg = S("hT_dbg", [128, 8, NT + 4], BF16)
        self.mod_dbg = S("mod_dbg", [2, 6 * D])
        self.gate_dbg = S("gate_dbg", [128, NTL, 16])
        self.hs_dbg = S("hs_dbg", [128, NTL, 512])

        with ExitStack() as es:
            self.es = es
            sb = lambda shape, dt=F32, name=None: k.sb(es, shape, dt, name)
            self.ident = sb([128, 128], F32, "ident")
            self.identb = sb([128, 128], BF16, "identb")
            self.maskF = sb([128, 128], F32, "maskF")
            self.maskB = sb([128, 128], F32, "maskB")
            self.ones = sb([128, 128], F32, "ones")
            self.permb = sb([128, 128], BF16, "permb")
            self.sel2 = sb([2, 2, 128], F32, "sel2")
            k.dma(self.ident, self.c_ident)
            k.dma(self.identb, self.c_ident, q="pool")
            k.dma(self.maskF, self.c_maskF)
            k.dma(self.maskB, self.c_maskB)
            k.dma(self.ones, self.c_ones)
            k.dma(self.permb, self.c_perm, q="pool")
            k.dma(self.sel2, self.c_sel2.rearrange("w t p -> t w p"))
            self.rw_sb = sb([128, 8, 16], F32, "rw")
            k.dma(self.rw_sb, self.rw)
            self.rb_bc = sb([128, 16], F32, "rb")
            k.dma(self.rb_bc, self.rb.partition_broadcast(128))
            self.eps_t = sb([128, 1], F32, "eps")
            k.memset(self.eps_t, EPS)
            self.silu_s = sb([128, 8, 2], F32, "silu_s")
            tmp = sb([128, 8, 2], F32, "sT")
            k.dma(tmp, self.sT_in)
            k.act(self.silu_s, tmp, AF.Silu)
            for layer in range(self.nlayers):
                self.layer(layer)
                if self.stop is not None and self.stop[0] == layer:
                    break
            k.finish()
        return nc

    def xsrc(self, layer, i):
        if layer == 0:
            return self.x_in[i * 128:(i + 1) * 128, :] if i < 32 else self.ctx_in[(i - 32) * 128:(i - 31) * 128, :]
        return self.xres[i * 128:(i + 1) * 128, :]

    def alloc_hT(self, es):
        k = self.k
        self.hT = k.sb(es, [128, 8, NT + 4], BF16, "hT")
        for j in range(8):
            for c0 in (0, L + 1, L + 2, NT + 3):
                k.memset(self.hT[:, j, c0:c0 + 1], 0.0, eng="pool")

    def hcol(self, tile_i):
        return 1 + tile_i * 128 if tile_i < 32 else L + 3 + (tile_i - 32) * 128

    def stopped(self, layer, phase):
        return self.stop is not None and self.stop == (layer, phase)


def _layer(self, layer):
    k = self.k
    last = layer == 1
    with ExitStack() as les:
        sbl = lambda shape, dt=F32, name=None: k.sb(les, shape, dt, name)
        self.modcol = sbl([128, 4, 8, 2], F32, "modcol")
        self.ws1 = sbl([128, 8, 2], F32, "ws1")
        self.ws2 = sbl([128, 8, 2], F32, "ws2")
        self.bc = {(v, w): sbl([128, D], F32, f"bc{v}{w}") for v in (2, 5) for w in (0, 1)}
        self.gate_sb = sbl([128, NTL, 16], F32, "gate")
        self.adaln(layer)
        if self.stopped(layer, "A"):
            return
        with ExitStack() as hs:
            self.alloc_hT(hs)
            self.phase_norm1(layer)
            if self.stopped(layer, "B"):
                k.dma(self.hT_dbg, self.hT)
                k.barrier()
                return
            self.phase_proj(layer)
            k.barrier()
        if self.stopped(layer, "C"):
            return
        if MERGED_MIXER:
            self.phase_mix(layer)
        else:
            self.phase_mlstm(layer)
            if self.stopped(layer, "D1"):
                return
            self.phase_na(layer)
        if self.stopped(layer, "D2"):
            return
        with ExitStack() as hs:
            self.alloc_hT(hs)
            self.phase_wout_norm2(layer)
            if self.stopped(layer, "E"):
                k.dma(self.hT_dbg, self.hT)
                k.dma(self.gate_dbg, self.gate_sb)
                k.barrier()
                return
            self.phase_moe(layer)
            k.barrier()


def _adaln(self, layer):
    k = self.k
    with ExitStack() as es:
        sb = lambda shape, dt=F32, name=None: k.sb(es, shape, dt, name)
        wbuf = [sb([128, 8, 512], F32, "adaw") for _ in range(2)]
        self.modrow = sb([2, 6 * D], F32, "modrow")
        adab = sb([2, 6 * D], F32, "adab")
        k.dma(adab, self.ada_b2[layer])
        pss = [k.ps(es, [2, 512], F32, "adaps") for _ in range(2)]
        wv = self.ada_w[layer].rearrange("(j p) n -> p j n", p=128)
        for n in range(12):
            w = wbuf[n % 2]
            k.dma(w, wv[:, :, n * 512:(n + 1) * 512])
            p = pss[n % 2]
            for j in range(8):
                k.mm(p, self.silu_s[:, j, :], w[:, j, :], start=(j == 0), stop=(j == 7))
            k.tt(self.modrow[:, n * 512:(n + 1) * 512], p, adab[:, n * 512:(n + 1) * 512], ALU.add)
        pcol = k.ps(es, [128, 4, 8, 2], F32, "pcol")
        for vi, v in enumerate((0, 1, 3, 4)):
            for j in range(8):
                k.tr(pcol[:, vi, j, :], self.modrow[:, v * D + j * 128: v * D + (j + 1) * 128], self.ident[0:2, 0:2])
        k.copy(self.modcol, pcol)
        nw1 = sb([128, 8], F32, "nw1")
        nw2 = sb([128, 8], F32, "nw2")
        k.dma(nw1, self.n1w[layer])
        k.dma(nw2, self.n2w[layer])
        for w in range(2):
            k.ts(self.ws1[:, :, w], self.modcol[:, 1, :, w], 1.0, ALU.add)
            k.tt(self.ws1[:, :, w], self.ws1[:, :, w], nw1, ALU.mult)
            k.ts(self.ws2[:, :, w], self.modcol[:, 3, :, w], 1.0, ALU.add)
            k.tt(self.ws2[:, :, w], self.ws2[:, :, w], nw2, ALU.mult)
        self.sh1 = self.modcol[:, 0]
        self.sh2 = self.modcol[:, 2]
        pb = [k.ps(es, [128, 512], F32, "pbc") for _ in range(2)]
        n = 0
        for v in (2, 5):
            for w in range(2):
                for half in range(2):
                    p = pb[n % 2]
                    n += 1
                    k.mm(p, self.sel2[:, w, :], self.modrow[:, v * D + half * 512: v * D + (half + 1) * 512])
                    k.copy(self.bc[(v, w)][:, half * 512:(half + 1) * 512], p, eng="act")
        if self.stopped(layer, "A"):
            k.dma(self.mod_dbg, self.modrow)
        k.barrier()


def _norm_stats(self, xt, bufs):
    k = self.k
    junk, xn, st = bufs
    k.act(junk, xt, AF.Square)
    k.reduce(st[:, 0:1], junk, ALU.add)
    k.act(st[:, 1:2], st[:, 0:1], AF.Sqrt, bias=self.eps_t, scale=1.0 / D)
    k.recip(st[:, 2:3], st[:, 1:2])
    k.ts(xn, xt, st[:, 2:3], ALU.mult)


def _norm_tr(self, i, xn, pt, ws, sh, w, r32=None):
    k = self.k
    for j in range(8):
        k.tr(pt[:, j, :], xn[:, j * 128:(j + 1) * 128], self.ident)
    c0 = self.hcol(i)
    for j in range(8):
        dst = (self.hT[:, j, c0:c0 + 128], (i, j))
        if r32 is not None:
            k.act(r32[:, j, :], pt[:, j, :], AF.Identity, bias=sh[:, j, w:w + 1], scale=ws[:, j, w:w + 1])
            k.copy(dst, r32[:, j, :], eng="dve")
        elif i % 2 == 0:
            k.act(dst, pt[:, j, :], AF.Identity, bias=sh[:, j, w:w + 1], scale=ws[:, j, w:w + 1])
        else:
            k.ts(dst, pt[:, j, :], ws[:, j, w:w + 1], ALU.mult, sh[:, j, w:w + 1], ALU.add)


def _phase_norm1(self, layer):
    k = self.k
    with ExitStack() as es:
        sb = lambda shape, dt=F32, name=None: k.sb(es, shape, dt, name)
        xts = [sb([128, D], F32, "xt") for _ in range(3)]
        nb = [(sb([128, D], F32, "junk"), sb([128, D], F32, "xn"), sb([128, 4], F32, "st")) for _ in range(3)]
        pts = [k.ps(es, [128, 8, 128], F32, "ptr") for _ in range(2)]

        def sa(i):
            k.dma(xts[i % 3], self.xsrc(layer, i))
            self.norm_stats(xts[i % 3], nb[i % 3])

        sa(0)
        for i in range(NTL):
            if i + 1 < NTL:
                sa(i + 1)
            self.norm_tr(i, nb[i % 3][1], pts[i % 2], self.ws1, self.sh1, 0 if i < 32 else 1)
        k.barrier()


Prog.layer = _layer
Prog.adaln = _adaln
Prog.norm_stats = _norm_stats
Prog.norm_tr = _norm_tr
Prog.phase_norm1 = _phase_norm1


def _col(v, n):
    return np.ascontiguousarray(np.asarray(v, np.float32).reshape(n, 128).T)


def make_in_maps(inputs):
    f32 = np.float32
    g = {k_: np.asarray(v) for k_, v in inputs.items()}
    consts = _host_consts()
    shared = dict(consts)
    shared["ada_w"] = np.ascontiguousarray(g["ada_w"], f32)
    shared["ada_b2"] = np.ascontiguousarray(np.repeat(g["ada_b"][:, None, :], 2, axis=1), f32)
    shared["n1w"] = np.stack([_col(g["norm1_w"][l], 8) for l in range(2)])
    shared["n2w"] = np.stack([_col(g["norm2_w"][l], 8) for l in range(2)])
    shared["w_in"] = np.ascontiguousarray(g["w_in"], f32)
    shared["convw"] = np.ascontiguousarray(g["conv_w"], f32)
    shared["convb"] = np.stack([_col(g["conv_b"][l], 8) for l in range(2)])
    shared["convwT"] = np.ascontiguousarray(np.stack(
        [np.stack([_col(g["conv_w"][l][j], 8) for j in range(3)], axis=-1) for l in range(2)]))
    shared["gate_b"] = np.ascontiguousarray(g["gate_b"], f32)
    shared["mnw"] = np.stack([_col(g["mnorm_w"][l], 4) for l in range(2)])
    shared["nabias"] = _na_bias_tables(np.asarray(g["rpb"], f32))
    shared["w_out"] = np.ascontiguousarray(g["w_out"], f32)
    shared["rw"] = np.ascontiguousarray(np.asarray(g["router_w"], f32).reshape(8, 128, 16).transpose(1, 0, 2))
    shared["rb"] = np.ascontiguousarray(g["router_b"], f32)
    shared["w1"] = np.ascontiguousarray(g["exp_w1"], f32)
    shared["w3"] = np.ascontiguousarray(g["exp_w3"], f32)
    shared["w2"] = np.ascontiguousarray(g["exp_w2"], f32)
    shared["fnw"] = np.ascontiguousarray(g["final_norm_w"], f32)
    cc = _col(g["c_ctx"], 8)
    maps = []
    for b in range(8):
        m = dict(shared)
        m["x"] = np.ascontiguousarray(g["x"][b], f32)
        m["ctxin"] = np.ascontiguousarray(g["ctx"][b], f32)
        m["sT"] = np.ascontiguousarray(np.stack([_col(g["c"][b], 8), cc], axis=-1))
        maps.append(m)
    return maps


_PROG = None


def kernel(**inputs):
    global _PROG
    if _PROG is None:
        p = Prog()
        p.build()
        _PROG = p
    p = _PROG
    maps = make_in_maps(inputs)
    maps = [{n: m[n] for n in p.inp} for m in maps]
    res = run_bass_kernel_spmd(p.nc, maps, core_ids=list(range(8)))
    return np.stack([np.asarray(r["out"], np.float32) for r in res.results])


def _phase_proj(self, layer):
    k = self.k
    wv = self.w_in[layer].rearrange("(j p) n -> p j n", p=128)
    groups = [(g * 512, 512, 1 + g * 512, False) for g in range(8)] + [(L, 256, L + 3, True)]
    ks = 128.0 ** -0.5
    with ExitStack() as es:
        sb = lambda shape, dt=F32, name=None: k.sb(es, shape, dt, name)
        psA = [k.ps(es, [128, 512], F32, "psA") for _ in range(3)]
        psB = [k.ps(es, [128, 512], F32, "psB") for _ in range(2)]
        convb = sb([128, 8], F32, "convb")
        k.dma(convb, self.convb[layer])
        gb_bc = sb([128, 16], F32, "gb_bc")
        k.dma(gb_bc, self.gate_b[layer].partition_broadcast(128))
        with ExitStack() as es1:
            sb1 = lambda shape, dt=F32, name=None: k.sb(es1, shape, dt, name)
            cw = sb1([128, 8, 3], F32, "cw")
            k.dma(cw, self.convwT[layer])
            wqk = [sb1([128, 8, 512], BF16, "wqk") for _ in range(2)]
            ropes = [sb1([128, 2, 512], F32, "rope") for _ in range(2)]
            accs = [sb1([128, 512], F32, "acc") for _ in range(2)]
            xbs = [sb1([128, 512], BF16, "xb") for _ in range(2)]
            t1s = [sb1([128, 512], F32, "t1") for _ in range(2)]
            t2s = [sb1([128, 512], F32, "t2") for _ in range(2)]
            obs = [sb1([128, 512], BF16, "ob") for _ in range(2)]
            cgroups = [(g * 510, 510, False) for g in range(8)] + [(4080, 16, False), (0, 256, True)]
            for blk in range(2):
                k.dma(wqk[blk], wv[:, :, (C_MQ, C_MK)[blk]:(C_MQ, C_MK)[blk] + 512], q="pool")
            its = [(blk, gi, hh) for blk in range(2) for gi in range(len(cgroups)) for hh in range(4)]

            def stage_X(n):
                blk, gi, hh = its[n]
                t0, nt, isctx = cgroups[gi]
                ridx = (0, 2)[blk]
                rp = ropes[gi % 2]
                if hh == 0 and not isctx:
                    k.dma(rp[:, :, :nt], self.c_rope[ridx:ridx + 2, :, t0:t0 + nt].rearrange("c p n -> p c n"))
                hc0 = (L + 2) if isctx else t0
                p, acc, xb = psA[n % 3], accs[n % 2], xbs[n % 2]
                wb = wqk[blk]
                for dch in range(8):
                    k.mm(p[:, :nt + 2], wb[:, dch, hh * 128:(hh + 1) * 128], self.hT[:, dch, hc0:hc0 + nt + 2],
                         start=(dch == 0), stop=(dch == 7))
                ti = blk * 4 + hh
                k.ts(acc[:, :nt], p[:, 1:nt + 1], cw[:, ti, 1:2], ALU.mult, convb[:, ti:ti + 1], ALU.add)
                k.stt(acc[:, :nt], p[:, 0:nt], cw[:, ti, 0:1], acc[:, :nt], ALU.mult, ALU.add)
                k.stt(acc[:, :nt], p[:, 2:nt + 2], cw[:, ti, 2:3], acc[:, :nt], ALU.mult, ALU.add)
                k.act(xb[:, :nt], acc[:, :nt], AF.Silu)

            def stage_Y(n):
                blk, gi, hh = its[n]
                t0, nt, isctx = cgroups[gi]
                dst_d = (self.qT_d, self.kT_d)[blk]
                rp = ropes[gi % 2]
                tg0 = (L + t0) if isctx else t0
                xb, t1, t2, ob = xbs[n % 2], t1s[n % 2], t2s[n % 2], obs[n % 2]
                if not isctx:
                    p2 = psB[n % 2]
                    k.mm(p2[:, :nt], self.permb, xb[:, :nt])
                    k.tt(t1[:, :nt], p2[:, :nt], rp[:, 1, :nt], ALU.mult)
                    k.tt(t2[:, :nt], xb[:, :nt], rp[:, 0, :nt], ALU.mult)
                    k.tt(ob[:, :nt], t1[:, :nt], t2[:, :nt], ALU.add)
                    k.dma(dst_d[hh, :, tg0:tg0 + nt], ob[:, :nt])
                elif blk == 0:
                    k.dma(dst_d[hh, :, tg0:tg0 + nt], xb[:, :nt])
                else:
                    k.act(ob[:, :nt], xb[:, :nt], AF.Copy, scale=ks)
                    k.dma(dst_d[hh, :, tg0:tg0 + nt], ob[:, :nt])

            stage_X(0)
            for n in range(len(its)):
                if n + 1 < len(its):
                    stage_X(n + 1)
                stage_Y(n)
            k.barrier()
        with ExitStack() as es2:
            sb2 = lambda shape, dt=F32, name=None: k.sb(es2, shape, dt, name)
            wbs = [sb2([128, 8, 512], BF16, "wb") for _ in range(2)]
            obs = [sb2([128, 512], BF16, "ob2") for _ in range(3)]
            o32s = [sb2([128, 512], F32, "o32") for _ in range(2)]
            n = 0
            wi = 0
            for (c0, dst_d, scale) in ((C_NQ, self.nqT_d, 0.125), (C_NK, self.nkT_d, 1.0)):
                wb = wbs[wi % 2]
                wi += 1
                k.dma(wb, wv[:, :, c0:c0 + 512], q="pool")
                for (t0, nt, hc0, isctx) in groups:
                    for tt_ in range(4):
                        p = psA[n % 3]
                        ob = obs[n % 3]
                        for dch in range(8):
                            k.mm(p[:, :nt], wb[:, dch, tt_ * 128:(tt_ + 1) * 128], self.hT[:, dch, hc0:hc0 + nt],
                                 start=(dch == 0), stop=(dch == 7))
                        k.act(ob[:, :nt], p[:, :nt], AF.Copy, scale=scale)
                        k.dma(dst_d[tt_, :, t0:t0 + nt], ob[:, :nt])
                        n += 1
            for (c0, ncol, kind) in ((C_MO, 512, "mo"), (C_MV, 512, "mv"), (C_G, 16, "g"), (C_NV, 512, "nv")):
                wb = wbs[wi % 2]
                wi += 1
                k.dma(wb[:, :, :ncol], wv[:, :, c0:c0 + ncol], q="pool")
                for i in range(NTL):
                    hc = self.hcol(i)
                    p = psA[n % 3]
                    for dch in range(8):
                        k.mm(p[:, :ncol], self.hT[:, dch, hc:hc + 128], wb[:, dch, :ncol],
                             start=(dch == 0), stop=(dch == 7))
                    rows = slice(i * 128, (i + 1) * 128)
                    if kind == "mo":
                        o = o32s[n % 2]
                        k.act(o, p, AF.Sigmoid)
                        k.dma(self.mo_d[rows, :], o)
                    elif kind == "g":
                        o = o32s[n % 2]
                        k.tt(o[:, :16], p[:, :16], gb_bc, ALU.add)
                        k.dma(self.g_d[rows, :], o[:, :16])
                    else:
                        ob = obs[n % 3]
                        if n % 2 == 0:
                            k.copy(ob, p, eng="act")
                        else:
                            k.copy(ob, p, eng="dve")
                        k.dma((self.mv_d if kind == "mv" else self.nv_d)[rows, :], ob)
                    n += 1
            k.barrier()


Prog.phase_proj = _phase_proj


def _phase_mlstm(self, layer):
    k = self.k
    with ExitStack() as es:
        sb = lambda shape, dt=F32, name=None: k.sb(es, shape, dt, name)
        EB = sb([128, NTL, 8], F32, "EB")
        WW = sb([128, NTL, 8], F32, "WW")
        UU = sb([128, NTL, 8], F32, "UU")
        ET = sb([128, NTL, 8], F32, "ET")
        hsum = sb([128, NTL, 512], F32, "hsum")
        one_c = self.ones[:, 0:1]
        with ExitStack() as es1:
            sb1 = lambda shape, dt=F32, name=None: k.sb(es1, shape, dt, name)
            gall = sb1([128, NTL, 2, 2, 4], F32, "gall")
            k.dma(gall, self.g_d.rearrange("(c p) (d t h) -> p c d t h", p=128, d=2, t=2))
            gv = gall.rearrange("p c d t h -> p d c t h")
            e1 = sb1([128, 2, NTL, 4], F32, "e1")
            l = sb1([128, 2, NTL, 4], F32, "lsp")
            cc = sb1([128, 2, NTL, 4], F32, "cc")
            ct = sb1([128, 2, NTL, 4], F32, "ct")
            t1 = sb1([128, 2, NTL, 4], F32, "gt1")
            t2 = sb1([128, 2, NTL, 4], F32, "gt2")
            pgf = k.ps(es1, [128, NTL, 4], F32, "pgf")
            pgb = k.ps(es1, [128, NTL, 4], F32, "pgb")
            ptot = k.ps(es1, [128, 2, NTL, 4], F32, "ptot")
            k.act(e1, gv[:, :, :, 1, :], AF.Exp, scale=-1.0)
            k.act(l, e1, AF.Ln, bias=one_c)
            k.mm(pgf, self.maskF, l[:, 0])
            k.mm(pgb, self.maskB, l[:, 1])
            k.mm(ptot, self.ones, l)
            k.copy(cc[:, 0], pgf, eng="act")
            k.copy(cc[:, 1], pgb, eng="act")
            k.copy(ct, ptot, eng="act")
            v = lambda X: X.rearrange("p c (d h) -> p d c h", d=2)
            k.act(v(EB), cc, AF.Exp, scale=-1.0)
            k.tt(t1, cc, gv[:, :, :, 0, :], ALU.add)
            k.act(v(WW), t1, AF.Exp)
            k.tt(t2, t1, ct, ALU.subtract)
            k.act(v(UU), t2, AF.Exp)
            k.act(v(ET), ct, AF.Exp, scale=-1.0)
            k.barrier()
        Cst = [sb([128, 129], F32, "Cst") for _ in range(8)]
        Cbf = [sb([128, 129], BF16, "Cbf") for _ in range(8)]
        for hd in range(8):
            k.memset(Cst[hd], 0.0)
            k.memset(Cbf[hd], 0.0, eng="pool")
        qTs = [[sb([128, 4, 128], BF16, "qT") for _ in range(2)] for _ in range(2)]
        kTs = [[sb([128, 4, 128], BF16, "kT") for _ in range(2)] for _ in range(2)]
        vaug = [[sb([128, 4, 129], BF16, "vaug") for _ in range(2)] for _ in range(2)]
        for vv in vaug:
            for v_ in vv:
                k.memset(v_[:, :, 128:129], 1.0)
        ktoks = [sb([128, 128], BF16, "ktok") for _ in range(3)]
        STs = [sb([128, 128], BF16, "ST") for _ in range(3)]
        uvs = [sb([128, 129], BF16, "uv") for _ in range(3)]
        sms = [sb([128, 6, 2, 2], F32, "sm") for _ in range(2)]
        sq = sb([128, 512], F32, "sq")
        tmpm = sb([128, 512], F32, "tmpm")
        mot = sb([128, 512], F32, "mot")
        mtok = sb([128, 512], BF16, "mtok")
        mixT = sb([128, 4, 128], BF16, "mixT")
        hst = sb([128, 12], F32, "hst")
        mnw = sb([128, 4], F32, "mnw")
        k.dma(mnw, self.mnw[layer])
        pks = [k.ps(es, [128, 1024], BF16, "pk") for _ in range(2)]
        pSs = [k.ps(es, [128, 512], F32, "pS") for _ in range(2)]
        pNt = k.ps(es, [128, 2, 512], F32, "pN")
        pCb = k.ps(es, [128, 512], F32, "pCb")
        pfin = k.ps(es, [128, 1024], BF16, "pfin")
        order = {0: [32, 33] + list(range(32)), 1: [33, 32] + list(range(31, -1, -1))}
        masks = (self.maskF, self.maskB)
        items = [(s_, d, h) for s_ in range(NTL) for d in (0, 1) for h in range(4)]
        seen = set()

        def bufs(s_, d):
            return qTs[d][s_ % 2], kTs[d][s_ % 2], vaug[d][s_ % 2]

        def stage_A(n):
            s_, d, h = items[n]
            c = order[d][s_]
            qT, kT, va = bufs(s_, d)
            if h == 0:
                rows = slice(c * 128, (c + 1) * 128)
                k.dma(qT, self.qT_d[:, :, rows].rearrange("h p n -> p h n"))
                k.dma(kT, self.kT_d[:, :, rows].rearrange("h p n -> p h n"))
                k.dma(va[:, :, 0:128], self.mv_d[rows, :].rearrange("p (h e) -> p h e", h=4))
            k.tr(pks[n % 2][:, 0:128], kT[:, h, :], self.identb)
            k.mm(pSs[n % 2][:, 0:128], kT[:, h, :], qT[:, h, :])

        def stage_B(n):
            s_, d, h = items[n]
            c = order[d][s_]
            hd = d * 4 + h
            qT, kT, va = bufs(s_, d)
            ktok, ST, uv = ktoks[n % 3], STs[n % 3], uvs[n % 3]
            k.copy(ktok, pks[n % 2][:, 0:128], eng="act")
            k.stt(ST, pSs[n % 2][:, 0:128], WW[:, c, hd:hd + 1], masks[d], ALU.mult, ALU.mult)
            k.act(uv, va[:, h, :], AF.Copy, scale=UU[:, c, hd:hd + 1])
            pN = pNt[:, h // 2, (h % 2) * 129:(h % 2) * 129 + 129]
            pC = pCb[:, 0:129]
            k.mm(pN, ST, va[:, h, :], start=True, stop=False)
            k.mm(pN, qT[:, h, :], Cbf[hd], start=False, stop=True)
            k.mm(pC, ktok, uv)
            k.stt(Cst[hd], Cst[hd], ET[:, c, hd:hd + 1], pC, ALU.mult, ALU.add)
            k.copy(Cbf[hd], Cst[hd], eng="act")

        def stage_E(s_, d):
            c = order[d][s_]
            sm = sms[d]
            eb4 = EB[:, c, d * 4:(d + 1) * 4].rearrange("p (b h) -> p b h", b=2)
            for b in range(2):
                qn2 = pNt[:, b, 0:258].rearrange("p (h e) -> p h e", e=129)[:, :, 128]
                k.tt(sm[:, 0, b, :], qn2, eb4[:, b, :], ALU.mult)
            k.ts(sm[:, 1], sm[:, 0], -1.0, ALU.mult)
            k.tt(sm[:, 2], sm[:, 0], sm[:, 1], ALU.max)
            k.ts(sm[:, 3], sm[:, 2], 1.0, ALU.max)
            k.recip(sm[:, 4], sm[:, 3])
            k.tt(sm[:, 5], sm[:, 4], eb4, ALU.mult)
            for h in range(4):
                sc = sm[:, 5, h // 2, h % 2:h % 2 + 1]
                num = pNt[:, h // 2, (h % 2) * 129:(h % 2) * 129 + 128]
                dst = (hsum[:, c, h * 128:(h + 1) * 128], c)
                if c not in seen:
                    k.ts(dst, num, sc, ALU.mult)
                else:
                    k.stt(dst, num, sc, dst, ALU.mult, ALU.add)
            if c in seen:
                return c
            seen.add(c)
            return None

        def finalize(c):
            rows = slice(c * 128, (c + 1) * 128)
            hs = (hsum[:, c, :], c)
            k.act(sq, hs, AF.Square)
            k.reduce(hst[:, 0:4], sq.rearrange("p (h e) -> p h e", h=4), ALU.add)
            k.act(hst[:, 4:8], hst[:, 0:4], AF.Sqrt, bias=self.eps_t, scale=1.0 / 128)
            k.recip(hst[:, 8:12], hst[:, 4:8])
            k.dma(mot, self.mo_d[rows, :])
            k.tt(tmpm, hs, mot, ALU.mult)
            for h in range(4):
                k.ts(mtok[:, h * 128:(h + 1) * 128], tmpm[:, h * 128:(h + 1) * 128], hst[:, 8 + h:9 + h], ALU.mult)
            for h in range(4):
                k.tr(pfin[:, h * 128:(h + 1) * 128], mtok[:, h * 128:(h + 1) * 128], self.identb)
            for h in range(4):
                k.act(mixT[:, h, :], pfin[:, h * 128:(h + 1) * 128], AF.Copy, scale=mnw[:, h:h + 1])
            k.dma(self.mixT_d[0:4, :, rows].rearrange("c p n -> p c n"), mixT)

        stage_A(0)
        pend_fin = []
        for n, (s_, d, h) in enumerate(items):
            if n + 1 < len(items):
                stage_A(n + 1)
            for c in pend_fin:
                finalize(c)
            pend_fin = []
            stage_B(n)
            if h == 3:
                done = stage_E(s_, d)
                if done is not None:
                    pend_fin.append(done)
        for c in pend_fin:
            finalize(c)
        if self.stopped(layer, "D1"):
            k.dma(self.hs_dbg, hsum)
        k.barrier()


Prog.phase_mlstm = _phase_mlstm


def _phase_na(self, layer):
    k = self.k
    last = layer == 1
    with ExitStack() as es:
        sb = lambda shape, dt=F32, name=None: k.sb(es, shape, dt, name)
        biasA = sb([128, 8, 640], BF16, "biasA")
        biasS = sb([128, 8, 640], BF16, "biasS")
        k.dma(biasA, self.nabias[layer, 2].rearrange("h q n -> q h n"), q="pool")
        nqv = self.nqT_d.rearrange("t (two p) n -> p (t two) n", p=64)
        nkv = self.nkT_d.rearrange("t (two p) n -> p (t two) n", p=64)
        kctx = sb([64, 8, 256], BF16, "kctx")
        k.dma(kctx, nkv[:, :, L:NT])
        vctx = sb([128, 2, 8, 65], BF16, "vctx")
        k.memset(vctx[:, :, :, 64:65], 1.0)
        for cc in range(2):
            k.dma(vctx[:, cc, :, 0:64],
                  self.nv_d[L + cc * 128:L + (cc + 1) * 128, :].rearrange("p (h e) -> p h e", h=8))
        qs = [sb([64, 8, 128], BF16, "naq") for _ in range(2)]
        kws = [sb([64, 8, 640], BF16, "nak") for _ in range(2)]
        vws = [sb([128, 5, 8, 65], BF16, "nav") for _ in range(2)]
        for v in vws:
            k.memset(v[:, :, :, 64:65], 1.0)
        PTs = [sb([128, 7, 128], BF16, "PT") for _ in range(2)]
        ots = [sb([128, 512], BF16, "otok") for _ in range(2)]
        rcs = [sb([128, 8], F32, "rc") for _ in range(2)]
        mxs = [sb([128, 512], BF16, "mx") for _ in range(2)]
        pSs = [k.ps(es, [128, 8, 128], F32, "naS") for _ in range(2)]
        pOs = [k.ps(es, [128, 512], F32, "naO") for _ in range(2)]
        ptr = k.ps(es, [128, 1024], BF16, "natr")
        types = {0: 0, 1: 1, 30: 3, 31: 4}
        blocks = [(j, True) for j in range(32)] + ([] if last else [(0, False), (1, False)])
        binfo = {}

        def prologue(bi):
            j, lat = blocks[bi]
            q, kw, vw = qs[bi % 2], kws[bi % 2], vws[bi % 2]
            bias = None
            if lat:
                tok0 = j * 128
                cs = min(max(j - 2, 0), 27)
                nwin = 5
                if j in types:
                    k.dma(biasS, self.nabias[layer, types[j]].rearrange("h q n -> q h n"), q="pool")
                    bias = biasS
                else:
                    bias = biasA
                k.dma(kw, nkv[:, :, cs * 128:cs * 128 + 640])
                for c in range(5):
                    k.dma(vw[:, c, :, 0:64],
                          self.nv_d[(cs + c) * 128:(cs + c + 1) * 128, :].rearrange("p (h e) -> p h e", h=8))
            else:
                tok0 = L + j * 128
                nwin = 0
            k.dma(q, nqv[:, :, tok0:tok0 + 128])
            binfo[bi] = (tok0, nwin, bias)

        def stage_S(bi, hh, n):
            if hh == 0:
                prologue(bi)
            tok0, nwin, bias = binfo[bi]
            q, kw = qs[bi % 2], kws[bi % 2]
            pS = pSs[n % 2]
            for c in range(nwin):
                k.mm(pS[:, c, :], kw[:, hh, c * 128:(c + 1) * 128], q[:, hh, :], start=True, stop=False)
                k.mm(pS[:, c, :], bias[:, hh, c * 128:(c + 1) * 128], self.identb, start=False, stop=True)
            for cc in range(2):
                k.mm(pS[:, nwin + cc, :], kctx[:, hh, cc * 128:(cc + 1) * 128], q[:, hh, :])

        def stage_rest(bi, hh, n):
            tok0, nwin, bias = binfo[bi]
            vw, ot, rc = vws[bi % 2], ots[bi % 2], rcs[bi % 2]
            pS, PT, pO = pSs[n % 2], PTs[n % 2], pOs[n % 2][:, 0:65]
            nch = nwin + 2
            if nch > 4:
                k.act(PT[:, 0:4, :], pS[:, 0:4, :], AF.Exp)
                k.act(PT[:, 4:nch, :], pS[:, 4:nch, :], AF.Exp)
            else:
                k.act(PT[:, 0:nch, :], pS[:, 0:nch, :], AF.Exp)
            for c in range(nch):
                rhs = vw[:, c, hh, :] if c < nwin else vctx[:, c - nwin, hh, :]
                k.mm(pO, PT[:, c, :], rhs, start=(c == 0), stop=(c == nch - 1))
            k.recip(rc[:, hh:hh + 1], pO[:, 64:65])
            k.ts(ot[:, hh * 64:(hh + 1) * 64], pO[:, 0:64], rc[:, hh:hh + 1], ALU.mult)

        def epilogue(bi):
            tok0 = binfo[bi][0]
            ot, mx = ots[bi % 2], mxs[bi % 2]
            po = (bi % 2) * 512
            for t4 in range(4):
                k.tr((ptr[:, po + t4 * 128:po + (t4 + 1) * 128], bi % 2), ot[:, t4 * 128:(t4 + 1) * 128], self.identb)
            k.copy(mx, (ptr[:, po:po + 512], bi % 2), eng="act")
            k.dma(self.mixT_d[4:8, :, tok0:tok0 + 128].rearrange("c p n -> p c n"),
                  mx.rearrange("p (c n) -> p c n", c=4))

        items = [(bi, hh) for bi in range(len(blocks)) for hh in range(8)]
        stage_S(items[0][0], items[0][1], 0)
        pending_epi = None
        for n, (bi, hh) in enumerate(items):
            if n + 1 < len(items):
                stage_S(items[n + 1][0], items[n + 1][1], n + 1)
            if pending_epi is not None:
                epilogue(pending_epi)
                pending_epi = None
            stage_rest(bi, hh, n)
            if hh == 7:
                pending_epi = bi
        epilogue(pending_epi)
        k.barrier()


Prog.phase_na = _phase_na


def _router(self, LG, n, es):
    k = self.k
    t16 = lambda nm: k.sb(es, [128, n, 16], F32, nm)
    t1 = lambda nm: k.sb(es, [128, n], F32, nm)
    B16 = lambda a: a.unsqueeze(2).to_broadcast([128, n, 16])
    mx, ssum, rs, gm, m1, m2, wsum, rws = [t1(f"r1_{j}") for j in range(8)]
    e, sc, sel, m16, tt16, msel, is1, msel2, is2, wts = [t16(f"r16_{j}") for j in range(10)]
    ps6 = k.sb(es, [128, n, 4, 6], F32, "ps6")
    gs = k.sb(es, [128, n, 4], F32, "gs")
    ing = k.sb(es, [128, n, 4], F32, "ing")
    k.reduce(mx, LG, ALU.max)
    k.tt(e, LG, B16(mx), ALU.subtract)
    k.act(e, e, AF.Exp)
    k.reduce(ssum, e, ALU.add)
    k.recip(rs, ssum)
    k.tt(sc, e, B16(rs), ALU.mult)
    k.tt(sel, sc, self.rb_bc.unsqueeze(1).to_broadcast([128, n, 16]), ALU.add)
    selv = sel.rearrange("p n (g e) -> p n g e", g=4)
    for pi, (a, b) in enumerate(((0, 1), (0, 2), (0, 3), (1, 2), (1, 3), (2, 3))):
        k.tt(ps6[:, :, :, pi], selv[:, :, :, a], selv[:, :, :, b], ALU.add)
    k.reduce(gs, ps6, ALU.max)
    k.reduce(gm, gs, ALU.max)
    k.tt(ing, gs, gm.unsqueeze(2).to_broadcast([128, n, 4]), ALU.is_equal)
    k.copy(m16.rearrange("p n (g e) -> p n g e", g=4), ing.unsqueeze(3).to_broadcast([128, n, 4, 4]))
    k.ts(tt16, m16, 10.0, ALU.mult, -10.0, ALU.add)
    k.tt(msel, sel, m16, ALU.mult)
    k.tt(msel, msel, tt16, ALU.add)
    k.reduce(m1, msel, ALU.max)
    k.tt(is1, msel, B16(m1), ALU.is_equal)
    k.stt(msel2, is1, -20.0, msel, ALU.mult, ALU.add)
    k.reduce(m2, msel2, ALU.max)
    k.tt(is2, msel2, B16(m2), ALU.is_equal)
    k.tt(is1, is1, is2, ALU.add)
    k.tt(wts, sc, is1, ALU.mult)
    k.reduce(wsum, wts, ALU.add)
    k.recip(rws, wsum)
    k.tt(self.gate_sb[:, 0:n, :], wts, B16(rws), ALU.mult)


def _phase_wout_norm2(self, layer):
    k = self.k
    last = layer == 1
    ntl = 32 if last else NTL
    with ExitStack() as es:
        sb = lambda shape, dt=F32, name=None: k.sb(es, shape, dt, name)
        wo = sb([128, 8, D], BF16, "wo")
        wov = self.w_out[layer].rearrange("(j p) n -> p j n", p=128)
        for half in range(2):
            k.dma(wo[:, :, half * 512:(half + 1) * 512], wov[:, :, half * 512:(half + 1) * 512], q="pool")
        mixs = [sb([128, 8, 128], BF16, "mix") for _ in range(2)]
        xts = [sb([128, D], F32, "xt2") for _ in range(4)]
        tmps = [sb([128, D], F32, "tmp2") for _ in range(2)]
        nb = [(sb([128, D], F32, "junk"), sb([128, D], F32, "xn"), sb([128, 4], F32, "st")) for _ in range(3)]
        pts = [k.ps(es, [128, 8, 128], F32, "ptr") for _ in range(2)]
        r32s = [sb([128, 8, 128], F32, "r32") for _ in range(3)]
        LG = sb([128, ntl, 16], F32, "LG")
        py = k.ps(es, [128, 2, 512], F32, "py")
        plogs = [k.ps(es, [128, 16], F32, "plog") for _ in range(2)]

        def stage1(i):
            w = 0 if i < 32 else 1
            rows = slice(i * 128, (i + 1) * 128)
            mix, xt, tmp = mixs[i % 2], xts[i % 4], tmps[i % 2]
            k.dma(mix, self.mixT_d[:, :, rows].rearrange("c p n -> p c n"))
            k.dma(xt, self.xsrc(layer, i))
            for half in range(2):
                for mch in range(8):
                    k.mm(py[:, half, :], mix[:, mch, :], wo[:, mch, half * 512:(half + 1) * 512],
                         start=(mch == 0), stop=(mch == 7))
            for half in range(2):
                k.tt(tmp[:, half * 512:(half + 1) * 512], py[:, half, :], self.bc[(2, w)][:, half * 512:(half + 1) * 512], ALU.mult)
            k.tt(xt, xt, tmp, ALU.add)
            k.dma((self.xres[rows, :], i), xt)

        def stage2a(i):
            self.norm_stats(xts[i % 4], nb[i % 3])

        def stage2b(i):
            self.norm_tr(i, nb[i % 3][1], pts[i % 2], self.ws2, self.sh2, 0 if i < 32 else 1, r32=r32s[i % 3])

        def stage2c(i):
            r32, plog = r32s[i % 3], plogs[i % 2]
            for dch in range(8):
                k.mm(plog, r32[:, dch, :], self.rw_sb[:, dch, :], start=(dch == 0), stop=(dch == 7))
            k.copy((LG[:, i, :], i), plog)

        for t in range(ntl + 3):
            if t < ntl:
                stage1(t)
            if 0 <= t - 1 < ntl:
                stage2a(t - 1)
            if 0 <= t - 2 < ntl:
                stage2b(t - 2)
            if 0 <= t - 3 < ntl:
                stage2c(t - 3)
        self.router(LG, ntl, es)
        k.barrier()


Prog.router = _router
Prog.phase_wout_norm2 = _phase_wout_norm2


def _phase_moe(self, layer):
    k = self.k
    last = layer == 1
    ntl = 32 if last else NTL
    nblk = 4
    bounds = [round(b * ntl / nblk) for b in range(nblk + 1)]
    with ExitStack() as es:
        sb = lambda shape, dt=F32, name=None: k.sb(es, shape, dt, name)
        nbmax = max(bounds[b + 1] - bounds[b] for b in range(nblk))
        yacc = sb([128, nbmax, 2, 512], F32, "yacc")
        w1s = [sb([128, 8, DFF], BF16, "w1") for _ in range(2)]
        w3s = [sb([128, 8, DFF], BF16, "w3") for _ in range(2)]
        w2s = [sb([128, 4, D], BF16, "w2") for _ in range(2)]
        aTs = [sb([128, 4, 512], BF16, "aT") for _ in range(2)]
        sTs = [sb([128, 512], F32, "sT") for _ in range(2)]
        xts = [sb([128, D], F32, "xt3") for _ in range(2)]
        tmps = [sb([128, D], F32, "tmp3") for _ in range(2)]
        st = sb([128, 4], F32, "st3")
        if last:
            fnw_bc = sb([128, D], F32, "fnw")
            k.dma(fnw_bc, self.fnw.partition_broadcast(128))
        p1s = [k.ps(es, [128, 512], F32, "p1") for _ in range(2)]
        p3s = [k.ps(es, [128, 512], F32, "p3") for _ in range(2)]
        pys = [k.ps(es, [128, 2, 512], F32, "pym") for _ in range(2)]
        w1v = self.w1[layer].rearrange("e (j p) n -> e p j n", p=128)
        w3v = self.w3[layer].rearrange("e (j p) n -> e p j n", p=128)
        w2v = self.w2[layer].rearrange("e (j p) n -> e p j n", p=128)
        cnt = {"nh": 0, "ny": 0}
        items = []
        blk_tiles = []
        for b in range(nblk):
            tiles = list(range(bounds[b], bounds[b + 1]))
            blk_tiles.append(tiles)
            groups = []
            for i in tiles:
                if groups and len(groups[-1]) < 4 and self.hcol(groups[-1][-1]) + 128 == self.hcol(i):
                    groups[-1].append(i)
                else:
                    groups.append([i])
            for e in range(NE):
                for gi, grp in enumerate(groups):
                    items.append((b, e, grp, gi == 0, e == NE - 1 and gi == len(groups) - 1))

        def h_steps(n):
            b, e, grp, first, _ = items[n]
            we = (b * NE + e) % 2
            w1, w3, w2 = w1s[we], w3s[we], w2s[we]
            nt = 128 * len(grp)
            c0 = self.hcol(grp[0])
            aT = aTs[n % 2]

            def step(fch):
                if first and fch == 0:
                    k.dma(w1, w1v[e], q="pool")
                    k.dma(w3, w3v[e], q="pool")
                    k.dma(w2, w2v[e], q="pool")
                p1, p3, sT = p1s[cnt["nh"] % 2], p3s[cnt["nh"] % 2], sTs[cnt["nh"] % 2]
                cnt["nh"] += 1
                for dch in range(8):
                    k.mm(p1[:, :nt], w1[:, dch, fch * 128:(fch + 1) * 128], self.hT[:, dch, c0:c0 + nt],
                         start=(dch == 0), stop=(dch == 7))
                for dch in range(8):
                    k.mm(p3[:, :nt], w3[:, dch, fch * 128:(fch + 1) * 128], self.hT[:, dch, c0:c0 + nt],
                         start=(dch == 0), stop=(dch == 7))
                k.act(sT[:, :nt], p1[:, :nt], AF.Silu)
                k.tt(aT[:, fch, :nt], p3[:, :nt], sT[:, :nt], ALU.mult)
            return [lambda fch=fch: step(fch) for fch in range(4)]

        def y_steps(n):
            b, e, grp, _, _ = items[n]
            w2 = w2s[(b * NE + e) % 2]
            aT = aTs[n % 2]

            def step(tl):
                i = grp[tl]
                bt = i - bounds[b]
                py = pys[cnt["ny"] % 2]
                cnt["ny"] += 1
                for half in range(2):
                    for fch in range(4):
                        k.mm(py[:, half, :], aT[:, fch, tl * 128:(tl + 1) * 128],
                             w2[:, fch, half * 512:(half + 1) * 512], start=(fch == 0), stop=(fch == 3))
                gcol = (self.gate_sb[:, i, e:e + 1], i)
                for half in range(2):
                    ya = (yacc[:, bt, half, :], (bt, half))
                    if e == 0:
                        k.ts(ya, py[:, half, :], gcol, ALU.mult)
                    else:
                        k.stt(ya, py[:, half, :], gcol, ya, ALU.mult, ALU.add)
            return [lambda tl=tl: step(tl) for tl in range(len(grp))]

        for f_ in h_steps(0):
            f_()
        for n in range(len(items)):
            hn = h_steps(n + 1) if n + 1 < len(items) else []
            yn = y_steps(n)
            for j in range(max(len(hn), len(yn))):
                if j < len(hn):
                    hn[j]()
                if j < len(yn):
                    yn[j]()
            if not items[n][4]:
                continue
            b = items[n][0]
            tiles = blk_tiles[b]
            for i in tiles:
                bt = i - bounds[b]
                w = 0 if i < 32 else 1
                rows = slice(i * 128, (i + 1) * 128)
                xt, tmp = xts[i % 2], tmps[i % 2]
                k.dma(xt, (self.xres[rows, :], i))
                for half in range(2):
                    k.tt(tmp[:, half * 512:(half + 1) * 512], (yacc[:, bt, half, :], (bt, half)),
                         self.bc[(5, w)][:, half * 512:(half + 1) * 512], ALU.mult)
                k.tt(xt, xt, tmp, ALU.add)
                if not last:
                    k.dma((self.xres[rows, :], i), xt)
                else:
                    k.act(tmp, xt, AF.Square)
                    k.reduce(st[:, 0:1], tmp, ALU.add)
                    k.act(st[:, 1:2], st[:, 0:1], AF.Sqrt, bias=self.eps_t, scale=1.0 / D)
                    k.recip(st[:, 2:3], st[:, 1:2])
                    k.stt(tmp, xt, st[:, 2:3], fnw_bc, ALU.mult, ALU.mult)
                    k.dma(self.out[rows, :], tmp, is_output=True)
        k.barrier()


Prog.phase_moe = _phase_moe


def _phase_mix(self, layer):
    k = self.k
    last = layer == 1
    with ExitStack() as es:
        sb = lambda shape, dt=F32, name=None: k.sb(es, shape, dt, name)
        EB = sb([128, NTL, 8], F32, "EB")
        WW = sb([128, NTL, 8], F32, "WW")
        UU = sb([128, NTL, 8], F32, "UU")
        ET = sb([128, NTL, 8], F32, "ET")
        hsum = sb([128, NTL, 512], F32, "hsum")
        one_c = self.ones[:, 0:1]
        with ExitStack() as es1:
            sb1 = lambda shape, dt=F32, name=None: k.sb(es1, shape, dt, name)
            gall = sb1([128, NTL, 2, 2, 4], F32, "gall")
            k.dma(gall, self.g_d.rearrange("(c p) (d t h) -> p c d t h", p=128, d=2, t=2))
            gv = gall.rearrange("p c d t h -> p d c t h")
            e1 = sb1([128, 2, NTL, 4], F32, "e1")
            l = sb1([128, 2, NTL, 4], F32, "lsp")
            cc = sb1([128, 2, NTL, 4], F32, "cc")
            ct = sb1([128, 2, NTL, 4], F32, "ct")
            t1 = sb1([128, 2, NTL, 4], F32, "gt1")
            t2 = sb1([128, 2, NTL, 4], F32, "gt2")
            pgf = k.ps(es1, [128, NTL, 4], F32, "pgf")
            pgb = k.ps(es1, [128, NTL, 4], F32, "pgb")
            ptot = k.ps(es1, [128, 2, NTL, 4], F32, "ptot")
            k.act(e1, gv[:, :, :, 1, :], AF.Exp, scale=-1.0)
            k.act(l, e1, AF.Ln, bias=one_c)
            k.mm(pgf, self.maskF, l[:, 0])
            k.mm(pgb, self.maskB, l[:, 1])
            k.mm(ptot, self.ones, l)
            k.copy(cc[:, 0], pgf, eng="act")
            k.copy(cc[:, 1], pgb, eng="act")
            k.copy(ct, ptot, eng="act")
            v = lambda X: X.rearrange("p c (d h) -> p d c h", d=2)
            k.act(v(EB), cc, AF.Exp, scale=-1.0)
            k.tt(t1, cc, gv[:, :, :, 0, :], ALU.add)
            k.act(v(WW), t1, AF.Exp)
            k.tt(t2, t1, ct, ALU.subtract)
            k.act(v(UU), t2, AF.Exp)
            k.act(v(ET), ct, AF.Exp, scale=-1.0)
            k.barrier()
        Cst = [sb([128, 129], F32, "Cst") for _ in range(8)]
        Cbf = [sb([128, 129], BF16, "Cbf") for _ in range(8)]
        for hd in range(8):
            k.memset(Cst[hd], 0.0)
            k.memset(Cbf[hd], 0.0, eng="pool")
        qTs = [[sb([128, 4, 128], BF16, "qT") for _ in range(2)] for _ in range(2)]
        kTs = [[sb([128, 4, 128], BF16, "kT") for _ in range(2)] for _ in range(2)]
        vaug = [[sb([128, 4, 129], BF16, "vaug") for _ in range(2)] for _ in range(2)]
        for vv in vaug:
            for v_ in vv:
                k.memset(v_[:, :, 128:129], 1.0)
        ktoks = [sb([128, 128], BF16, "ktok") for _ in range(3)]
        STs = [sb([128, 128], BF16, "ST") for _ in range(3)]
        uvs = [sb([128, 129], BF16, "uv") for _ in range(3)]
        sms = [sb([128, 6, 2, 2], F32, "sm") for _ in range(2)]
        sq = sb([128, 512], F32, "sq")
        tmpm = sb([128, 512], F32, "tmpm")
        mot = sb([128, 512], F32, "mot")
        mtok = sb([128, 512], BF16, "mtok")
        mixTm = sb([128, 4, 128], BF16, "mixTm")
        hst = sb([128, 12], F32, "hst")
        mnw = sb([128, 4], F32, "mnw")
        k.dma(mnw, self.mnw[layer])
        biasA = sb([128, 8, 640], BF16, "biasA")
        biasS = sb([128, 8, 640], BF16, "biasS")
        k.dma(biasA, self.nabias[layer, 2].rearrange("h q n -> q h n"), q="pool")
        nqv = self.nqT_d.rearrange("t (two p) n -> p (t two) n", p=64)
        nkv = self.nkT_d.rearrange("t (two p) n -> p (t two) n", p=64)
        kctx = sb([64, 8, 256], BF16, "kctx")
        k.dma(kctx, nkv[:, :, L:NT])
        vctx = sb([128, 2, 8, 65], BF16, "vctx")
        k.memset(vctx[:, :, :, 64:65], 1.0)
        for cc_ in range(2):
            k.dma(vctx[:, cc_, :, 0:64],
                  self.nv_d[L + cc_ * 128:L + (cc_ + 1) * 128, :].rearrange("p (h e) -> p h e", h=8))
        qs = [sb([64, 8, 128], BF16, "naq") for _ in range(2)]
        kws = [sb([64, 8, 640], BF16, "nak") for _ in range(2)]
        vws = [sb([128, 5, 8, 65], BF16, "nav") for _ in range(2)]
        for v_ in vws:
            k.memset(v_[:, :, :, 64:65], 1.0)
        PTs = [sb([128, 7, 128], BF16, "PT") for _ in range(2)]
        ots = [sb([128, 512], BF16, "otok") for _ in range(2)]
        rcs = [sb([128, 8], F32, "rc") for _ in range(2)]
        mxs = [sb([128, 512], BF16, "mx") for _ in range(2)]
        naS = k.ps(es, [128, 4, 128], F32, "naS")
        naO = k.ps(es, [128, 512], F32, "naO")
        trb = k.ps(es, [128, 1024], BF16, "trb")
        pk = k.ps(es, [128, 1024], BF16, "pk")
        pS = k.ps(es, [128, 512], F32, "pS")
        pNt = k.ps(es, [128, 2, 512], F32, "pN")
        pCb = k.ps(es, [128, 512], F32, "pCb")

        order = {0: [32, 33] + list(range(32)), 1: [33, 32] + list(range(31, -1, -1))}
        masks = (self.maskF, self.maskB)
        mitems = [(s_, d, h) for s_ in range(NTL) for d in (0, 1) for h in range(4)]
        seen = set()

        def mbufs(s_, d):
            return qTs[d][s_ % 2], kTs[d][s_ % 2], vaug[d][s_ % 2]

        def ml_A(n):
            s_, d, h = mitems[n]
            c = order[d][s_]
            qT, kT, va = mbufs(s_, d)
            if h == 0:
                rows = slice(c * 128, (c + 1) * 128)
                k.dma(qT, self.qT_d[:, :, rows].rearrange("h p n -> p h n"))
                k.dma(kT, self.kT_d[:, :, rows].rearrange("h p n -> p h n"))
                k.dma(va[:, :, 0:128], self.mv_d[rows, :].rearrange("p (h e) -> p h e", h=4))
            k.tr(pk[:, 0:128], kT[:, h, :], self.identb)
            k.mm(pS[:, 0:128], kT[:, h, :], qT[:, h, :])

        def ml_B(n):
            s_, d, h = mitems[n]
            c = order[d][s_]
            hd = d * 4 + h
            qT, kT, va = mbufs(s_, d)
            ktok, ST, uv = ktoks[n % 3], STs[n % 3], uvs[n % 3]
            k.copy(ktok, pk[:, 0:128], eng="act")
            k.stt(ST, pS[:, 0:128], WW[:, c, hd:hd + 1], masks[d], ALU.mult, ALU.mult)
            k.act(uv, va[:, h, :], AF.Copy, scale=UU[:, c, hd:hd + 1])

        def ml_C(n):
            s_, d, h = mitems[n]
            c = order[d][s_]
            hd = d * 4 + h
            qT, kT, va = mbufs(s_, d)
            ktok, ST, uv = ktoks[n % 3], STs[n % 3], uvs[n % 3]
            pN = pNt[:, h // 2, (h % 2) * 129:(h % 2) * 129 + 129]
            pC = pCb[:, 0:129]
            k.mm(pN, ST, va[:, h, :], start=True, stop=False)
            k.mm(pN, qT[:, h, :], Cbf[hd], start=False, stop=True)
            k.mm(pC, ktok, uv)
            k.stt(Cst[hd], Cst[hd], ET[:, c, hd:hd + 1], pC, ALU.mult, ALU.add)
            k.copy(Cbf[hd], Cst[hd], eng="pool")

        def ml_E(s_, d):
            c = order[d][s_]
            sm = sms[d]
            eb4 = EB[:, c, d * 4:(d + 1) * 4].rearrange("p (b h) -> p b h", b=2)
            for b in range(2):
                qn2 = pNt[:, b, 0:258].rearrange("p (h e) -> p h e", e=129)[:, :, 128]
                k.tt(sm[:, 0, b, :], qn2, eb4[:, b, :], ALU.mult)
            k.ts(sm[:, 1], sm[:, 0], -1.0, ALU.mult)
            k.tt(sm[:, 2], sm[:, 0], sm[:, 1], ALU.max)
            k.ts(sm[:, 3], sm[:, 2], 1.0, ALU.max)
            k.recip(sm[:, 4], sm[:, 3])
            k.tt(sm[:, 5], sm[:, 4], eb4, ALU.mult)
            for h in range(4):
                sc = sm[:, 5, h // 2, h % 2:h % 2 + 1]
                num = pNt[:, h // 2, (h % 2) * 129:(h % 2) * 129 + 128]
                dst = (hsum[:, c, h * 128:(h + 1) * 128], c)
                if c not in seen:
                    k.ts(dst, num, sc, ALU.mult)
                else:
                    k.stt(dst, num, sc, dst, ALU.mult, ALU.add)
            if c in seen:
                return c
            seen.add(c)
            return None

        def ml_fin(c):
            rows = slice(c * 128, (c + 1) * 128)
            hs = (hsum[:, c, :], c)
            k.act(sq, hs, AF.Square)
            k.reduce(hst[:, 0:4], sq.rearrange("p (h e) -> p h e", h=4), ALU.add)
            k.act(hst[:, 4:8], hst[:, 0:4], AF.Sqrt, bias=self.eps_t, scale=1.0 / 128)
            k.recip(hst[:, 8:12], hst[:, 4:8])
            k.dma(mot, self.mo_d[rows, :])
            k.tt(tmpm, hs, mot, ALU.mult)
            for h in range(4):
                k.ts(mtok[:, h * 128:(h + 1) * 128], tmpm[:, h * 128:(h + 1) * 128], hst[:, 8 + h:9 + h], ALU.mult)
            for h in range(4):
                k.tr(trb[:, h * 128:(h + 1) * 128], mtok[:, h * 128:(h + 1) * 128], self.identb)
            for h in range(4):
                k.act(mixTm[:, h, :], trb[:, h * 128:(h + 1) * 128], AF.Copy, scale=mnw[:, h:h + 1])
            k.dma(self.mixT_d[0:4, :, rows].rearrange("c p n -> p c n"), mixTm)

        types = {0: 0, 1: 1, 30: 3, 31: 4}
        blocks = [(j, True) for j in range(32)] + ([] if last else [(0, False), (1, False)])
        nitems = [(bi, hh) for bi in range(len(blocks)) for hh in range(8)]
        binfo = {}

        def na_prologue(bi):
            j, lat = blocks[bi]
            q, kw, vw = qs[bi % 2], kws[bi % 2], vws[bi % 2]
            bias = None
            if lat:
                tok0 = j * 128
                cs = min(max(j - 2, 0), 27)
                nwin = 5
                if j in types:
                    k.dma(biasS, self.nabias[layer, types[j]].rearrange("h q n -> q h n"), q="pool")
                    bias = biasS
                else:
                    bias = biasA
                k.dma(kw, nkv[:, :, cs * 128:cs * 128 + 640])
                for c in range(5):
                    k.dma(vw[:, c, :, 0:64],
                          self.nv_d[(cs + c) * 128:(cs + c + 1) * 128, :].rearrange("p (h e) -> p h e", h=8))
            else:
                tok0 = L + j * 128
                nwin = 0
            k.dma(q, nqv[:, :, tok0:tok0 + 128])
            binfo[bi] = (tok0, nwin, bias)

        def na_S(m, lo, hi):
            bi, hh = nitems[m]
            tok0, nwin, bias = binfo[bi]
            q, kw = qs[bi % 2], kws[bi % 2]
            for c in range(lo, hi):
                o = naS[:, c - lo, :]
                if c < nwin:
                    k.mm(o, kw[:, hh, c * 128:(c + 1) * 128], q[:, hh, :], start=True, stop=False)
                    k.mm(o, bias[:, hh, c * 128:(c + 1) * 128], self.identb, start=False, stop=True)
                else:
                    cc_ = c - nwin
                    k.mm(o, kctx[:, hh, cc_ * 128:(cc_ + 1) * 128], q[:, hh, :])

        def na_exp(m, lo, hi):
            PT = PTs[m % 2]
            k.act(PT[:, lo:hi, :], naS[:, 0:hi - lo, :], AF.Exp)

        def na_PV(m):
            bi, hh = nitems[m]
            tok0, nwin, bias = binfo[bi]
            vw, ot, rc = vws[bi % 2], ots[bi % 2], rcs[bi % 2]
            PT, pO = PTs[m % 2], naO[:, 0:65]
            nch = nwin + 2
            for c in range(nch):
                rhs = vw[:, c, hh, :] if c < nwin else vctx[:, c - nwin, hh, :]
                k.mm(pO, PT[:, c, :], rhs, start=(c == 0), stop=(c == nch - 1))
            k.recip(rc[:, hh:hh + 1], pO[:, 64:65])
            k.ts(ot[:, hh * 64:(hh + 1) * 64], pO[:, 0:64], rc[:, hh:hh + 1], ALU.mult)

        def na_epi(bi):
            tok0 = binfo[bi][0]
            ot, mx = ots[bi % 2], mxs[bi % 2]
            for t4 in range(4):
                k.tr(trb[:, 512 + t4 * 128:512 + (t4 + 1) * 128], ot[:, t4 * 128:(t4 + 1) * 128], self.identb)
            k.copy(mx, trb[:, 512:1024], eng="act")
            k.dma(self.mixT_d[4:8, :, tok0:tok0 + 128].rearrange("c p n -> p c n"),
                  mx.rearrange("p (c n) -> p c n", c=4))

        npair = max(len(mitems), len(nitems))
        pend_fin = []
        pend_epi = None
        for n in range(npair):
            has_m = n < len(mitems)
            has_n = n < len(nitems)
            if has_n:
                bi, hh = nitems[n]
                if hh == 0:
                    na_prologue(bi)
                nch = binfo[bi][1] + 2
                n1 = min(4, nch)
                na_S(n, 0, n1)
            if has_m:
                ml_A(n)
            if has_n:
                na_exp(n, 0, n1)
            if has_m:
                ml_B(n)
            if has_n and nch > 4:
                na_S(n, 4, nch)
            if has_m:
                ml_C(n)
            if has_n:
                if nch > 4:
                    na_exp(n, 4, nch)
                if pend_epi is not None:
                    na_epi(pend_epi)
                    pend_epi = None
                na_PV(n)
                if hh == 7:
                    pend_epi = bi
            for c in pend_fin:
                ml_fin(c)
            pend_fin = []
            if has_m and mitems[n][2] == 3:
                done = ml_E(mitems[n][0], mitems[n][1])
                if done is not None:
                    pend_fin.append(done)
        if pend_epi is not None:
            na_epi(pend_epi)
        for c in pend_fin:
            ml_fin(c)
        if self.stopped(layer, "D2"):
            k.dma(self.hs_dbg, hsum)
        k.barrier()


Prog.phase_mix = _phase_mix
```
